# Optimizing a Trainium2 kernel written in Bass

```python
import math
import jax
import jax.numpy as jnp
from jax import lax
import numpy as np

D_MODEL = 1024
BATCH = 4
SEQ = 4096
DEPTH = 4

GRID_W = 64
NORM_EPS = 1e-6
ROPE_THETA = 10000.0
Q_BLOCK = 128

DN_HEADS = 6
DN_DK = 64
DN_DV = 64
DN_CHUNK = 64
DN_CONV_W = 5
GQA_HEADS = 6
GQA_KV_HEADS = 2
GQA_HD = 64
MLA_HEADS = 4
MLA_Q_RANK = 192
MLA_KV_RANK = 128
MLA_NOPE = 64
MLA_ROPE = 32
MLA_V = 64
N_GROUPS = 4
EXPERTS_PER_GROUP = 8
N_EXPERTS = N_GROUPS * EXPERTS_PER_GROUP
TOP_K_IN_GROUP = 2
D_EXPERT = 256
MOE_BLOCK = 128

DN_WIDTH = DN_HEADS * DN_DV
GQA_WIDTH = GQA_HEADS * GQA_HD
MLA_WIDTH = MLA_HEADS * MLA_V
D_MIX = DN_WIDTH + GQA_WIDTH + MLA_WIDTH
IN_SPLIT_SIZES = (2 * DN_HEADS * DN_DK + DN_WIDTH, DN_WIDTH, 2 * DN_HEADS, 2 * DN_HEADS,
                  GQA_WIDTH, GQA_KV_HEADS * GQA_HD, GQA_KV_HEADS * GQA_HD,
                  MLA_Q_RANK, MLA_KV_RANK, MLA_ROPE)
IN_COLS = sum(IN_SPLIT_SIZES)

kernel_name = "hybrid_parallel_heads_moe_encoder"


def rms_norm(x, gain):
    xf = x.astype(jnp.float32)
    y = xf * lax.rsqrt(jnp.mean(jnp.square(xf), axis=-1, keepdims=True) + NORM_EPS)
    return (y * gain.astype(jnp.float32)).astype(x.dtype)


def l2_normalize(x):
    xf = x.astype(jnp.float32)
    return (xf * lax.rsqrt(jnp.sum(jnp.square(xf), axis=-1, keepdims=True) + NORM_EPS)).astype(x.dtype)


def axial_rope_table(seq_len, rot_dim):
    rows = seq_len // GRID_W
    row = jnp.broadcast_to(jnp.arange(rows, dtype=jnp.float32)[:, None], (rows, GRID_W)).reshape(-1)
    col = jnp.broadcast_to(jnp.arange(GRID_W, dtype=jnp.float32)[None, :], (rows, GRID_W)).reshape(-1)
    n_freq = rot_dim // 4
    inv_freq = ROPE_THETA ** (-jnp.arange(n_freq, dtype=jnp.float32) / n_freq)
    ang = jnp.concatenate([row[:, None] * inv_freq, col[:, None] * inv_freq], axis=-1)
    return jnp.cos(ang), jnp.sin(ang)


def apply_rope(x, cos, sin):
    x1, x2 = jnp.split(x, 2, axis=-1)
    c = cos[:, None, :].astype(x.dtype)
    s = sin[:, None, :].astype(x.dtype)
    return jnp.concatenate([x1 * c - x2 * s, x1 * s + x2 * c], axis=-1)


def blocked_attention(q, k, v, scale):
    B, KH, G, S, Dk = q.shape
    Dv = v.shape[-1]
    nb = S // Q_BLOCK
    qb = jnp.moveaxis(q.reshape(B, KH, G, nb, Q_BLOCK, Dk), 3, 0)

    def attend(q_blk):
        s = jnp.einsum('bhgqd,bhkd->bhgqk', q_blk, k, preferred_element_type=jnp.float32) * scale
        p = jax.nn.softmax(s, axis=-1)
        return jnp.einsum('bhgqk,bhkd->bhgqd', p.astype(v.dtype), v)

    o = lax.map(attend, qb)
    return jnp.moveaxis(o, 0, 3).reshape(B, KH, G, S, Dv)


def depthwise_conv_centred(x, w):
    pad = (DN_CONV_W - 1) // 2
    return lax.conv_general_dilated(x, w[:, None, :].astype(x.dtype), window_strides=(1,),
                                    padding=[(pad, pad)], dimension_numbers=('NWC', 'WIO', 'NWC'),
                                    feature_group_count=x.shape[-1])


def gated_delta_rule(q, k, v, g, beta):
    out_dtype = v.dtype
    f32 = jnp.float32
    B, S, H, Dk = q.shape
    Dv = v.shape[-1]
    C = DN_CHUNK
    nc = S // C

    def chunks(t):
        t = t.astype(f32).reshape((B, nc, C, H) + t.shape[3:])
        return jnp.moveaxis(t, 3, 1)

    q = chunks(q) * (Dk ** -0.5)
    k = chunks(k)
    v = chunks(v)
    g = chunks(g)
    beta = chunks(beta)
    gc = jnp.cumsum(g, axis=-1)
    lower = jnp.tril(jnp.ones((C, C), dtype=bool))
    strict = jnp.tril(jnp.ones((C, C), dtype=bool), -1)
    decay = jnp.exp(jnp.where(lower, gc[..., :, None] - gc[..., None, :], -jnp.inf))
    kb = k * beta[..., None]
    vb = v * beta[..., None]
    lmat = jnp.where(strict, jnp.einsum('bhnid,bhnjd->bhnij', kb, k) * decay, 0.0)
    rhs = jnp.concatenate([vb, kb * jnp.exp(gc)[..., None]], axis=-1)
    sol = lax.linalg.triangular_solve(lmat + jnp.eye(C, dtype=f32), rhs, left_side=True,
                                      lower=True, unit_diagonal=True)
    u, w = sol[..., :Dv], sol[..., Dv:]
    a_qk = jnp.einsum('bhnid,bhnjd->bhnij', q, k) * decay
    g_last = gc[..., -1]
    k_tail = k * jnp.exp(g_last[..., None] - gc)[..., None]
    q_dec = q * jnp.exp(gc)[..., None]
    xs = tuple(jnp.moveaxis(t, 2, 0) for t in (q_dec, k_tail, u, w, a_qk, jnp.exp(g_last)))

    def step(state, inp):
        qd, kt, uc, wc, aq, dl = inp
        v_new = uc - jnp.einsum('bhck,bhkv->bhcv', wc, state)
        o = jnp.einsum('bhck,bhkv->bhcv', qd, state) + jnp.einsum('bhcj,bhjv->bhcv', aq, v_new)
        state = state * dl[..., None, None] + jnp.einsum('bhck,bhcv->bhkv', kt, v_new)
        return state, o

    state0 = jnp.zeros((B, H, Dk, Dv), f32)
    _, o = lax.scan(step, state0, xs)
    o = jnp.transpose(o, (1, 0, 3, 2, 4)).reshape(B, S, H, Dv)
    return o.astype(out_dtype)


def deltanet_group(qkv, z, b_raw, a_raw, conv_w, a_log, dt_bias, out_gain):
    B, S, _ = qkv.shape
    H = DN_HEADS
    f32 = jnp.float32
    qkv = jax.nn.silu(depthwise_conv_centred(qkv, conv_w))
    q, k, v = jnp.split(qkv, [H * DN_DK, 2 * H * DN_DK], axis=-1)
    q = l2_normalize(q.reshape(B, S, H, DN_DK))
    k = l2_normalize(k.reshape(B, S, H, DN_DK))
    v = v.reshape(B, S, H, DN_DV)
    beta = jax.nn.sigmoid(b_raw.astype(f32)).reshape(B, S, 2, H)
    g = -jnp.exp(a_log.astype(f32)) * jax.nn.softplus(a_raw.astype(f32).reshape(B, S, 2, H) + dt_bias.astype(f32))
    o_fwd = gated_delta_rule(q, k, v, g[:, :, 0], beta[:, :, 0])
    flip = lambda t: jnp.flip(t, axis=1)
    o_bwd = flip(gated_delta_rule(flip(q), flip(k), flip(v), flip(g[:, :, 1]), flip(beta[:, :, 1])))
    o = rms_norm(o_fwd + o_bwd, out_gain) * jax.nn.silu(z.reshape(B, S, H, DN_DV))
    return o.reshape(B, S, DN_WIDTH)


def gqa_group(q, k, v, q_gain, k_gain, cos, sin):
    B, S, _ = q.shape
    G = GQA_HEADS // GQA_KV_HEADS
    q = apply_rope(rms_norm(q.reshape(B, S, GQA_HEADS, GQA_HD), q_gain), cos, sin)
    k = apply_rope(rms_norm(k.reshape(B, S, GQA_KV_HEADS, GQA_HD), k_gain), cos, sin)
    v = v.reshape(B, S, GQA_KV_HEADS, GQA_HD)
    qh = q.reshape(B, S, GQA_KV_HEADS, G, GQA_HD).transpose(0, 2, 3, 1, 4)
    o = blocked_attention(qh, k.transpose(0, 2, 1, 3), v.transpose(0, 2, 1, 3), GQA_HD ** -0.5)
    return o.transpose(0, 3, 1, 2, 4).reshape(B, S, GQA_WIDTH)


def mla_group(cq, ckv, kr, q_lat_gain, kv_lat_gain, w_uq, w_ukv, qn_gain, qr_gain, kn_gain, kr_gain, cos, sin):
    B, S, _ = cq.shape
    H = MLA_HEADS
    q = (rms_norm(cq, q_lat_gain) @ w_uq).reshape(B, S, H, MLA_NOPE + MLA_ROPE)
    kv = (rms_norm(ckv, kv_lat_gain) @ w_ukv).reshape(B, S, H, MLA_NOPE + MLA_V)
    q_nope = rms_norm(q[..., :MLA_NOPE], qn_gain)
    q_rope = apply_rope(rms_norm(q[..., MLA_NOPE:], qr_gain), cos, sin)
    k_nope = rms_norm(kv[..., :MLA_NOPE], kn_gain)
    v = kv[..., MLA_NOPE:]
    k_rope = apply_rope(rms_norm(kr[:, :, None, :], kr_gain), cos, sin)
    qh = jnp.concatenate([q_nope, q_rope], axis=-1).transpose(0, 2, 1, 3)[:, :, None]
    kh = jnp.concatenate([k_nope, jnp.broadcast_to(k_rope, (B, S, H, MLA_ROPE))], axis=-1).transpose(0, 2, 1, 3)
    o = blocked_attention(qh, kh, v.transpose(0, 2, 1, 3), (MLA_NOPE + MLA_ROPE) ** -0.5)
    return o[:, :, 0].transpose(0, 2, 1, 3).reshape(B, S, MLA_WIDTH)


def parallel_head_mixer(h, w_in, dn_conv, dn_a_log, dn_dt_bias, dn_out_g, gqa_q_g, gqa_k_g,
                        mla_q_lat_g, mla_kv_lat_g, mla_w_uq, mla_w_ukv, mla_qn_g, mla_qr_g,
                        mla_kn_g, mla_kr_g, w_out, rope_gqa, rope_mla):
    proj = h @ w_in
    splits = np.cumsum(IN_SPLIT_SIZES)[:-1].tolist()
    dn_qkv, dn_z, dn_b, dn_a, gq, gk, gv, mcq, mckv, mkr = jnp.split(proj, splits, axis=-1)
    o_a = deltanet_group(dn_qkv, dn_z, dn_b, dn_a, dn_conv, dn_a_log, dn_dt_bias, dn_out_g)
    o_b = gqa_group(gq, gk, gv, gqa_q_g, gqa_k_g, rope_gqa[0], rope_gqa[1])
    o_c = mla_group(mcq, mckv, mkr, mla_q_lat_g, mla_kv_lat_g, mla_w_uq, mla_w_ukv,
                    mla_qn_g, mla_qr_g, mla_kn_g, mla_kr_g, rope_mla[0], rope_mla[1])
    return jnp.concatenate([o_a, o_b, o_c], axis=-1) @ w_out


def hierarchical_moe(h, w_group, b_group, w_router, b_router, w1, w3, w2):
    B, S, D = h.shape
    N = B * S
    f32 = jnp.float32
    xf = h.reshape(N, D)
    g_prob = jax.nn.softmax((xf @ w_group).astype(f32) + b_group.astype(f32), axis=-1)
    g_top_p, g_top = lax.top_k(g_prob, 1)
    e_logits = ((xf @ w_router).astype(f32) + b_router.astype(f32)).reshape(N, N_GROUPS, EXPERTS_PER_GROUP)
    e_sel = jnp.take_along_axis(e_logits, g_top[:, :, None], axis=1)[:, 0]
    e_top_p, e_top = lax.top_k(jax.nn.softmax(e_sel, axis=-1), TOP_K_IN_GROUP)
    e_top_p = e_top_p / jnp.sum(e_top_p, axis=-1, keepdims=True)
    weights = g_top_p * e_top_p
    expert_id = g_top * EXPERTS_PER_GROUP + e_top
    A = N * TOP_K_IN_GROUP
    e_flat = expert_id.reshape(A)
    tok_flat = jnp.repeat(jnp.arange(N, dtype=jnp.int32), TOP_K_IN_GROUP)
    w_flat = weights.reshape(A)
    order = jnp.argsort(e_flat)
    e_sorted = e_flat[order]
    counts = jnp.bincount(e_flat, length=N_EXPERTS)
    padded = ((counts + MOE_BLOCK - 1) // MOE_BLOCK) * MOE_BLOCK
    pad_end = jnp.cumsum(padded)
    pad_start = pad_end - padded
    start = jnp.cumsum(counts) - counts
    dest = pad_start[e_sorted] + (jnp.arange(A, dtype=jnp.int32) - start[e_sorted])
    n_blocks = (A + N_EXPERTS * (MOE_BLOCK - 1) + MOE_BLOCK - 1) // MOE_BLOCK
    P = n_blocks * MOE_BLOCK
    slot_tok = jnp.full((P,), N, dtype=jnp.int32).at[dest].set(tok_flat[order])
    slot_w = jnp.zeros((P,), f32).at[dest].set(w_flat[order])
    block_e = jnp.clip(jnp.searchsorted(pad_end, jnp.arange(n_blocks, dtype=jnp.int32) * MOE_BLOCK, side='right'),
                       0, N_EXPERTS - 1)
    xpad = jnp.concatenate([xf, jnp.zeros((1, D), xf.dtype)], axis=0)

    def expert_block(args):
        tok, e = args
        xb = xpad[tok]
        return (jax.nn.silu(xb @ w1[e]) * (xb @ w3[e])) @ w2[e]

    y = lax.map(expert_block, (slot_tok.reshape(n_blocks, MOE_BLOCK), block_e)).reshape(P, D)
    y = y * slot_w[:, None].astype(y.dtype)
    out = jnp.zeros((N + 1, D), y.dtype).at[slot_tok].add(y)[:N]
    return out.reshape(B, S, D)


def setup_inputs(seed: int = 0) -> dict:
    key = jax.random.key(seed)
    ks = iter(jax.random.split(key, 32))
    f32 = jnp.float32
    L, D = DEPTH, D_MODEL

    def nrm(shape, scale):
        return jax.random.normal(next(ks), shape, f32) * scale

    def gain(shape):
        return 1.0 + 0.02 * jax.random.normal(next(ks), shape, f32)

    x = nrm((BATCH, SEQ, D), 1.0)
    c = nrm((BATCH, D), 1.0)
    ada_w = nrm((L, D, 6 * D), 0.5 * D ** -0.5)
    ada_b = nrm((L, 6 * D), 0.02)
    norm1_g = gain((L, D))
    norm2_g = gain((L, D))
    w_in = nrm((L, D, IN_COLS), D ** -0.5)
    dn_conv = nrm((L, DN_CONV_W, 2 * DN_HEADS * DN_DK + DN_WIDTH), DN_CONV_W ** -0.5)
    dn_a_log = jnp.log(jax.random.uniform(next(ks), (L, 2, DN_HEADS), f32, 1.0, 16.0))
    dt = jnp.exp(jax.random.uniform(next(ks), (L, 2, DN_HEADS), f32, math.log(1e-3), math.log(1e-1)))
    dn_dt_bias = dt + jnp.log(-jnp.expm1(-dt))
    dn_out_g = gain((L, DN_DV))
    gqa_q_g = gain((L, GQA_HD))
    gqa_k_g = gain((L, GQA_HD))
    mla_q_lat_g = gain((L, MLA_Q_RANK))
    mla_kv_lat_g = gain((L, MLA_KV_RANK))
    mla_w_uq = nrm((L, MLA_Q_RANK, MLA_HEADS * (MLA_NOPE + MLA_ROPE)), MLA_Q_RANK ** -0.5)
    mla_w_ukv = nrm((L, MLA_KV_RANK, MLA_HEADS * (MLA_NOPE + MLA_V)), MLA_KV_RANK ** -0.5)
    mla_qn_g = gain((L, MLA_NOPE))
    mla_qr_g = gain((L, MLA_ROPE))
    mla_kn_g = gain((L, MLA_NOPE))
    mla_kr_g = gain((L, MLA_ROPE))
    w_out = nrm((L, D_MIX, D), D_MIX ** -0.5)
    moe_w_group = nrm((L, D, N_GROUPS), D ** -0.5)
    moe_b_group = nrm((L, N_GROUPS), 0.01)
    moe_w_router = nrm((L, D, N_EXPERTS), D ** -0.5)
    moe_b_router = nrm((L, N_EXPERTS), 0.01)
    moe_w1 = nrm((L, N_EXPERTS, D, D_EXPERT), D ** -0.5)
    moe_w3 = nrm((L, N_EXPERTS, D, D_EXPERT), D ** -0.5)
    moe_w2 = nrm((L, N_EXPERTS, D_EXPERT, D), D_EXPERT ** -0.5)
    return {"x": x, "c": c, "ada_w": ada_w, "ada_b": ada_b, "norm1_g": norm1_g, "norm2_g": norm2_g,
            "w_in": w_in, "dn_conv": dn_conv, "dn_a_log": dn_a_log, "dn_dt_bias": dn_dt_bias,
            "dn_out_g": dn_out_g, "gqa_q_g": gqa_q_g, "gqa_k_g": gqa_k_g,
            "mla_q_lat_g": mla_q_lat_g, "mla_kv_lat_g": mla_kv_lat_g, "mla_w_uq": mla_w_uq,
            "mla_w_ukv": mla_w_ukv, "mla_qn_g": mla_qn_g, "mla_qr_g": mla_qr_g, "mla_kn_g": mla_kn_g,
            "mla_kr_g": mla_kr_g, "w_out": w_out, "moe_w_group": moe_w_group, "moe_b_group": moe_b_group,
            "moe_w_router": moe_w_router, "moe_b_router": moe_b_router, "moe_w1": moe_w1,
            "moe_w3": moe_w3, "moe_w2": moe_w2}


def reference(x, c, ada_w, ada_b, norm1_g, norm2_g, w_in, dn_conv, dn_a_log, dn_dt_bias, dn_out_g,
              gqa_q_g, gqa_k_g, mla_q_lat_g, mla_kv_lat_g, mla_w_uq, mla_w_ukv, mla_qn_g, mla_qr_g,
              mla_kn_g, mla_kr_g, w_out, moe_w_group, moe_b_group, moe_w_router, moe_b_router,
              moe_w1, moe_w3, moe_w2):
    S = x.shape[1]
    rope_gqa = axial_rope_table(S, GQA_HD)
    rope_mla = axial_rope_table(S, MLA_ROPE)
    c_act = jax.nn.silu(c)
    for l in range(DEPTH):
        mod = (c_act @ ada_w[l] + ada_b[l])[:, None, :]
        sh1, sc1, gt1, sh2, sc2, gt2 = jnp.split(mod, 6, axis=-1)
        h = rms_norm(x, norm1_g[l]) * (1 + sc1) + sh1
        x = x + gt1 * parallel_head_mixer(h, w_in[l], dn_conv[l], dn_a_log[l], dn_dt_bias[l], dn_out_g[l],
                                          gqa_q_g[l], gqa_k_g[l], mla_q_lat_g[l], mla_kv_lat_g[l],
                                          mla_w_uq[l], mla_w_ukv[l], mla_qn_g[l], mla_qr_g[l],
                                          mla_kn_g[l], mla_kr_g[l], w_out[l], rope_gqa, rope_mla)
        h = rms_norm(x, norm2_g[l]) * (1 + sc2) + sh2
        x = x + gt2 * hierarchical_moe(h, moe_w_group[l], moe_b_group[l], moe_w_router[l], moe_b_router[l],
                                       moe_w1[l], moe_w3[l], moe_w2[l])
    return x
```

```python
import math
import os
import numpy as np
from contextlib import ExitStack
import concourse.bass as bass
import concourse.mybir as mybir
from concourse.bass_utils import run_bass_kernel_spmd

F32 = mybir.dt.float32
BF16 = mybir.dt.bfloat16
I32 = mybir.dt.int32
AF = mybir.ActivationFunctionType
ALU = mybir.AluOpType
AX = mybir.AxisListType
NDS = 20
import re
PSUM_RE = re.compile(r'p\d+_')

D = 1024
INC = 2552
EPS = 1e-6


class FW:
    def __init__(self, nc, stack):
        self.nc = nc
        self.stack = stack
        self.eng = {'pe': nc.tensor, 'act': nc.scalar, 'dve': nc.vector, 'pool': nc.gpsimd, 'sp': nc.sync}
        self.sem = {}
        self.cnt = {}
        for e in self.eng:
            self.sem[('c', e)] = stack.enter_context(nc.semaphore('s_' + e))
            self.cnt[('c', e)] = 0
        self.dring = {}
        self.dnext = {}
        for q in ('sp', 'act', 'pool'):
            self.dring[q] = []
            for i in range(NDS):
                k = ('d', q, i)
                self.sem[k] = stack.enter_context(nc.semaphore(f'd_{q}_{i}'))
                self.cnt[k] = 0
                self.dring[q].append(k)
            self.dnext[q] = 0
        self.waited = {e: {} for e in self.eng}
        self.W = {}
        self.R = {}
        self.ninst = 0

    @staticmethod
    def _res(x):
        if isinstance(x, tuple):
            if len(x) == 2 and not isinstance(x[0], (str, int)):
                return x[1]
            return x
        if isinstance(x, str):
            return x
        return x.name

    @staticmethod
    def ap(x):
        if isinstance(x, tuple):
            return x[0]
        return x

    def _wait(self, e, toks):
        eng = self.eng[e]
        for k, v in toks.items():
            if self.waited[e].get(k, 0) < v:
                eng.wait_ge(self.sem[k], v)
                self.waited[e][k] = v
                self.ninst += 1

    def _deps(self, reads, writes):
        toks = {}

        def add(d):
            for k, v in d.items():
                if toks.get(k, 0) < v:
                    toks[k] = v
        for r in reads:
            add(self.W.get(r, {}))
        for w in writes:
            add(self.W.get(w, {}))
            add(self.R.get(w, {}))
        return toks

    def _commit(self, tok, reads, writes):
        k, v = tok
        for r in reads:
            d = self.R.setdefault(r, {})
            d[k] = max(d.get(k, 0), v)
        for w in writes:
            self.W[w] = {k: v}
            self.R[w] = {}

    def op(self, e, fn, reads, writes):
        reads = [self._res(r) for r in reads if r is not None and not isinstance(r, (int, float))]
        writes = [self._res(w) for w in writes]
        writes = writes + [r for r in reads if isinstance(r, str) and PSUM_RE.match(r)]
        toks = self._deps(reads, writes)
        if e == 'pe':
            toks.pop(('c', 'pe'), None)
        self._wait(e, toks)
        ins = fn()
        k = ('c', e)
        self.cnt[k] += 1
        ins.then_inc(self.sem[k], 1)
        self.ninst += 1
        self._commit((k, self.cnt[k]), reads, writes)

    def dma(self, q, out, in_, extra_reads=(), extra_writes=()):
        reads = [self._res(in_)] + [self._res(r) for r in extra_reads]
        writes = [self._res(out)] + [self._res(w) for w in extra_writes]
        toks = self._deps(reads, writes)
        k = self.dring[q][self.dnext[q]]
        self.dnext[q] = (self.dnext[q] + 1) % NDS
        if self.cnt[k] > 0:
            toks[k] = max(toks.get(k, 0), self.cnt[k])
        self._wait(q, toks)
        ins = self.eng[q].dma_start(out=self.ap(out), in_=self.ap(in_))
        self.cnt[k] += 16
        ins.then_inc(self.sem[k], 16)
        self.ninst += 1
        self._commit((k, self.cnt[k]), reads, writes)

    def barrier(self, engines=('pe', 'act', 'dve', 'pool', 'sp')):
        toks = {k: v for k, v in self.cnt.items() if v > 0}
        for e in engines:
            self._wait(e, dict(toks))

    def act(self, out, in_, func, bias=None, scale=1.0, accum_out=None, e='act'):
        kw = {}
        if bias is not None:
            kw['bias'] = self.ap(bias)
        if accum_out is not None:
            kw['accum_out'] = self.ap(accum_out)
        sc = self.ap(scale) if not isinstance(scale, (int, float)) else scale
        wr = [out] + ([accum_out] if accum_out is not None else [])
        self.op(e, lambda: self.eng[e].activation(out=self.ap(out), in_=self.ap(in_), func=func, scale=sc, **kw),
                [in_, bias, scale], wr)

    def tt(self, out, in0, in1, op, e='dve'):
        self.op(e, lambda: self.eng[e].tensor_tensor(out=self.ap(out), in0=self.ap(in0), in1=self.ap(in1), op=op),
                [in0, in1], [out])

    def ts(self, out, in0, s1, s2, op0, op1=None, e='dve'):
        a1 = self.ap(s1) if not isinstance(s1, (int, float)) else s1
        a2 = (self.ap(s2) if not isinstance(s2, (int, float)) else s2) if s2 is not None else None
        kw = {}
        if op1 is not None:
            kw['op1'] = op1
        self.op(e, lambda: self.eng[e].tensor_scalar(out=self.ap(out), in0=self.ap(in0), scalar1=a1, scalar2=a2,
                                                     op0=op0, **kw), [in0, s1, s2], [out])

    def stt(self, out, in0, scalar, in1, op0, op1, e='dve'):
        a = self.ap(scalar) if not isinstance(scalar, (int, float)) else scalar
        self.op(e, lambda: self.eng[e].scalar_tensor_tensor(out=self.ap(out), in0=self.ap(in0), scalar=a,
                                                            in1=self.ap(in1), op0=op0, op1=op1),
                [in0, scalar, in1], [out])

    def red(self, out, in_, op=ALU.add, e='dve'):
        self.op(e, lambda: self.eng[e].tensor_reduce(out=self.ap(out), in_=self.ap(in_), axis=AX.X, op=op),
                [in_], [out])

    def recip(self, out, in_):
        self.op('dve', lambda: self.nc.vector.reciprocal(out=self.ap(out), in_=self.ap(in_)), [in_], [out])

    def copy(self, out, in_, e='dve'):
        if e == 'act':
            self.op(e, lambda: self.eng[e].copy(out=self.ap(out), in_=self.ap(in_)), [in_], [out])
        else:
            self.op(e, lambda: self.eng[e].tensor_copy(out=self.ap(out), in_=self.ap(in_)), [in_], [out])

    def memset(self, out, val, e='pool'):
        self.op(e, lambda: self.eng[e].memset(self.ap(out), val), [], [out])

    def mm(self, items, extra_reads=()):
        rd = list(extra_reads)
        wr = []
        for o, l, r, _, _ in items:
            rd += [l, r]
            wr.append(o)

        def fn():
            ins = None
            for o, l, r, st, sp in items:
                ins = self.nc.tensor.matmul(self.ap(o), self.ap(l), self.ap(r), start=st, stop=sp)
            return ins
        self.op('pe', fn, rd, wr)

    def mmk(self, out, pairs):
        n = len(pairs)
        self.mm([(out, l, r, i == 0, i == n - 1) for i, (l, r) in enumerate(pairs)])

    def tr(self, items):
        rd = []
        wr = []
        for o, i, idt in items:
            rd += [i, idt]
            wr.append(o)

        def fn():
            ins = None
            for o, i, idt in items:
                ins = self.nc.tensor.transpose(self.ap(o), self.ap(i), self.ap(idt))
            return ins
        self.op('pe', fn, rd, wr)


def bc(ap, axis, n):
    a = ap.unsqueeze(axis)
    shp = list(a.shape)
    shp[axis] = n
    return a.to_broadcast(shp)


def build(S=4096, depth=4, stop_after=None, dbg=False):
    NT = S // 128
    NQB = S // 512
    nc = bass.Bass("TRN2", target_bir_lowering=False)

    def din(name, shape, dt=F32):
        return nc.dram_tensor(name, list(shape), dt, kind="ExternalInput").ap()

    def scr(name, shape, dt):
        return nc.dram_tensor(name, list(shape), dt, kind="Internal").ap()

    x_in = din("x", [S, D])
    c_pk = din("c_pk", [128, 8])
    ada_w = din("ada_w", [depth, D, 6 * D])
    ada_b = din("ada_b", [depth, 128, 6 * D])
    g1_d = din("g1", [depth, 128, D])
    g2_d = din("g2", [depth, 128, D])
    w_in = din("w_in", [depth, D, INC])
    w_out = din("w_out", [depth, D, D])
    gq_g = din("gq_g", [depth, 128, 64])
    gk_g = din("gk_g", [depth, 128, 64])
    mql_g = din("mql_g", [depth, 192, 1])
    mkvl_g = din("mkvl_g", [depth, 128, 1])
    w_uq = din("w_uq", [depth, 192, 384])
    w_ukv = din("w_ukv", [depth, 128, 512])
    mqn_g = din("mqn_g", [depth, 128, 64])
    mqr_g = din("mqr_g", [depth, 128, 32])
    mkn_g = din("mkn_g", [depth, 128, 64])
    mkr_g = din("mkr_g", [depth, 128, 32])
    cosg_d = din("cosg", [128, NT, 32])
    sing_d = din("sing", [128, NT, 32])
    cosm_d = din("cosm", [128, NT, 16])
    sinm_d = din("sinm", [128, NT, 16])
    identf_d = din("identf", [128, 128])
    w_r = din("w_r", [depth, D, 36])
    b_r = din("b_r", [depth, 128, 36])
    moe_w1 = din("moe_w1", [depth, 32, D, 256])
    moe_w3 = din("moe_w3", [depth, 32, D, 256])
    moe_w2 = din("moe_w2", [depth, 32, 256, D])
    wconv_d = din("wconv", [depth, 128, 5 * 1152])
    alog_d = din("alog", [depth, 128, 12])
    dtb_d = din("dtb", [depth, 128, 12])
    dng_d = din("dng", [depth, 128, 64])
    masks_d = din("masks", [128, 9, 128])
    y_out = nc.dram_tensor("y", [S, D], F32, kind="ExternalOutput").ap()

    qTg = scr("qTg", [3, 128, S], BF16)
    kTg = scr("kTg", [128, S], BF16)
    vg = scr("vg", [S, 130], BF16)
    qTm = scr("qTm", [4, 96, S], BF16)
    kTm = scr("kTm", [4, 96, S], BF16)
    vm = scr("vm", [S, 260], BF16)
    omix = scr("omix", [S, D], BF16)
    xs = [scr("xs0", [S, D], F32), scr("xs1", [S, D], F32), scr("xs2", [S, D], F32)]
    h2Td = scr("h2Td", [128, 8, S], BF16)
    Gd = scr("Gd", [S, 32], F32)
    dnpre = scr("dnpre", [S + 4, 1152], F32)
    dnz = scr("dnz", [S, 384], F32)
    dU = scr("dU", [S, 768], F32)
    dWT = scr("dWT", [NT, 64, 12 * 128], BF16)
    dAT = scr("dAT", [NT, 128, 12 * 128], BF16)
    dKT = scr("dKT", [S, 768], BF16)
    dQT = scr("dQT", [NT, 64, 6 * 128], BF16)
    dE = scr("dE", [S, 24], F32)
    dO = scr("dO", [2, S, 384], F32)
    dbg_out = None
    if dbg:
        dbg_out = nc.dram_tensor("dbg", [S, D], F32, kind="ExternalOutput").ap()

    with ExitStack() as st0:
        fw = FW(nc, st0)

        uid = [0]

        def sb(stk, name, shape, dt):
            uid[0] += 1
            return stk.enter_context(nc.sbuf_tensor(f"s{uid[0]}_{name}", list(shape), dt))

        def ps(stk, name, shape, dt=F32):
            uid[0] += 1
            return stk.enter_context(nc.psum_tensor(f"p{uid[0]}_{name}", list(shape), dt))

        identf = sb(st0, "identf", [128, 128], F32)
        identb = sb(st0, "identb", [128, 128], BF16)
        epsT = sb(st0, "epsT", [128, 1], F32)
        cactB = sb(st0, "cactB", [128, 8, 128], F32)
        mod = sb(st0, "mod", [128, 6 * D], F32)
        A1 = sb(st0, "A1", [128, D], F32)
        A2 = sb(st0, "A2", [128, D], F32)
        fw.dma('sp', identf[:], identf_d)
        fw.memset(epsT[:], EPS)
        oneT = sb(st0, "oneT", [128, 1], F32)
        fw.memset(oneT[:], 1.0)
        onesf = sb(st0, "onesf", [128, 128], F32)
        fw.memset(onesf[:], 1.0)
        masks = sb(st0, "masks", [128, 9, 128], F32)
        fw.dma('sp', masks[:], masks_d)
        gT = sb(st0, "gT", [128, NT, 12], F32)
        betaT = sb(st0, "betaT", [128, NT, 12], F32)
        with ExitStack() as stp:
            zt = sb(stp, "zt", [128, 1152], F32)
            fw.memset(zt[:], 0.0)
            fw.dma('sp', dnpre[0:2, :], zt[0:2, :])
            fw.dma('sp', dnpre[S + 2:S + 4, :], zt[0:2, :])
            fw.barrier()
        fw.copy(identb[:], identf[:], e='dve')
        with ExitStack() as stp:
            ct = sb(stp, "ct", [128, 8], F32)
            ce = sb(stp, "ce", [128, 8], F32)
            fw.dma('sp', ct[:], c_pk)
            fw.act(ce[:], ct[:], AF.Exp, scale=-1.0)
            fw.ts(ce[:], ce[:], 1.0, None, ALU.add)
            fw.recip(ce[:], ce[:])
            fw.tt(ce[:], ce[:], ct[:], ALU.mult)
            fw.copy(cactB[:], bc(ce[:, :], 2, 128))
            fw.barrier()

        for l in range(depth):
            x_src = x_in if l == 0 else xs[1 + (l - 1) % 2]
            x_dst = y_out if l == depth - 1 else xs[1 + l % 2]
            last = (l == depth - 1)
            with ExitStack() as stp:
                psM = [ps(stp, f"psM{i}", [128, 512]) for i in range(2)]
                awt = [sb(stp, f"awt{i}", [128, 8, 512], F32) for i in range(2)]
                abt = sb(stp, "abt", [128, 6 * D], F32)
                g1t = sb(stp, "g1t", [128, D], F32)
                g2t = sb(stp, "g2t", [128, D], F32)
                fw.dma('sp', abt[:], ada_b[l])
                fw.dma('sp', g1t[:], g1_d[l])
                fw.dma('sp', g2t[:], g2_d[l])
                for cb in range(12):
                    a = awt[cb % 2]
                    fw.dma('sp', a[:], ada_w[l][:, cb * 512:(cb + 1) * 512].rearrange("(k p) n -> p k n", p=128))
                    fw.mmk(psM[cb % 2][:], [(cactB[:, k, :], a[:, k, :]) for k in range(8)])
                    fw.tt(mod[:, cb * 512:(cb + 1) * 512], psM[cb % 2][:], abt[:, cb * 512:(cb + 1) * 512], ALU.add)
                fw.stt(A1[:], mod[:, D:2 * D], 1.0, g1t[:], ALU.add, ALU.mult)
                fw.stt(A2[:], mod[:, 4 * D:5 * D], 1.0, g2t[:], ALU.add, ALU.mult)
                fw.barrier()
            sh1 = mod[:, 0:D]
            gt1 = mod[:, 2 * D:3 * D]
            sh2 = mod[:, 3 * D:4 * D]
            gt2 = mod[:, 5 * D:6 * D]

            with ExitStack() as stp:
                win = sb(stp, "win", [128, 8, INC], BF16)
                cosg = sb(stp, "cosg", [128, NT, 32], F32)
                sing = sb(stp, "sing", [128, NT, 32], F32)
                cosm = sb(stp, "cosm", [128, NT, 16], F32)
                sinm = sb(stp, "sinm", [128, NT, 16], F32)
                fw.dma('sp', cosg[:], cosg_d)
                fw.dma('sp', sing[:], sing_d)
                fw.dma('sp', cosm[:], cosm_d)
                fw.dma('sp', sinm[:], sinm_d)
                wst = [sb(stp, f"wst{i}", [128, INC], F32) for i in range(2)]
                for k in range(8):
                    fw.dma('sp', wst[k % 2][:], w_in[l][k * 128:(k + 1) * 128, :])
                    fw.copy(win[:, k, :], wst[k % 2][:], e=('pool' if k % 2 else 'dve'))
                wuq = sb(stp, "wuq", [128, 2, 384], BF16)
                wukv = sb(stp, "wukv", [128, 512], BF16)
                wuqs = sb(stp, "wuqs", [128, 2, 384], F32)
                wukvs = sb(stp, "wukvs", [128, 512], F32)
                gql = sb(stp, "gql", [128, 2], F32)
                gkvl = sb(stp, "gkvl", [128, 1], F32)
                fw.dma('sp', wuqs[:, 0, :], w_uq[l][0:128, :])
                fw.dma('sp', wuqs[0:64, 1, :], w_uq[l][128:192, :])
                fw.dma('sp', wukvs[:], w_ukv[l])
                fw.dma('sp', gql[:, 0:1], mql_g[l][0:128, :])
                fw.dma('sp', gql[0:64, 1:2], mql_g[l][128:192, :])
                fw.dma('sp', gkvl[:], mkvl_g[l])
                fw.ts(wuq[:, 0, :], wuqs[:, 0, :], gql[:, 0:1], None, ALU.mult)
                fw.ts(wuq[0:64, 1, :], wuqs[0:64, 1, :], gql[0:64, 1:2], None, ALU.mult)
                fw.ts(wukv[:], wukvs[:], gkvl[:, 0:1], None, ALU.mult)
                gains = {}
                for nm, dd, w_ in (("gq", gq_g, 64), ("gk", gk_g, 64), ("mqn", mqn_g, 64), ("mqr", mqr_g, 32),
                                   ("mkn", mkn_g, 64), ("mkr", mkr_g, 32)):
                    gains[nm] = sb(stp, "gn_" + nm, [128, w_], F32)
                    fw.dma('sp', gains[nm][:], dd[l])

                xt = [sb(stp, f"xt{i}", [128, D], F32) for i in range(2)]
                junk = sb(stp, "junk", [128, D], F32)
                ss = sb(stp, "ss", [128, 1], F32)
                hh = sb(stp, "hh", [128, D], F32)
                hT = sb(stp, "hT", [128, 8, 128], BF16)
                pr = sb(stp, "pr", [128, INC], F32)
                tmpa = sb(stp, "tmpa", [128, 512], F32)
                tmpb = sb(stp, "tmpb", [128, 512], F32)
                nrm = sb(stp, "nrm", [128, 512], F32)
                s6 = sb(stp, "s6", [128, 16], F32)
                qb_ = sb(stp, "qb_", [128, 384], BF16)
                kb_ = sb(stp, "kb_", [128, 128], BF16)
                vb_ = sb(stp, "vb_", [128, 2, 65], BF16)
                qTs = sb(stp, "qTs", [128, 3, 128], BF16)
                kTs = sb(stp, "kTs", [128, 128], BF16)
                cqn = sb(stp, "cqn", [128, 192], BF16)
                ckvn = sb(stp, "ckvn", [128, 128], BF16)
                cqT = sb(stp, "cqT", [128, 2, 128], BF16)
                ckvT = sb(stp, "ckvT", [128, 128], BF16)
                qup = sb(stp, "qup", [128, 384], F32)
                kvup = sb(stp, "kvup", [128, 512], F32)
                qm = sb(stp, "qm", [128, 4, 96], BF16)
                km = sb(stp, "km", [128, 4, 96], BF16)
                vmb = sb(stp, "vmb", [128, 4, 65], BF16)
                krr = sb(stp, "krr", [128, 32], F32)
                qTms = sb(stp, "qTms", [96, 4, 128], BF16)
                kTms = sb(stp, "kTms", [96, 4, 128], BF16)
                psW = ps(stp, "psW", [128, 8, 128])
                psP = [ps(stp, f"psP{i}", [128, 512]) for i in range(2)]
                psTb = ps(stp, "psTb", [128, 8, 128], BF16)
                psU = ps(stp, "psU", [128, 512])
                fw.memset(vb_[:], 1.0)
                fw.memset(vmb[:], 1.0)
                negA = sb(stp, "negA", [128, 12], F32)
                dtb = sb(stp, "dtb", [128, 12], F32)
                t12 = [sb(stp, f"t12_{i}", [128, 12], F32) for i in range(3)]
                fw.dma('sp', negA[:], alog_d[l])
                fw.dma('sp', dtb[:], dtb_d[l])
                fw.act(negA[:], negA[:], AF.Exp)
                fw.ts(negA[:], negA[:], -1.0, None, ALU.mult)

                def headnorm(src, H, Dh, gain, out, mean):
                    t = tmpa[:, 0:H * Dh].rearrange("p (h d) -> p h d", h=H)
                    fw.tt(t, src, src, ALU.mult)
                    fw.red(s6[:, 0:H], t)
                    fw.act(s6[:, 0:H], s6[:, 0:H], AF.Ln, bias=epsT[:], scale=(1.0 / Dh if mean else 1.0))
                    fw.act(s6[:, 0:H], s6[:, 0:H], AF.Exp, scale=-0.5)
                    fw.tt(t, src, bc(s6[:, 0:H], 2, Dh), ALU.mult)
                    if gain is not None:
                        fw.tt(out, t, bc(gain[:, :], 1, H), ALU.mult, e='pool')
                    else:
                        fw.copy(out, t, e='pool')

                def rope(src, H, R, cs, sn, out):
                    h2 = R // 2
                    x1 = src[:, :, 0:h2]
                    x2 = src[:, :, h2:R]
                    c_ = bc(cs, 1, H)
                    s_ = bc(sn, 1, H)
                    ta = tmpa[:, 0:H * h2].rearrange("p (h d) -> p h d", h=H)
                    tb = tmpb[:, 0:H * h2].rearrange("p (h d) -> p h d", h=H)
                    fw.tt(ta, x1, c_, ALU.mult)
                    fw.tt(tb, x2, s_, ALU.mult, e='pool')
                    fw.tt(out[:, :, 0:h2], ta, tb, ALU.subtract)
                    fw.tt(ta, x1, s_, ALU.mult)
                    fw.tt(tb, x2, c_, ALU.mult, e='pool')
                    fw.tt(out[:, :, h2:R], ta, tb, ALU.add)

                fw.dma('sp', xt[0][:], x_src[0:128, :])
                for i in range(NT):
                    xc = xt[i % 2]
                    if i + 1 < NT:
                        fw.dma('sp', xt[(i + 1) % 2][:], x_src[(i + 1) * 128:(i + 2) * 128, :])
                    fw.act(junk[:], xc[:], AF.Square, accum_out=ss[:])
                    fw.act(ss[:], ss[:], AF.Ln, bias=epsT[:], scale=1.0 / D)
                    fw.act(ss[:], ss[:], AF.Exp, scale=-0.5)
                    fw.stt(hh[:], xc[:], ss[:, 0:1], A1[:], ALU.mult, ALU.mult)
                    fw.tt(hh[:], hh[:], sh1, ALU.add, e='pool')
                    fw.tr([(psW[:, k, :], hh[:, k * 128:(k + 1) * 128], identf[:]) for k in range(8)])
                    fw.copy(hT[:], psW[:], e='act')
                    for cb in range(5):
                        c0 = cb * 512
                        c1 = min(INC, c0 + 512)
                        pp = psP[cb % 2]
                        fw.mmk(pp[:, 0:c1 - c0], [(hT[:, k, :], win[:, k, c0:c1]) for k in range(8)])
                        fw.copy(pr[:, c0:c1], pp[:, 0:c1 - c0], e=('dve' if cb % 2 == 0 else 'act'))
                    tsl = slice(i * 128, (i + 1) * 128)
                    fw.dma('pool', (dnpre[2 + i * 128:2 + (i + 1) * 128, :], ("dnpre", i)), pr[:, 0:1152])
                    fw.dma('pool', (dnz[tsl, :], ("dnz", i)), pr[:, 1152:1536])
                    fw.act(t12[0][:], pr[:, 1536:1548], AF.Exp, scale=-1.0)
                    fw.ts(t12[0][:], t12[0][:], 1.0, None, ALU.add)
                    fw.recip(betaT[:, i, :], t12[0][:])
                    fw.tt(t12[1][:], pr[:, 1548:1560], dtb[:], ALU.add)
                    fw.ts(t12[2][:], t12[1][:], -1.0, None, ALU.mult)
                    fw.tt(t12[2][:], t12[2][:], t12[1][:], ALU.max)
                    fw.act(t12[2][:], t12[2][:], AF.Exp, scale=-1.0)
                    fw.act(t12[2][:], t12[2][:], AF.Ln, bias=oneT[:], scale=1.0)
                    fw.ts(t12[1][:], t12[1][:], 0.0, None, ALU.max)
                    fw.tt(t12[1][:], t12[1][:], t12[2][:], ALU.add)
                    fw.tt(gT[:, i, :], t12[1][:], negA[:], ALU.mult)
                    qv = pr[:, 1560:1944].rearrange("p (h d) -> p h d", h=6)
                    nq = nrm[:, 0:384].rearrange("p (h d) -> p h d", h=6)
                    headnorm(qv, 6, 64, gains["gq"], nq, True)
                    rope(nq, 6, 64, cosg[:, i, :], sing[:, i, :], qb_[:, :].rearrange("p (h d) -> p h d", h=6))
                    fw.tr([(psTb[:, j, :], qb_[:, j * 128:(j + 1) * 128], identb[:]) for j in range(3)])
                    fw.copy(qTs[:], psTb[:, 0:3, :])
                    fw.dma('pool', (qTg[:, :, tsl].rearrange("j p t -> p j t"), ("qTg", i)), qTs[:])
                    kv_ = pr[:, 1944:2072].rearrange("p (h d) -> p h d", h=2)
                    nk = nrm[:, 0:128].rearrange("p (h d) -> p h d", h=2)
                    headnorm(kv_, 2, 64, gains["gk"], nk, True)
                    rope(nk, 2, 64, cosg[:, i, :], sing[:, i, :], kb_[:, :].rearrange("p (h d) -> p h d", h=2))
                    fw.tr([(psTb[:, 3, :], kb_[:], identb[:])])
                    fw.copy(kTs[:], psTb[:, 3, :])
                    fw.dma('pool', (kTg[:, tsl], ("kTg", i)), kTs[:])
                    fw.copy(vb_[:, :, 0:64], pr[:, 2072:2200].rearrange("p (h d) -> p h d", h=2), e='pool')
                    fw.dma('pool', (vg[tsl, :], ("vg", i)), vb_[:, :, :].rearrange("p h d -> p (h d)"))
                    cq = pr[:, 2200:2392].rearrange("p (h d) -> p h d", h=1)
                    headnorm(cq, 1, 192, None, cqn[:, :].rearrange("p (h d) -> p h d", h=1), True)
                    ckv = pr[:, 2392:2520].rearrange("p (h d) -> p h d", h=1)
                    headnorm(ckv, 1, 128, None, ckvn[:, :].rearrange("p (h d) -> p h d", h=1), True)
                    fw.tr([(psTb[:, 4, :], cqn[:, 0:128], identb[:]),
                           (psTb[0:64, 5, :], cqn[:, 128:192], identb[:]),
                           (psTb[:, 6, :], ckvn[:], identb[:])])
                    fw.copy(cqT[:, 0, :], psTb[:, 4, :])
                    fw.copy(cqT[0:64, 1, :], psTb[0:64, 5, :])
                    fw.copy(ckvT[:], psTb[:, 6, :])
                    fw.mm([(psU[:, 0:384], cqT[:, 0, :], wuq[:, 0, :], True, False),
                           (psU[:, 0:384], cqT[0:64, 1, :], wuq[0:64, 1, :], False, True)])
                    fw.copy(qup[:], psU[:, 0:384])
                    fw.mm([(psU[:], ckvT[:], wukv[:], True, True)])
                    fw.copy(kvup[:], psU[:])
                    qu = qup[:, :].rearrange("p (h d) -> p h d", h=4)
                    qmv = qm[:, :, :]
                    n4 = nrm[:, 0:256].rearrange("p (h d) -> p h d", h=4)
                    headnorm(qu[:, :, 0:64], 4, 64, gains["mqn"], qmv[:, :, 0:64], True)
                    n4r = nrm[:, 256:384].rearrange("p (h d) -> p h d", h=4)
                    headnorm(qu[:, :, 64:96], 4, 32, gains["mqr"], n4r, True)
                    rope(n4r, 4, 32, cosm[:, i, :], sinm[:, i, :], qmv[:, :, 64:96])
                    ku = kvup[:, :].rearrange("p (h d) -> p h d", h=4)
                    headnorm(ku[:, :, 0:64], 4, 64, gains["mkn"], km[:, :, 0:64], True)
                    krv = pr[:, 2520:2552].rearrange("p (h d) -> p h d", h=1)
                    n1r = nrm[:, 384:416].rearrange("p (h d) -> p h d", h=1)
                    headnorm(krv, 1, 32, gains["mkr"], n1r, True)
                    rope(n1r, 1, 32, cosm[:, i, :], sinm[:, i, :], krr[:, :].rearrange("p (h d) -> p h d", h=1))
                    fw.copy(km[:, :, 64:96], bc(krr[:, :], 1, 4), e='pool')
                    fw.copy(vmb[:, :, 0:64], ku[:, :, 64:128], e='pool')
                    fw.dma('pool', (vm[tsl, :], ("vm", i)), vmb[:, :, :].rearrange("p h d -> p (h d)"))
                    fw.tr([(psTb[0:96, hh_, :], qm[:, hh_, :], identb[:]) for hh_ in range(4)])
                    fw.copy(qTms[:], psTb[0:96, 0:4, :])
                    fw.dma('pool', (qTm[:, :, tsl].rearrange("h p t -> p h t"), ("qTm", i)), qTms[:])
                    fw.tr([(psTb[0:96, 4 + hh_, :], km[:, hh_, :], identb[:]) for hh_ in range(4)])
                    fw.copy(kTms[:], psTb[0:96, 4:8, :])
                    fw.dma('pool', (kTm[:, :, tsl].rearrange("h p t -> p h t"), ("kTm", i)), kTms[:])
                fw.barrier()
            if stop_after == 'A':
                break

            with ExitStack() as stp:
                kT_all = sb(stp, "kT_all", [128, S], BF16)
                v_all = sb(stp, "v_all", [128, NT, 130], BF16)
                vm_all = sb(stp, "vm_all", [128, NT, 260], BF16)
                kTm_h = [sb(stp, f"kTm_h{i}", [96, S], BF16) for i in range(2)]
                qTt = [sb(stp, f"qTt{i}", [128, 512], BF16) for i in range(2)]
                pT = [sb(stp, f"pT{i}", [128, 2, 512], BF16) for i in range(2)]
                oTs = sb(stp, "oTs", [65, 512], F32)
                rcp = sb(stp, "rcp", [128, 4], F32)
                ob = [sb(stp, f"ob{i}", [128, 4, 64], BF16) for i in range(2)]
                psS = [ps(stp, f"psS{i}", [128, 2, 512]) for i in range(2)]
                acc = [ps(stp, f"acc{i}", [65, 512]) for i in range(2)]
                psO = ps(stp, "psO", [128, 4, 128])
                allq = [("qTg", i) for i in range(NT)]
                fw.dma('sp', kT_all[:], kTg, extra_reads=[("kTg", i) for i in range(NT)])
                fw.dma('sp', v_all[:], vg.rearrange("(t p) c -> p t c", p=128), extra_reads=[("vg", i) for i in range(NT)])
                fw.dma('sp', vm_all[:], vm.rearrange("(t p) c -> p t c", p=128), extra_reads=[("vm", i) for i in range(NT)])
                ucount = [0]

                pend = [None]

                def unit(kT_ap, qT_ap, vfn, scale, qb, col):
                    u = ucount[0]
                    ucount[0] += 1
                    ac = acc[u % 2]
                    NP = NT // 2

                    def Smm(kp):
                        pss = psS[kp % 2]
                        fw.mm([(pss[:, 0, :], kT_ap[:, (2 * kp) * 128:(2 * kp + 1) * 128], qT_ap, True, True),
                               (pss[:, 1, :], kT_ap[:, (2 * kp + 1) * 128:(2 * kp + 2) * 128], qT_ap, True, True)])
                    Smm(0)
                    if pend[0] is not None:
                        pend[0]()
                        pend[0] = None
                    for kp in range(NP):
                        if kp + 1 < NP:
                            Smm(kp + 1)
                        pt = pT[kp % 2]
                        fw.act(pt[:], psS[kp % 2][:], AF.Exp, scale=scale)
                        fw.mm([(ac[:], vfn(2 * kp), pt[:, 0, :], kp == 0, False),
                               (ac[:], vfn(2 * kp + 1), pt[:, 1, :], False, kp == NP - 1)])

                    def tail():
                        fw.copy(oTs[:], ac[:], e='dve')
                        fw.tr([(psO[:, s_, 0:65], oTs[:, s_ * 128:(s_ + 1) * 128], identf[0:65, 0:65]) for s_ in range(4)])
                        fw.recip(rcp[:], psO[:, :, 64])
                        o_ = ob[u % 2]
                        fw.tt(o_[:], psO[:, :, 0:64], bc(rcp[:, :], 2, 64), ALU.mult)
                        fw.dma('pool', (omix[qb * 512:(qb + 1) * 512, col:col + 64].rearrange("(s p) d -> p s d", p=128),
                                        ("omix", qb, col)), o_[:])
                    pend[0] = tail

                qcnt = 0
                for qb in range(NQB):
                    for j in range(3):
                        qt = qTt[qcnt % 2]
                        qcnt += 1
                        fw.dma('sp', qt[:], qTg[j, :, qb * 512:(qb + 1) * 512], extra_reads=allq)
                        for half in range(2):
                            head = j + 3 * half
                            rs = slice(half * 64, (half + 1) * 64)
                            unit(kT_all[rs, :], qt[rs, :],
                                 (lambda kt, half=half: v_all[:, kt, half * 65:(half + 1) * 65]),
                                 0.125, qb, 384 + head * 64)
                allqm = [("qTm", i) for i in range(NT)]
                for h in range(4):
                    kh = kTm_h[h % 2]
                    fw.dma('sp', kh[:], kTm[h], extra_reads=[("kTm", i) for i in range(NT)])
                    for qb in range(NQB):
                        qt = qTt[qcnt % 2]
                        qcnt += 1
                        fw.dma('sp', qt[0:96, :], qTm[h, :, qb * 512:(qb + 1) * 512], extra_reads=allqm)
                        unit(kh[:, :], qt[0:96, :], (lambda kt, h=h: vm_all[:, kt, h * 65:(h + 1) * 65]),
                             96 ** -0.5, qb, 768 + h * 64)
                if pend[0] is not None:
                    pend[0]()
                    pend[0] = None
                fw.barrier()
            if stop_after == 'B':
                break
            with ExitStack() as stp:
                wcv = sb(stp, "wcv", [128, 5, 1152], F32)
                fw.dma('sp', wcv[:], wconv_d[l].rearrange("p (j c) -> p j c", j=5))
                shf1 = [sb(stp, f"shf_{j}", [128, 1152], F32) for j in range(5)]
                shf = [shf1, shf1]
                cacc = sb(stp, "cacc", [128, 1152], F32)
                ctmp = sb(stp, "ctmp", [128, 1152], F32)
                qkv = sb(stp, "qkv", [128, 1152], F32)
                s12 = sb(stp, "s12", [128, 12], F32)
                qnb = sb(stp, "qnb", [128, 384], BF16)
                knb = sb(stp, "knb", [128, 384], BF16)
                kn32 = sb(stp, "kn32", [128, 384], F32)
                qkT = sb(stp, "qkT", [64, 12, 128], BF16)
                gcs = sb(stp, "gcs", [128, 24], F32)
                ex = sb(stp, "ex", [128, 36], F32)
                Xd = sb(stp, "Xd", [128, 12, 128], F32)
                Dm = sb(stp, "Dm", [128, 12, 128], F32)
                dec = sb(stp, "dec", [128, 12, 128], F32)
                Lt = sb(stp, "Lt", [128, 12, 128], F32)
                Pb = [sb(stp, f"Pb{i}", [128, 12, 128], F32) for i in range(2)]
                Qb = [sb(stp, f"Qb{i}", [128, 12, 128], F32) for i in range(2)]
                aqk = sb(stp, "aqk", [128, 12, 128], BF16)
                aqkT = sb(stp, "aqkT", [128, 12, 128], BF16)
                X32 = sb(stp, "X32", [128, 12, 128], F32)
                Wb = sb(stp, "Wb", [128, 12, 64], BF16)
                wT = sb(stp, "wT", [64, 12, 128], BF16)
                ktl = sb(stp, "ktl", [128, 12, 64], BF16)
                coef = sb(stp, "coef", [128, 12], F32)
                psA3 = ps(stp, "psA3", [128, 12, 128])
                psB3 = ps(stp, "psB3", [128, 12, 128])
                psT12f = ps(stp, "psT12", [128, 16, 128], BF16)
                psT12 = psT12f[:, 0:12, :]
                UTm, LTm = masks[:, 0, :], masks[:, 1, :]
                negm = masks[:, 2:4, :]
                strm = masks[:, 4:6, :]
                m0, m1s, m2s = masks[:, 6, :], masks[:, 7, :], masks[:, 8, :]
                bfn = {}
                for nm_ in ('L1Tb', 'L2Tb', 'T32b', 'T32Tb', 'Wz', 'T64b', 'T64Tb', 'Rb', 'Yb'):
                    bfn[nm_] = sb(stp, nm_, [128, 12, 128], BF16)

                def v4(t, w=128):
                    return t[:, :, :].rearrange("p (a h) d -> p a h d", a=2)

                def load_shift(i):
                    for j in range(5):
                        fw.dma('sp', shf[i % 2][j][:], dnpre[i * 128 + j:i * 128 + j + 128, :])
                load_shift(0)
                for i in range(NT):
                    sh = shf[i % 2]
                    tsl = slice(i * 128, (i + 1) * 128)
                    fw.tt(cacc[:], sh[0][:], wcv[:, 0, :], ALU.mult, e='pool')
                    for j in range(1, 5):
                        fw.tt(ctmp[:], sh[j][:], wcv[:, j, :], ALU.mult, e='pool')
                        fw.tt(cacc[:], cacc[:], ctmp[:], ALU.add)
                    if i + 1 < NT:
                        load_shift(i + 1)
                    fw.act(ctmp[:], cacc[:], AF.Exp, scale=-1.0)
                    fw.act(ctmp[:], ctmp[:], AF.Ln, bias=oneT[:], scale=1.0)
                    fw.act(ctmp[:], ctmp[:], AF.Exp, scale=-1.0)
                    fw.tt(qkv[:], cacc[:], ctmp[:], ALU.mult)
                    qk12 = qkv[:, 0:768].rearrange("p (h d) -> p h d", h=12)
                    c12 = cacc[:, 0:768].rearrange("p (h d) -> p h d", h=12)
                    fw.tt(c12, qk12, qk12, ALU.mult, e='pool')
                    fw.red(s12[:], c12)
                    fw.act(s12[:], s12[:], AF.Ln, bias=epsT[:], scale=1.0)
                    fw.act(s12[:], s12[:], AF.Exp, scale=-0.5)
                    fw.ts(s12[:, 0:6], s12[:, 0:6], 0.125, None, ALU.mult)
                    fw.tt(qnb[:, :].rearrange("p (h d) -> p h d", h=6), qk12[:, 0:6, :], bc(s12[:, 0:6], 2, 64), ALU.mult)
                    fw.tt(kn32[:, :].rearrange("p (h d) -> p h d", h=6), qk12[:, 6:12, :], bc(s12[:, 6:12], 2, 64), ALU.mult)
                    fw.copy(knb[:], kn32[:], e='pool')
                    fw.tr([(psT12[0:64, h, :], qnb[:, h * 64:(h + 1) * 64], identb[:]) for h in range(6)] +
                          [(psT12[0:64, 6 + h, :], knb[:, h * 64:(h + 1) * 64], identb[:]) for h in range(6)])
                    fw.copy(qkT[:], psT12[0:64, :, :], e='act')
                    fw.dma('pool', dQT[i].rearrange("p (h t) -> p h t", h=6), qkT[:, 0:6, :])
                    g_ = gT[:, i, :]
                    psG = psB3[:, 11, 0:24]
                    fw.mm([(psB3[:, 11, 0:6], UTm, g_[:, 0:6], True, True),
                           (psB3[:, 11, 6:12], LTm, g_[:, 6:12], True, True),
                           (psB3[:, 11, 12:24], onesf[:], g_, True, True)])
                    fw.copy(gcs[:], psG)
                    fw.act(ex[:, 0:24], gcs[:], AF.Exp)
                    fw.tt(ex[:, 24:36], gcs[:, 12:24], gcs[:, 0:12], ALU.subtract)
                    fw.act(ex[:, 24:36], ex[:, 24:36], AF.Exp)
                    fw.dma('pool', dE[tsl, :], ex[:, 0:24])
                    fw.tt(Xd[:], bc(identf[:, :], 1, 12), bc(gcs[:, 0:12], 2, 128), ALU.mult, e='pool')
                    Xf = Xd[:, :, :].rearrange("p u j -> p (u j)")
                    Af = psA3[:, :, :].rearrange("p u j -> p (u j)")
                    fw.mm([(Af[:, c * 512:(c + 1) * 512], onesf[:], Xf[:, c * 512:(c + 1) * 512], True, True) for c in range(3)])
                    fw.tt(Dm[:], bc(gcs[:, 0:12], 2, 128), psA3[:], ALU.subtract)
                    fw.tt(v4(Dm), v4(Dm), bc(negm, 2, 6), ALU.add, e='pool')
                    fw.act(dec[:], Dm[:], AF.Exp)
                    fw.mm([(psB3[:, h, :], qkT[:, 6 + h, :], qkT[:, 6 + h, :], True, True) for h in range(6)] +
                          [(psB3[:, 6 + h, :], qkT[:, h, :], qkT[:, 6 + h, :], True, True) for h in range(6)])
                    fw.tt(v4(Lt), v4(dec), bc(psB3[:, 0:6, :], 1, 2), ALU.mult)
                    fw.tt(v4(aqk), v4(dec), bc(psB3[:, 6:12, :], 1, 2), ALU.mult)
                    fw.stt(Xd[:], Lt[:], -1.0, bc(betaT[:, i, :], 2, 128), ALU.mult, ALU.mult)
                    fw.tr([(psB3[:, u, :], Xd[:, u, :], identf[:]) for u in range(12)])
                    fw.tt(Pb[0][:], Xd[:], bc(m0, 1, 12), ALU.mult, e='pool')
                    fw.tt(Qb[0][:], psB3[:], bc(m0, 1, 12), ALU.mult)
                    fw.tt(bfn['L1Tb'][:], psB3[:], bc(m1s, 1, 12), ALU.mult)
                    fw.tt(bfn['L2Tb'][:], psB3[:], bc(m2s, 1, 12), ALU.mult)
                    fw.tr([(psT12[:, u, :], aqk[:, u, :], identb[:]) for u in range(12)])
                    fw.copy(aqkT[:], psT12, e='act')
                    fw.dma('pool', dAT[i].rearrange("p (u t) -> p u t", u=12), aqkT[:])
                    fw.tt(coef[:], betaT[:, i, :], ex[:, 0:12], ALU.mult)
                    X4 = v4(bfn['Rb'])
                    v6 = qkv[:, 768:1152].rearrange("p (h d) -> p h d", h=6)
                    k6 = kn32[:, :].rearrange("p (h d) -> p h d", h=6)
                    b4 = betaT[:, i, :].rearrange("p (a h) -> p a h", a=2)
                    c4 = coef[:, :].rearrange("p (a h) -> p a h", a=2)
                    e4 = ex[:, 24:36].rearrange("p (a h) -> p a h", a=2)
                    fw.tt(X4[:, :, :, 0:64], bc(v6, 1, 2), bc(b4, 3, 64), ALU.mult)
                    fw.tt(X4[:, :, :, 64:128], bc(k6, 1, 2), bc(c4, 3, 64), ALU.mult, e='pool')
                    fw.tt(ktl[:, :, :].rearrange("p (a h) d -> p a h d", a=2), bc(k6, 1, 2), bc(e4, 3, 64), ALU.mult, e='pool')
                    fw.dma('pool', dKT[tsl, :], ktl[:, :, :].rearrange("p u d -> p (u d)"))
                    fw.tt(Lt[:], Pb[0][:], bc(identf[:, :], 1, 12), ALU.add, e='pool')
                    for k in range(4):
                        P_, Q_ = Pb[k % 2], Qb[k % 2]
                        Pn, Qn = Pb[(k + 1) % 2], Qb[(k + 1) % 2]
                        fw.mm([(psA3[:, u, :], P_[:, u, :], Q_[:, u, :], True, True) for u in range(12)])
                        fw.copy(Qn[:], psA3[:], e='act')
                        if k < 3:
                            fw.mm([(psB3[:, u, :], Q_[:, u, :], P_[:, u, :], True, True) for u in range(12)])
                            fw.copy(Pn[:], psB3[:], e='act')
                        fw.mm([(psA3[:, u, :], Qn[:, u, :], Lt[:, u, :], True, True) for u in range(12)])
                        if k < 3:
                            fw.tt(Lt[:], Lt[:], psA3[:], ALU.add)
                        else:
                            fw.tt(bfn['T32b'][:], Lt[:], psA3[:], ALU.add)
                    fw.tr([(psT12[:, u, :], bfn['T32b'][:, u, :], identb[:]) for u in range(12)])
                    fw.copy(bfn['T32Tb'][:], psT12, e='act')
                    fw.mm([(psA3[:, u, :], bfn['L1Tb'][:, u, :], bfn['T32b'][:, u, :], True, True) for u in range(12)])
                    fw.copy(bfn['Wz'][:], psA3[:], e='act')
                    fw.mm([(psB3[:, u, :], bfn['T32Tb'][:, u, :], bfn['Wz'][:, u, :], True, True) for u in range(12)])
                    fw.tt(bfn['T64b'][:], bfn['T32b'][:], psB3[:], ALU.subtract)
                    fw.tr([(psT12[:, u, :], bfn['T64b'][:, u, :], identb[:]) for u in range(12)])
                    fw.copy(bfn['T64Tb'][:], psT12, e='act')
                    fw.mm([(psA3[:, u, :], bfn['T64Tb'][:, u, :], bfn['Rb'][:, u, :], True, True) for u in range(12)])
                    fw.copy(dec[:], psA3[:], e='act')
                    fw.copy(bfn['Yb'][:], dec[:], e='act')
                    fw.mm([(psB3[:, u, :], bfn['L2Tb'][:, u, :], bfn['Yb'][:, u, :], True, True) for u in range(12)])
                    fw.copy(bfn['Wz'][:], psB3[:], e='act')
                    fw.mm([(psA3[:, u, :], bfn['T64Tb'][:, u, :], bfn['Wz'][:, u, :], True, True) for u in range(12)])
                    fw.tt(X32[:], dec[:], psA3[:], ALU.subtract)
                    fw.dma('pool', dU[tsl, :].rearrange("p (u d) -> p u d", u=12), X32[:, :, 0:64])
                    fw.copy(Wb[:], X32[:, :, 64:128], e='act')
                    fw.tr([(psT12[0:64, u, :], Wb[:, u, :], identb[:]) for u in range(12)])
                    fw.copy(wT[:], psT12[0:64, :, :], e='act')
                    fw.dma('pool', dWT[i].rearrange("p (u t) -> p u t", u=12), wT[:])
                fw.barrier()

            with ExitStack() as stp:
                S32 = [sb(stp, f"S32_{d}", [64, 6, 64], F32) for d in range(2)]
                Sb = [sb(stp, f"Sb_{d}", [64, 6, 64], BF16) for d in range(2)]
                for d in range(2):
                    fw.memset(S32[d][:], 0.0)
                    fw.memset(Sb[d][:], 0.0)
                Ud = [[sb(stp, f"Ud{d}{b}", [128, 6, 64], F32) for b in range(2)] for d in range(2)]
                WTd = [[sb(stp, f"WTd{d}{b}", [64, 6, 128], BF16) for b in range(2)] for d in range(2)]
                ATd = [[sb(stp, f"ATd{d}{b}", [128, 6, 128], BF16) for b in range(2)] for d in range(2)]
                KTd = [[sb(stp, f"KTd{d}{b}", [128, 6, 64], BF16) for b in range(2)] for d in range(2)]
                QTd = [[sb(stp, f"QTd{d}{b}", [64, 6, 128], BF16) for b in range(2)] for d in range(2)]
                Ed = [[sb(stp, f"Ed{d}{b}", [128, 24], F32) for b in range(2)] for d in range(2)]
                vnew = [sb(stp, f"vnew{d}", [128, 6, 64], BF16) for d in range(2)]
                ot = [sb(stp, f"ot{d}", [128, 6, 64], F32) for d in range(2)]
                od = [sb(stp, f"od{d}", [128, 6, 64], F32) for d in range(2)]
                stmp = [sb(stp, f"stmp{d}", [64, 6, 64], F32) for d in range(2)]
                psV = [ps(stp, f"psV{d}", [128, 8, 64])[:, 0:6, :] for d in range(2)]
                psO1 = [ps(stp, f"psO1{d}", [128, 8, 64])[:, 0:6, :] for d in range(2)]
                psO2 = [ps(stp, f"psO2{d}", [128, 8, 64])[:, 0:6, :] for d in range(2)]
                psS = [ps(stp, f"psS{d}", [64, 8, 64])[:, 0:6, :] for d in range(2)]

                def load_scan(s_):
                    for d in range(2):
                        n = s_ if d == 0 else NT - 1 - s_
                        b = s_ % 2
                        rows = slice(n * 128, (n + 1) * 128)
                        fw.dma('sp', Ud[d][b][:], dU[rows, d * 384:(d + 1) * 384].rearrange("p (h v) -> p h v", h=6))
                        fw.dma('sp', WTd[d][b][:], dWT[n][:, d * 768:(d + 1) * 768].rearrange("p (h t) -> p h t", h=6))
                        fw.dma('sp', ATd[d][b][:], dAT[n][:, d * 768:(d + 1) * 768].rearrange("p (h t) -> p h t", h=6))
                        fw.dma('sp', KTd[d][b][:], dKT[rows, d * 384:(d + 1) * 384].rearrange("p (h v) -> p h v", h=6))
                        fw.dma('sp', QTd[d][b][:], dQT[n].rearrange("p (h t) -> p h t", h=6))
                        fw.dma('sp', Ed[d][b][:], dE[rows, :])
                load_scan(0)
                for s_ in range(NT):
                    if s_ + 1 < NT:
                        load_scan(s_ + 1)
                    b = s_ % 2
                    ns = [s_, NT - 1 - s_]
                    for d in range(2):
                        fw.tt(stmp[d][:], S32[d][:], bc(Ed[d][b][0:64, 12 + d * 6:18 + d * 6], 2, 64), ALU.mult)
                    for d in range(2):
                        fw.mm([(psV[d][:, h, :], WTd[d][b][:, h, :], Sb[d][:, h, :], True, True) for h in range(6)] +
                              [(psO1[d][:, h, :], QTd[d][b][:, h, :], Sb[d][:, h, :], True, True) for h in range(6)])
                    for d in range(2):
                        fw.tt(vnew[d][:], Ud[d][b][:], psV[d][:], ALU.subtract)
                    for d in range(2):
                        fw.mm([(psS[d][:, h, :], KTd[d][b][:, h, :], vnew[d][:, h, :], True, True) for h in range(6)] +
                              [(psO2[d][:, h, :], ATd[d][b][:, h, :], vnew[d][:, h, :], True, True) for h in range(6)])
                    for d in range(2):
                        fw.tt(S32[d][:], stmp[d][:], psS[d][:], ALU.add)
                        fw.copy(Sb[d][:], S32[d][:], e='act')
                    for d in range(2):
                        fw.tt(ot[d][:], psO1[d][:], bc(Ed[d][b][:, d * 6:d * 6 + 6], 2, 64), ALU.mult)
                        fw.tt(od[d][:], ot[d][:], psO2[d][:], ALU.add)
                        fw.dma('pool', dO[d, ns[d] * 128:(ns[d] + 1) * 128, :], od[d][:, :, :].rearrange("p h v -> p (h v)"))
                fw.barrier()

            with ExitStack() as stp:
                gdn = sb(stp, "gdn", [128, 64], F32)
                fw.dma('sp', gdn[:], dng_d[l])
                o0 = [sb(stp, f"o0_{b}", [128, 384], F32) for b in range(2)]
                o1 = [sb(stp, f"o1_{b}", [128, 384], F32) for b in range(2)]
                zt_ = [sb(stp, f"zt_{b}", [128, 384], F32) for b in range(2)]
                osum = sb(stp, "osum", [128, 384], F32)
                otmp = sb(stp, "otmp", [128, 384], F32)
                ze = sb(stp, "ze", [128, 384], F32)
                s6c = sb(stp, "s6c", [128, 6], F32)
                oab = sb(stp, "oab", [128, 384], BF16)

                def load_c3(i):
                    fw.dma('sp', o0[i % 2][:], dO[0, i * 128:(i + 1) * 128, :])
                    fw.dma('sp', o1[i % 2][:], dO[1, i * 128:(i + 1) * 128, :])
                    fw.dma('sp', zt_[i % 2][:], dnz[i * 128:(i + 1) * 128, :])
                load_c3(0)
                for i in range(NT):
                    if i + 1 < NT:
                        load_c3(i + 1)
                    b = i % 2
                    fw.tt(osum[:], o0[b][:], o1[b][:], ALU.add)
                    o6 = osum[:, :].rearrange("p (h d) -> p h d", h=6)
                    t6 = otmp[:, :].rearrange("p (h d) -> p h d", h=6)
                    fw.tt(t6, o6, o6, ALU.mult, e='pool')
                    fw.red(s6c[:], t6)
                    fw.act(s6c[:], s6c[:], AF.Ln, bias=epsT[:], scale=1.0 / 64)
                    fw.act(s6c[:], s6c[:], AF.Exp, scale=-0.5)
                    fw.tt(t6, o6, bc(s6c[:, :], 2, 64), ALU.mult)
                    fw.tt(t6, t6, bc(gdn[:, :], 1, 6), ALU.mult, e='pool')
                    fw.act(ze[:], zt_[b][:], AF.Exp, scale=-1.0)
                    fw.ts(ze[:], ze[:], 1.0, None, ALU.add, e='pool')
                    fw.recip(ze[:], ze[:])
                    fw.tt(ze[:], ze[:], zt_[b][:], ALU.mult)
                    fw.tt(oab[:], otmp[:], ze[:], ALU.mult)
                    fw.dma('pool', omix[i * 128:(i + 1) * 128, 0:384], oab[:])
                fw.barrier()
            if stop_after == 'C':
                break

            with ExitStack() as stp:
                wo = sb(stp, "wo", [128, 8, D], BF16)
                wos = [sb(stp, f"wos{i}", [128, D], F32) for i in range(2)]
                for k in range(8):
                    fw.dma('sp', wos[k % 2][:], w_out[l][k * 128:(k + 1) * 128, :])
                    fw.copy(wo[:, k, :], wos[k % 2][:], e=('pool' if k % 2 else 'dve'))
                wr = sb(stp, "wr", [128, 8, 36], F32)
                br = sb(stp, "br", [128, 36], F32)
                fw.dma('sp', wr[:], w_r[l].rearrange("(k p) n -> p k n", p=128))
                fw.dma('sp', br[:], b_r[l])
                om = [sb(stp, f"om{i}", [128, D], BF16) for i in range(2)]
                xd = [sb(stp, f"xd{i}", [128, D], F32) for i in range(2)]
                oT = sb(stp, "oT", [128, 8, 128], BF16)
                x1 = sb(stp, "x1", [128, D], F32)
                dtmp = sb(stp, "dtmp", [128, D], F32)
                h2 = sb(stp, "h2", [128, D], F32)
                h2T32 = sb(stp, "h2T32", [128, 8, 128], F32)
                h2Tb = sb(stp, "h2Tb", [128, 8, 128], BF16)
                ssd = sb(stp, "ssd", [128, 1], F32)
                lg = sb(stp, "lg", [128, 36], F32)
                r1 = [sb(stp, f"r1_{i}", [128, 1], F32) for i in range(8)]
                ohg = sb(stp, "ohg", [128, 4], F32)
                eg4 = sb(stp, "eg4", [128, 4], F32)
                t32 = sb(stp, "t32", [128, 32], F32)
                esel = sb(stp, "esel", [128, 8], F32)
                es2 = sb(stp, "es2", [128, 8], F32)
                mk1 = sb(stp, "mk1", [128, 8], F32)
                mk2 = sb(stp, "mk2", [128, 8], F32)
                ge = sb(stp, "ge", [128, 8], F32)
                Gt = sb(stp, "Gt", [128, 32], F32)
                psTb2 = ps(stp, "psTb2", [128, 8, 128], BF16)
                psY2 = ps(stp, "psY2", [128, D])
                psW2 = ps(stp, "psW2", [128, 8, 128])
                psR = ps(stp, "psR", [128, 512])[:, 0:36]

                def load_d(i):
                    fw.dma('sp', om[i % 2][:], omix[i * 128:(i + 1) * 128, :])
                    fw.dma('sp', xd[i % 2][:], x_src[i * 128:(i + 1) * 128, :])
                load_d(0)
                for i in range(NT):
                    if i + 1 < NT:
                        load_d(i + 1)
                    tsl = slice(i * 128, (i + 1) * 128)
                    o_, x_ = om[i % 2], xd[i % 2]
                    fw.tr([(psTb2[:, k, :], o_[:, k * 128:(k + 1) * 128], identb[:]) for k in range(8)])
                    fw.copy(oT[:], psTb2[:], e='act')
                    fw.mm([(psY2[:, 0:512], oT[:, k, :], wo[:, k, 0:512], k == 0, k == 7) for k in range(8)] +
                          [(psY2[:, 512:1024], oT[:, k, :], wo[:, k, 512:1024], k == 0, k == 7) for k in range(8)])
                    fw.tt(dtmp[:], psY2[:], gt1, ALU.mult)
                    fw.tt(x1[:], dtmp[:], x_[:], ALU.add, e='pool')
                    if not os.environ.get('SKIP_XS0'):
                        fw.dma('pool', xs[0][tsl, :], x1[:])
                    fw.act(dtmp[:], x1[:], AF.Square, accum_out=ssd[:])
                    fw.act(ssd[:], ssd[:], AF.Ln, bias=epsT[:], scale=1.0 / D)
                    fw.act(ssd[:], ssd[:], AF.Exp, scale=-0.5)
                    fw.stt(h2[:], x1[:], ssd[:, 0:1], A2[:], ALU.mult, ALU.mult)
                    fw.tt(h2[:], h2[:], sh2, ALU.add, e='pool')
                    fw.tr([(psW2[:, k, :], h2[:, k * 128:(k + 1) * 128], identf[:]) for k in range(8)])
                    fw.copy(h2T32[:], psW2[:])
                    fw.copy(h2Tb[:], psW2[:], e='act')
                    if not os.environ.get('SKIP_H2TD'):
                        fw.dma('pool', h2Td[:, :, tsl], h2Tb[:])
                    if os.environ.get("SKIP_ROUTER"):
                        continue
                    fw.mmk(psR[:], [(h2T32[:, k, :], wr[:, k, :]) for k in range(8)])
                    fw.tt(lg[:], psR[:], br[:], ALU.add)
                    gm, ngm, sume, gtp, m1, m2, dd, w1_ = r1
                    fw.red(gm[:], lg[:, 0:4], op=ALU.max)
                    fw.ts(ohg[:], lg[:, 0:4], gm[:, 0:1], None, ALU.is_equal)
                    fw.ts(ngm[:], gm[:], -1.0, None, ALU.mult)
                    fw.act(eg4[:], lg[:, 0:4], AF.Exp, bias=ngm[:], scale=1.0, accum_out=sume[:])
                    fw.recip(gtp[:], sume[:])
                    fw.tt(t32[:, :].rearrange("p (g e) -> p g e", g=4), lg[:, 4:36].rearrange("p (g e) -> p g e", g=4),
                          bc(ohg[:, :], 2, 8), ALU.mult)
                    fw.red(esel[:], t32[:, :].rearrange("p (g e) -> p e g", g=4))
                    fw.red(m1[:], esel[:], op=ALU.max)
                    fw.ts(mk1[:], esel[:], m1[:, 0:1], None, ALU.is_equal)
                    fw.stt(es2[:], mk1[:], -1e30, esel[:], ALU.mult, ALU.add)
                    fw.red(m2[:], es2[:], op=ALU.max)
                    fw.ts(mk2[:], es2[:], m2[:, 0:1], None, ALU.is_equal)
                    fw.tt(dd[:], m2[:], m1[:], ALU.subtract)
                    fw.act(dd[:], dd[:], AF.Exp)
                    fw.ts(w1_[:], dd[:], 1.0, None, ALU.add)
                    fw.recip(w1_[:], w1_[:])
                    fw.tt(dd[:], dd[:], w1_[:], ALU.mult)
                    fw.tt(w1_[:], w1_[:], gtp[:], ALU.mult)
                    fw.tt(dd[:], dd[:], gtp[:], ALU.mult)
                    fw.ts(ge[:], mk1[:], w1_[:, 0:1], None, ALU.mult)
                    fw.stt(ge[:], mk2[:], dd[:, 0:1], ge[:], ALU.mult, ALU.add)
                    fw.tt(Gt[:, :].rearrange("p (g e) -> p g e", g=4), bc(ohg[:, :], 2, 8), bc(ge[:, :], 1, 4), ALU.mult)
                    fw.dma('pool', Gd[tsl, :], Gt[:])
                fw.barrier()

            if stop_after == 'D':
                break
            SBK = min(S, 2048)
            TPB = SBK // 128
            NB = SBK // 512
            with ExitStack() as stp:
                h2Ts = sb(stp, "h2Ts", [128, 8, SBK], BF16)
                yacc = sb(stp, "yacc", [128, TPB, D], F32)
                Gs = sb(stp, "Gs", [128, TPB, 32], F32)
                w1b = [sb(stp, f"w1b_{i}", [128, 8, 256], BF16) for i in range(2)]
                w3b = [sb(stp, f"w3b_{i}", [128, 8, 256], BF16) for i in range(2)]
                w2b = [sb(stp, f"w2b_{i}", [128, 2, D], BF16) for i in range(2)]
                st1 = sb(stp, "st1", [128, 8, 256], F32)
                st3 = sb(stp, "st3", [128, 8, 256], F32)
                st2 = sb(stp, "st2", [128, 2, D], F32)
                e1_ = [sb(stp, f"e1_{i}", [128, 512], F32) for i in range(2)]
                p_ = [sb(stp, f"p_{i}", [128, 512], F32) for i in range(2)]
                hidT = [sb(stp, f"hidT{i}", [128, 2, 512], BF16) for i in range(2)]
                xe1 = st1[:, 0:4, :].rearrange("p k n -> p (k n)")
                xo1 = st3[:, 0:4, :].rearrange("p k n -> p (k n)")
                psH1 = [ps(stp, f"psH1{i}", [128, 512]) for i in range(2)]
                psH3 = [ps(stp, f"psH3{i}", [128, 512]) for i in range(2)]
                psY = [ps(stp, f"psY{i}", [128, D]) for i in range(2)]
                bcount = 0
                ycount = [0]
                pendE = [None]
                for sbk in range(S // SBK):
                    t0 = sbk * SBK
                    fw.dma('sp', h2Ts[:], h2Td[:, :, t0:t0 + SBK])
                    fw.dma('sp', Gs[:], Gd[t0:t0 + SBK, :].rearrange("(t p) e -> p t e", p=128))
                    for e_ in range(32):
                        wa1, wa3, wb2 = w1b[e_ % 2], w3b[e_ % 2], w2b[e_ % 2]
                        fw.dma('sp', st1[:], moe_w1[l, e_].rearrange("(k p) n -> p k n", p=128))
                        fw.dma('sp', st3[:], moe_w3[l, e_].rearrange("(k p) n -> p k n", p=128))
                        fw.dma('sp', st2[:], moe_w2[l, e_].rearrange("(k p) n -> p k n", p=128))
                        fw.copy(wa1[:], st1[:], e='pool')
                        fw.copy(wa3[:], st3[:], e='pool')
                        fw.copy(wb2[:], st2[:], e='pool')
                        for b in range(NB):
                            hT_ = hidT[bcount % 2]
                            bcount += 1
                            tk = slice(b * 512, (b + 1) * 512)
                            for c in range(2):
                                fs = slice(c * 128, (c + 1) * 128)
                                fw.mm([(psH1[c][:], wa1[:, k, fs], h2Ts[:, k, tk], k == 0, k == 7) for k in range(8)] +
                                      [(psH3[c][:], wa3[:, k, fs], h2Ts[:, k, tk], k == 0, k == 7) for k in range(8)])
                                fw.act(e1_[c][:], psH1[c][:], AF.Exp, scale=-1.0)
                                fw.act(e1_[c][:], e1_[c][:], AF.Ln, bias=oneT[:], scale=1.0)
                                fw.act(e1_[c][:], e1_[c][:], AF.Exp, scale=-1.0)
                                fw.tt(p_[c][:], psH1[c][:], e1_[c][:], ALU.mult)
                                fw.tt(hT_[:, c, :], p_[c][:], psH3[c][:], ALU.mult)
                                if c == 0 and pendE[0] is not None:
                                    pendE[0]()
                                    pendE[0] = None

                            def mk(bb=b, hh=hT_, ee=e_, wb=wb2):
                                def f():
                                    for t4 in range(4):
                                        t = bb * 4 + t4
                                        py = psY[ycount[0] % 2]
                                        ycount[0] += 1
                                        ts4 = slice(t4 * 128, (t4 + 1) * 128)
                                        fw.mm([(py[:, 0:512], hh[:, j, ts4], wb[:, j, 0:512], j == 0, j == 1) for j in range(2)] +
                                              [(py[:, 512:1024], hh[:, j, ts4], wb[:, j, 512:1024], j == 0, j == 1) for j in range(2)])
                                        if ee == 0:
                                            fw.ts(yacc[:, t, :], py[:], Gs[:, t, ee:ee + 1], None, ALU.mult)
                                        else:
                                            fw.stt(yacc[:, t, :], py[:], Gs[:, t, ee:ee + 1], yacc[:, t, :], ALU.mult, ALU.add)
                                return f
                            pendE[0] = mk()
                    if pendE[0] is not None:
                        pendE[0]()
                        pendE[0] = None
                    for t in range(TPB):
                        rows = slice(t0 + t * 128, t0 + (t + 1) * 128)
                        fw.dma('sp', xe1, xs[0][rows, :])
                        fw.tt(xo1, yacc[:, t, :], gt2, ALU.mult, e='pool')
                        fw.tt(xo1, xo1, xe1, ALU.add)
                        fw.dma('pool', x_dst[rows, :], xo1)
                fw.barrier()

        if dbg and os.environ.get("DBG_DN"):
            with ExitStack() as stp:
                t_f = sb(stp, "dbgdn_f", [128, D], F32)
                fw.memset(t_f[:], 0.0)
                for i in range(NT):
                    rows = slice(i * 128, (i + 1) * 128)
                    if os.environ.get("DBG_DN") == "U":
                        fw.dma('sp', t_f[:, 0:768], dU[rows, :])
                    else:
                        fw.dma('sp', t_f[:, 0:384], dO[0, rows, :])
                        fw.dma('sp', t_f[:, 384:768], dO[1, rows, :])
                    fw.dma('sp', t_f[:, 768:792], dE[rows, :])
                    fw.dma('sp', t_f[:, 800:812], gT[:, i, :])
                    fw.dma('sp', t_f[:, 812:824], betaT[:, i, :])
                    fw.dma('sp', dbg_out[rows, :], t_f[:])
                fw.barrier()
        elif dbg and stop_after in ('B', 'C'):
            with ExitStack() as stp:
                t_b = sb(stp, "dbg_b", [128, D], BF16)
                t_f = sb(stp, "dbg_f", [128, D], F32)
                for i in range(NT):
                    fw.dma('sp', t_b[:], omix[i * 128:(i + 1) * 128, :],
                           extra_reads=[("omix", qb, col) for qb in range(NQB) for col in range(384, 1024, 64)])
                    fw.copy(t_f[:], t_b[:])
                    fw.dma('sp', dbg_out[i * 128:(i + 1) * 128, :], t_f[:])
                fw.barrier()
        fw.barrier()
        print("instructions emitted:", fw.ninst)
    return nc


def rope_tables(S, rot_dim, grid_w=64):
    t = np.arange(S)
    row = (t // grid_w).astype(np.float32)
    col = (t % grid_w).astype(np.float32)
    n_freq = rot_dim // 4
    inv = (10000.0 ** (-np.arange(n_freq, dtype=np.float32) / n_freq)).astype(np.float32)
    ang = np.concatenate([row[:, None] * inv, col[:, None] * inv], axis=-1).astype(np.float32)
    return np.cos(ang).astype(np.float32), np.sin(ang).astype(np.float32)


def rep(v, n=128):
    v = np.asarray(v, np.float32)
    return np.ascontiguousarray(np.broadcast_to(v[..., None, :], v.shape[:-1] + (n, v.shape[-1])))


def prep_inputs(inp, S, depth):
    NT = S // 128
    f = lambda a: np.ascontiguousarray(np.asarray(a, np.float32))
    perm = np.arange(INC)
    base = 1560
    order = [0, 3, 1, 4, 2, 5]
    perm[base:base + 384] = np.concatenate([base + h * 64 + np.arange(64) for h in order])
    com = {}
    com["ada_w"] = f(inp["ada_w"][:depth])
    com["ada_b"] = rep(inp["ada_b"][:depth])
    com["g1"] = rep(inp["norm1_g"][:depth])
    com["g2"] = rep(inp["norm2_g"][:depth])
    com["w_in"] = f(np.asarray(inp["w_in"])[:depth][:, :, perm])
    com["w_out"] = f(inp["w_out"][:depth])
    com["gq_g"] = rep(inp["gqa_q_g"][:depth])
    com["gk_g"] = rep(inp["gqa_k_g"][:depth])
    com["mql_g"] = f(np.asarray(inp["mla_q_lat_g"])[:depth, :, None])
    com["mkvl_g"] = f(np.asarray(inp["mla_kv_lat_g"])[:depth, :, None])
    com["w_uq"] = f(inp["mla_w_uq"][:depth])
    com["w_ukv"] = f(inp["mla_w_ukv"][:depth])
    com["mqn_g"] = rep(inp["mla_qn_g"][:depth])
    com["mqr_g"] = rep(inp["mla_qr_g"][:depth])
    com["mkn_g"] = rep(inp["mla_kn_g"][:depth])
    com["mkr_g"] = rep(inp["mla_kr_g"][:depth])
    cg, sg = rope_tables(S, 64)
    cm, sm = rope_tables(S, 32)
    tm = lambda a: np.ascontiguousarray(a.reshape(NT, 128, -1).transpose(1, 0, 2))
    com["cosg"], com["sing"], com["cosm"], com["sinm"] = tm(cg), tm(sg), tm(cm), tm(sm)
    com["identf"] = np.eye(128, dtype=np.float32)
    com["w_r"] = f(np.concatenate([np.asarray(inp["moe_w_group"])[:depth], np.asarray(inp["moe_w_router"])[:depth]], axis=-1))
    com["b_r"] = rep(np.concatenate([np.asarray(inp["moe_b_group"])[:depth], np.asarray(inp["moe_b_router"])[:depth]], axis=-1))
    com["moe_w1"] = f(inp["moe_w1"][:depth])
    com["moe_w3"] = f(inp["moe_w3"][:depth])
    com["moe_w2"] = f(inp["moe_w2"][:depth])
    com["wconv"] = rep(np.asarray(inp["dn_conv"])[:depth].reshape(depth, 5 * 1152))
    com["alog"] = rep(np.asarray(inp["dn_a_log"])[:depth].reshape(depth, 12))
    com["dtb"] = rep(np.asarray(inp["dn_dt_bias"])[:depth].reshape(depth, 12))
    com["dng"] = rep(inp["dn_out_g"][:depth])
    ii = np.arange(128)[:, None]
    jj = np.arange(128)[None, :]
    NEG = -30000.0
    mk = np.stack([(ii <= jj), (ii >= jj),
                   np.where(jj <= ii, 0.0, NEG), np.where(jj >= ii, 0.0, NEG),
                   (jj < ii), (jj > ii),
                   (ii // 32 == jj // 32) & (ii != jj), (ii // 64 == jj // 64) & (ii // 32 != jj // 32), (ii // 64 != jj // 64)],
                  axis=1).astype(np.float32)
    mk[:, 7:9, :] *= -1.0
    com["masks"] = np.ascontiguousarray(mk)
    return com


def kernel(**inp):
    S, depth = 4096, 4
    com = prep_inputs(inp, S, depth)
    x = np.asarray(inp["x"], np.float32)
    c = np.asarray(inp["c"], np.float32)
    nc = build(S, depth)
    maps = []
    for core in range(8):
        b = core % 4
        m = dict(com)
        m["x"] = np.ascontiguousarray(x[b])
        m["c_pk"] = np.ascontiguousarray(c[b].reshape(8, 128).T)
        maps.append(m)
    res = run_bass_kernel_spmd(nc, maps, core_ids=list(range(8)))
    return np.stack([res.results[b]["y"] for b in range(4)], axis=0).astype(np.float32)
```

```python
import math
import os
import numpy as np
from contextlib import ExitStack
import concourse.bass as bass
import concourse.mybir as mybir
from concourse.bass_utils import run_bass_kernel_spmd

F32 = mybir.dt.float32
BF16 = mybir.dt.bfloat16
I32 = mybir.dt.int32
AF = mybir.ActivationFunctionType
ALU = mybir.AluOpType
AX = mybir.AxisListType
NDS = 20
import re
PSUM_RE = re.compile(r'p\d+_')

D = 1024
INC = 2552
EPS = 1e-6


class FW:
    def __init__(self, nc, stack):
        self.nc = nc
        self.stack = stack
        self.eng = {'pe': nc.tensor, 'act': nc.scalar, 'dve': nc.vector, 'pool': nc.gpsimd, 'sp': nc.sync}
        self.sem = {}
        self.cnt = {}
        for e in self.eng:
            self.sem[('c', e)] = stack.enter_context(nc.semaphore('s_' + e))
            self.cnt[('c', e)] = 0
        self.dring = {}
        self.dnext = {}
        for q in ('sp', 'act', 'pool'):
            self.dring[q] = []
            for i in range(NDS):
                k = ('d', q, i)
                self.sem[k] = stack.enter_context(nc.semaphore(f'd_{q}_{i}'))
                self.cnt[k] = 0
                self.dring[q].append(k)
            self.dnext[q] = 0
        self.waited = {e: {} for e in self.eng}
        self.W = {}
        self.R = {}
        self.ninst = 0

    @staticmethod
    def _res(x):
        if isinstance(x, tuple):
            if len(x) == 2 and not isinstance(x[0], (str, int)):
                return x[1]
            return x
        if isinstance(x, str):
            return x
        return x.name

    @staticmethod
    def ap(x):
        if isinstance(x, tuple):
            return x[0]
        return x

    def _wait(self, e, toks):
        eng = self.eng[e]
        for k, v in toks.items():
            if self.waited[e].get(k, 0) < v:
                eng.wait_ge(self.sem[k], v)
                self.waited[e][k] = v
                self.ninst += 1

    def _deps(self, reads, writes):
        toks = {}

        def add(d):
            for k, v in d.items():
                if toks.get(k, 0) < v:
                    toks[k] = v
        for r in reads:
            add(self.W.get(r, {}))
        for w in writes:
            add(self.W.get(w, {}))
            add(self.R.get(w, {}))
        return toks

    def _commit(self, tok, reads, writes):
        k, v = tok
        for r in reads:
            d = self.R.setdefault(r, {})
            d[k] = max(d.get(k, 0), v)
        for w in writes:
            self.W[w] = {k: v}
            self.R[w] = {}

    def op(self, e, fn, reads, writes):
        reads = [self._res(r) for r in reads if r is not None and not isinstance(r, (int, float))]
        writes = [self._res(w) for w in writes]
        writes = writes + [r for r in reads if isinstance(r, str) and PSUM_RE.match(r)]
        toks = self._deps(reads, writes)
        if e == 'pe':
            toks.pop(('c', 'pe'), None)
        self._wait(e, toks)
        ins = fn()
        k = ('c', e)
        self.cnt[k] += 1
        ins.then_inc(self.sem[k], 1)
        self.ninst += 1
        self._commit((k, self.cnt[k]), reads, writes)

    def dma(self, q, out, in_, extra_reads=(), extra_writes=()):
        reads = [self._res(in_)] + [self._res(r) for r in extra_reads]
        writes = [self._res(out)] + [self._res(w) for w in extra_writes]
        toks = self._deps(reads, writes)
        k = self.dring[q][self.dnext[q]]
        self.dnext[q] = (self.dnext[q] + 1) % NDS
        if self.cnt[k] > 0:
            toks[k] = max(toks.get(k, 0), self.cnt[k])
        self._wait(q, toks)
        ins = self.eng[q].dma_start(out=self.ap(out), in_=self.ap(in_))
        self.cnt[k] += 16
        ins.then_inc(self.sem[k], 16)
        self.ninst += 1
        self._commit((k, self.cnt[k]), reads, writes)

    def barrier(self, engines=('pe', 'act', 'dve', 'pool', 'sp')):
        toks = {k: v for k, v in self.cnt.items() if v > 0}
        for e in engines:
            self._wait(e, dict(toks))

    def act(self, out, in_, func, bias=None, scale=1.0, accum_out=None, e='act'):
        kw = {}
        if bias is not None:
            kw['bias'] = self.ap(bias)
        if accum_out is not None:
            kw['accum_out'] = self.ap(accum_out)
        sc = self.ap(scale) if not isinstance(scale, (int, float)) else scale
        wr = [out] + ([accum_out] if accum_out is not None else [])
        self.op(e, lambda: self.eng[e].activation(out=self.ap(out), in_=self.ap(in_), func=func, scale=sc, **kw),
                [in_, bias, scale], wr)

    def tt(self, out, in0, in1, op, e='dve'):
        self.op(e, lambda: self.eng[e].tensor_tensor(out=self.ap(out), in0=self.ap(in0), in1=self.ap(in1), op=op),
                [in0, in1], [out])

    def ts(self, out, in0, s1, s2, op0, op1=None, e='dve'):
        a1 = self.ap(s1) if not isinstance(s1, (int, float)) else s1
        a2 = (self.ap(s2) if not isinstance(s2, (int, float)) else s2) if s2 is not None else None
        kw = {}
        if op1 is not None:
            kw['op1'] = op1
        self.op(e, lambda: self.eng[e].tensor_scalar(out=self.ap(out), in0=self.ap(in0), scalar1=a1, scalar2=a2,
                                                     op0=op0, **kw), [in0, s1, s2], [out])

    def stt(self, out, in0, scalar, in1, op0, op1, e='dve'):
        a = self.ap(scalar) if not isinstance(scalar, (int, float)) else scalar
        self.op(e, lambda: self.eng[e].scalar_tensor_tensor(out=self.ap(out), in0=self.ap(in0), scalar=a,
                                                            in1=self.ap(in1), op0=op0, op1=op1),
                [in0, scalar, in1], [out])

    def red(self, out, in_, op=ALU.add, e='dve'):
        self.op(e, lambda: self.eng[e].tensor_reduce(out=self.ap(out), in_=self.ap(in_), axis=AX.X, op=op),
                [in_], [out])

    def recip(self, out, in_):
        self.op('dve', lambda: self.nc.vector.reciprocal(out=self.ap(out), in_=self.ap(in_)), [in_], [out])

    def copy(self, out, in_, e='dve'):
        if e == 'act':
            self.op(e, lambda: self.eng[e].copy(out=self.ap(out), in_=self.ap(in_)), [in_], [out])
        else:
            self.op(e, lambda: self.eng[e].tensor_copy(out=self.ap(out), in_=self.ap(in_)), [in_], [out])

    def memset(self, out, val, e='pool'):
        self.op(e, lambda: self.eng[e].memset(self.ap(out), val), [], [out])

    def mm(self, items, extra_reads=()):
        rd = list(extra_reads)
        wr = []
        for o, l, r, _, _ in items:
            rd += [l, r]
            wr.append(o)

        def fn():
            ins = None
            for o, l, r, st, sp in items:
                ins = self.nc.tensor.matmul(self.ap(o), self.ap(l), self.ap(r), start=st, stop=sp)
            return ins
        self.op('pe', fn, rd, wr)

    def mmk(self, out, pairs):
        n = len(pairs)
        self.mm([(out, l, r, i == 0, i == n - 1) for i, (l, r) in enumerate(pairs)])

    def tr(self, items):
        rd = []
        wr = []
        for o, i, idt in items:
            rd += [i, idt]
            wr.append(o)

        def fn():
            ins = None
            for o, i, idt in items:
                ins = self.nc.tensor.transpose(self.ap(o), self.ap(i), self.ap(idt))
            return ins
        self.op('pe', fn, rd, wr)


def bc(ap, axis, n):
    a = ap.unsqueeze(axis)
    shp = list(a.shape)
    shp[axis] = n
    return a.to_broadcast(shp)


def build(S=4096, depth=4, stop_after=None, dbg=False):
    NT = S // 128
    NQB = S // 512
    nc = bass.Bass("TRN2", target_bir_lowering=False)

    def din(name, shape, dt=F32):
        return nc.dram_tensor(name, list(shape), dt, kind="ExternalInput").ap()

    def scr(name, shape, dt):
        return nc.dram_tensor(name, list(shape), dt, kind="Internal").ap()

    x_in = din("x", [S, D])
    c_pk = din("c_pk", [128, 8])
    ada_w = din("ada_w", [depth, D, 6 * D])
    ada_b = din("ada_b", [depth, 128, 6 * D])
    g1_d = din("g1", [depth, 128, D])
    g2_d = din("g2", [depth, 128, D])
    w_in = din("w_in", [depth, D, INC])
    w_out = din("w_out", [depth, D, D])
    gq_g = din("gq_g", [depth, 128, 64])
    gk_g = din("gk_g", [depth, 128, 64])
    mql_g = din("mql_g", [depth, 192, 1])
    mkvl_g = din("mkvl_g", [depth, 128, 1])
    w_uq = din("w_uq", [depth, 192, 384])
    w_ukv = din("w_ukv", [depth, 128, 512])
    mqn_g = din("mqn_g", [depth, 128, 64])
    mqr_g = din("mqr_g", [depth, 128, 32])
    mkn_g = din("mkn_g", [depth, 128, 64])
    mkr_g = din("mkr_g", [depth, 128, 32])
    cosg_d = din("cosg", [128, NT, 32])
    sing_d = din("sing", [128, NT, 32])
    cosm_d = din("cosm", [128, NT, 16])
    sinm_d = din("sinm", [128, NT, 16])
    identf_d = din("identf", [128, 128])
    w_r = din("w_r", [depth, D, 36])
    b_r = din("b_r", [depth, 128, 36])
    moe_w1 = din("moe_w1", [depth, 32, D, 256])
    moe_w3 = din("moe_w3", [depth, 32, D, 256])
    moe_w2 = din("moe_w2", [depth, 32, 256, D])
    wconv_d = din("wconv", [depth, 128, 5 * 1152])
    alog_d = din("alog", [depth, 128, 12])
    dtb_d = din("dtb", [depth, 128, 12])
    dng_d = din("dng", [depth, 128, 64])
    masks_d = din("masks", [128, 9, 128])
    y_out = nc.dram_tensor("y", [S, D], F32, kind="ExternalOutput").ap()

    qTg = scr("qTg", [3, 128, S], BF16)
    kTg = scr("kTg", [128, S], BF16)
    vg = scr("vg", [S, 130], BF16)
    qTm = scr("qTm", [4, 96, S], BF16)
    kTm = scr("kTm", [4, 96, S], BF16)
    vm = scr("vm", [S, 260], BF16)
    omix = scr("omix", [S, D], BF16)
    xs = [scr("xs0", [S, D], F32), scr("xs1", [S, D], F32), scr("xs2", [S, D], F32)]
    h2Td = scr("h2Td", [128, 8, S], BF16)
    Gd = scr("Gd", [S, 32], F32)
    dnpre = scr("dnpre", [S + 4, 1152], F32)
    dnz = scr("dnz", [S, 384], F32)
    dU = scr("dU", [S, 768], F32)
    dWT = scr("dWT", [NT, 64, 12 * 128], BF16)
    dAT = scr("dAT", [NT, 128, 12 * 128], BF16)
    dKT = scr("dKT", [S, 768], BF16)
    dQT = scr("dQT", [NT, 64, 6 * 128], BF16)
    dE = scr("dE", [S, 24], F32)
    dO = scr("dO", [2, S, 384], F32)
    dbg_out = None
    if dbg:
        dbg_out = nc.dram_tensor("dbg", [S, D], F32, kind="ExternalOutput").ap()

    with ExitStack() as st0:
        fw = FW(nc, st0)

        uid = [0]

        def sb(stk, name, shape, dt):
            uid[0] += 1
            return stk.enter_context(nc.sbuf_tensor(f"s{uid[0]}_{name}", list(shape), dt))

        def ps(stk, name, shape, dt=F32):
            uid[0] += 1
            return stk.enter_context(nc.psum_tensor(f"p{uid[0]}_{name}", list(shape), dt))

        identf = sb(st0, "identf", [128, 128], F32)
        identb = sb(st0, "identb", [128, 128], BF16)
        epsT = sb(st0, "epsT", [128, 1], F32)
        cactB = sb(st0, "cactB", [128, 8, 128], F32)
        mod = sb(st0, "mod", [128, 6 * D], F32)
        A1 = sb(st0, "A1", [128, D], F32)
        A2 = sb(st0, "A2", [128, D], F32)
        fw.dma('sp', identf[:], identf_d)
        fw.memset(epsT[:], EPS)
        oneT = sb(st0, "oneT", [128, 1], F32)
        fw.memset(oneT[:], 1.0)
        onesf = sb(st0, "onesf", [128, 128], F32)
        fw.memset(onesf[:], 1.0)
        masks = sb(st0, "masks", [128, 9, 128], F32)
        fw.dma('sp', masks[:], masks_d)
        gT = sb(st0, "gT", [128, NT, 12], F32)
        betaT = sb(st0, "betaT", [128, NT, 12], F32)
        with ExitStack() as stp:
            zt = sb(stp, "zt", [128, 1152], F32)
            fw.memset(zt[:], 0.0)
            fw.dma('sp', dnpre[0:2, :], zt[0:2, :])
            fw.dma('sp', dnpre[S + 2:S + 4, :], zt[0:2, :])
            fw.barrier()
        fw.copy(identb[:], identf[:], e='dve')
        with ExitStack() as stp:
            ct = sb(stp, "ct", [128, 8], F32)
            ce = sb(stp, "ce", [128, 8], F32)
            fw.dma('sp', ct[:], c_pk)
            fw.act(ce[:], ct[:], AF.Exp, scale=-1.0)
            fw.ts(ce[:], ce[:], 1.0, None, ALU.add)
            fw.recip(ce[:], ce[:])
            fw.tt(ce[:], ce[:], ct[:], ALU.mult)
            fw.copy(cactB[:], bc(ce[:, :], 2, 128))
            fw.barrier()

        for l in range(depth):
            x_src = x_in if l == 0 else xs[1 + (l - 1) % 2]
            x_dst = y_out if l == depth - 1 else xs[1 + l % 2]
            last = (l == depth - 1)
            with ExitStack() as stp:
                psM = [ps(stp, f"psM{i}", [128, 512]) for i in range(2)]
                awt = [sb(stp, f"awt{i}", [128, 8, 512], F32) for i in range(2)]
                abt = sb(stp, "abt", [128, 6 * D], F32)
                g1t = sb(stp, "g1t", [128, D], F32)
                g2t = sb(stp, "g2t", [128, D], F32)
                fw.dma('sp', abt[:], ada_b[l])
                fw.dma('sp', g1t[:], g1_d[l])
                fw.dma('sp', g2t[:], g2_d[l])
                for cb in range(12):
                    a = awt[cb % 2]
                    fw.dma('sp', a[:], ada_w[l][:, cb * 512:(cb + 1) * 512].rearrange("(k p) n -> p k n", p=128))
                    fw.mmk(psM[cb % 2][:], [(cactB[:, k, :], a[:, k, :]) for k in range(8)])
                    fw.tt(mod[:, cb * 512:(cb + 1) * 512], psM[cb % 2][:], abt[:, cb * 512:(cb + 1) * 512], ALU.add)
                fw.stt(A1[:], mod[:, D:2 * D], 1.0, g1t[:], ALU.add, ALU.mult)
                fw.stt(A2[:], mod[:, 4 * D:5 * D], 1.0, g2t[:], ALU.add, ALU.mult)
                fw.barrier()
            sh1 = mod[:, 0:D]
            gt1 = mod[:, 2 * D:3 * D]
            sh2 = mod[:, 3 * D:4 * D]
            gt2 = mod[:, 5 * D:6 * D]

            with ExitStack() as stp:
                win = sb(stp, "win", [128, 8, INC], BF16)
                cosg = sb(stp, "cosg", [128, NT, 32], F32)
                sing = sb(stp, "sing", [128, NT, 32], F32)
                cosm = sb(stp, "cosm", [128, NT, 16], F32)
                sinm = sb(stp, "sinm", [128, NT, 16], F32)
                fw.dma('sp', cosg[:], cosg_d)
                fw.dma('sp', sing[:], sing_d)
                fw.dma('sp', cosm[:], cosm_d)
                fw.dma('sp', sinm[:], sinm_d)
                wst = [sb(stp, f"wst{i}", [128, INC], F32) for i in range(2)]
                for k in range(8):
                    fw.dma('sp', wst[k % 2][:], w_in[l][k * 128:(k + 1) * 128, :])
                    fw.copy(win[:, k, :], wst[k % 2][:], e=('pool' if k % 2 else 'dve'))
                wuq = sb(stp, "wuq", [128, 2, 384], BF16)
                wukv = sb(stp, "wukv", [128, 512], BF16)
                wuqs = sb(stp, "wuqs", [128, 2, 384], F32)
                wukvs = sb(stp, "wukvs", [128, 512], F32)
                gql = sb(stp, "gql", [128, 2], F32)
                gkvl = sb(stp, "gkvl", [128, 1], F32)
                fw.dma('sp', wuqs[:, 0, :], w_uq[l][0:128, :])
                fw.dma('sp', wuqs[0:64, 1, :], w_uq[l][128:192, :])
                fw.dma('sp', wukvs[:], w_ukv[l])
                fw.dma('sp', gql[:, 0:1], mql_g[l][0:128, :])
                fw.dma('sp', gql[0:64, 1:2], mql_g[l][128:192, :])
                fw.dma('sp', gkvl[:], mkvl_g[l])
                fw.ts(wuq[:, 0, :], wuqs[:, 0, :], gql[:, 0:1], None, ALU.mult)
                fw.ts(wuq[0:64, 1, :], wuqs[0:64, 1, :], gql[0:64, 1:2], None, ALU.mult)
                fw.ts(wukv[:], wukvs[:], gkvl[:, 0:1], None, ALU.mult)
                gains = {}
                for nm, dd, w_ in (("gq", gq_g, 64), ("gk", gk_g, 64), ("mqn", mqn_g, 64), ("mqr", mqr_g, 32),
                                   ("mkn", mkn_g, 64), ("mkr", mkr_g, 32)):
                    gains[nm] = sb(stp, "gn_" + nm, [128, w_], F32)
                    fw.dma('sp', gains[nm][:], dd[l])

                xt = [sb(stp, f"xt{i}", [128, D], F32) for i in range(2)]
                junk = sb(stp, "junk", [128, D], F32)
                ss = sb(stp, "ss", [128, 1], F32)
                hh = sb(stp, "hh", [128, D], F32)
                hT = sb(stp, "hT", [128, 8, 128], BF16)
                prs = [sb(stp, f"pr{i}", [128, INC], F32) for i in range(2)]
                tmpa = sb(stp, "tmpa", [128, 512], F32)
                tmpb = sb(stp, "tmpb", [128, 512], F32)
                nrm = sb(stp, "nrm", [128, 512], F32)
                s6 = sb(stp, "s6", [128, 16], F32)
                qb_ = sb(stp, "qb_", [128, 384], BF16)
                kb_ = sb(stp, "kb_", [128, 128], BF16)
                vb_ = sb(stp, "vb_", [128, 2, 65], BF16)
                qTs = sb(stp, "qTs", [128, 3, 128], BF16)
                kTs = sb(stp, "kTs", [128, 128], BF16)
                cqn = sb(stp, "cqn", [128, 192], BF16)
                ckvn = sb(stp, "ckvn", [128, 128], BF16)
                cqT = sb(stp, "cqT", [128, 2, 128], BF16)
                ckvT = sb(stp, "ckvT", [128, 128], BF16)
                qup = sb(stp, "qup", [128, 384], F32)
                kvup = sb(stp, "kvup", [128, 512], F32)
                qm = sb(stp, "qm", [128, 4, 96], BF16)
                km = sb(stp, "km", [128, 4, 96], BF16)
                vmb = sb(stp, "vmb", [128, 4, 65], BF16)
                krr = sb(stp, "krr", [128, 32], F32)
                qTms = sb(stp, "qTms", [96, 4, 128], BF16)
                kTms = sb(stp, "kTms", [96, 4, 128], BF16)
                psW = ps(stp, "psW", [128, 8, 128])
                psP = [ps(stp, f"psP{i}", [128, 512]) for i in range(2)]
                psTb = ps(stp, "psTb", [128, 8, 128], BF16)
                psU = ps(stp, "psU", [128, 512])
                fw.memset(vb_[:], 1.0)
                fw.memset(vmb[:], 1.0)
                negA = sb(stp, "negA", [128, 12], F32)
                dtb = sb(stp, "dtb", [128, 12], F32)
                t12 = [sb(stp, f"t12_{i}", [128, 12], F32) for i in range(3)]
                fw.dma('sp', negA[:], alog_d[l])
                fw.dma('sp', dtb[:], dtb_d[l])
                fw.act(negA[:], negA[:], AF.Exp)
                fw.ts(negA[:], negA[:], -1.0, None, ALU.mult)

                def headnorm(src, H, Dh, gain, out, mean):
                    t = tmpa[:, 0:H * Dh].rearrange("p (h d) -> p h d", h=H)
                    fw.tt(t, src, src, ALU.mult)
                    fw.red(s6[:, 0:H], t)
                    fw.act(s6[:, 0:H], s6[:, 0:H], AF.Ln, bias=epsT[:], scale=(1.0 / Dh if mean else 1.0))
                    fw.act(s6[:, 0:H], s6[:, 0:H], AF.Exp, scale=-0.5)
                    fw.tt(t, src, bc(s6[:, 0:H], 2, Dh), ALU.mult)
                    if gain is not None:
                        fw.tt(out, t, bc(gain[:, :], 1, H), ALU.mult, e='pool')
                    else:
                        fw.copy(out, t, e='pool')

                def rope(src, H, R, cs, sn, out):
                    h2 = R // 2
                    x1 = src[:, :, 0:h2]
                    x2 = src[:, :, h2:R]
                    c_ = bc(cs, 1, H)
                    s_ = bc(sn, 1, H)
                    ta = tmpa[:, 0:H * h2].rearrange("p (h d) -> p h d", h=H)
                    tb = tmpb[:, 0:H * h2].rearrange("p (h d) -> p h d", h=H)
                    fw.tt(ta, x1, c_, ALU.mult)
                    fw.tt(tb, x2, s_, ALU.mult, e='pool')
                    fw.tt(out[:, :, 0:h2], ta, tb, ALU.subtract)
                    fw.tt(ta, x1, s_, ALU.mult)
                    fw.tt(tb, x2, c_, ALU.mult, e='pool')
                    fw.tt(out[:, :, h2:R], ta, tb, ALU.add)

                fw.dma('sp', xt[0][:], x_src[0:128, :])

                def frontA(i):
                    xc = xt[i % 2]
                    pr = prs[i % 2]
                    if i + 1 < NT:
                        fw.dma('sp', xt[(i + 1) % 2][:], x_src[(i + 1) * 128:(i + 2) * 128, :])
                    fw.act(junk[:], xc[:], AF.Square, accum_out=ss[:])
                    fw.act(ss[:], ss[:], AF.Ln, bias=epsT[:], scale=1.0 / D)
                    fw.act(ss[:], ss[:], AF.Exp, scale=-0.5)
                    fw.stt(hh[:], xc[:], ss[:, 0:1], A1[:], ALU.mult, ALU.mult)
                    fw.tt(hh[:], hh[:], sh1, ALU.add, e='pool')
                    fw.tr([(psW[:, k, :], hh[:, k * 128:(k + 1) * 128], identf[:]) for k in range(8)])
                    fw.copy(hT[:], psW[:], e='act')
                    for cb in range(5):
                        c0 = cb * 512
                        c1 = min(INC, c0 + 512)
                        pp = psP[cb % 2]
                        fw.mmk(pp[:, 0:c1 - c0], [(hT[:, k, :], win[:, k, c0:c1]) for k in range(8)])
                        fw.copy(pr[:, c0:c1], pp[:, 0:c1 - c0], e=('dve' if cb % 2 == 0 else 'act'))

                def backA(i):
                    pr = prs[i % 2]
                    tsl = slice(i * 128, (i + 1) * 128)
                    fw.dma('pool', (dnpre[2 + i * 128:2 + (i + 1) * 128, :], ("dnpre", i)), pr[:, 0:1152])
                    fw.dma('pool', (dnz[tsl, :], ("dnz", i)), pr[:, 1152:1536])
                    fw.act(t12[0][:], pr[:, 1536:1548], AF.Exp, scale=-1.0)
                    fw.ts(t12[0][:], t12[0][:], 1.0, None, ALU.add)
                    fw.recip(betaT[:, i, :], t12[0][:])
                    fw.tt(t12[1][:], pr[:, 1548:1560], dtb[:], ALU.add)
                    fw.ts(t12[2][:], t12[1][:], -1.0, None, ALU.mult)
                    fw.tt(t12[2][:], t12[2][:], t12[1][:], ALU.max)
                    fw.act(t12[2][:], t12[2][:], AF.Exp, scale=-1.0)
                    fw.act(t12[2][:], t12[2][:], AF.Ln, bias=oneT[:], scale=1.0)
                    fw.ts(t12[1][:], t12[1][:], 0.0, None, ALU.max)
                    fw.tt(t12[1][:], t12[1][:], t12[2][:], ALU.add)
                    fw.tt(gT[:, i, :], t12[1][:], negA[:], ALU.mult)
                    qv = pr[:, 1560:1944].rearrange("p (h d) -> p h d", h=6)
                    nq = nrm[:, 0:384].rearrange("p (h d) -> p h d", h=6)
                    headnorm(qv, 6, 64, gains["gq"], nq, True)
                    rope(nq, 6, 64, cosg[:, i, :], sing[:, i, :], qb_[:, :].rearrange("p (h d) -> p h d", h=6))
                    fw.tr([(psTb[:, j, :], qb_[:, j * 128:(j + 1) * 128], identb[:]) for j in range(3)])
                    fw.copy(qTs[:], psTb[:, 0:3, :])
                    fw.dma('pool', (qTg[:, :, tsl].rearrange("j p t -> p j t"), ("qTg", i)), qTs[:])
                    kv_ = pr[:, 1944:2072].rearrange("p (h d) -> p h d", h=2)
                    nk = nrm[:, 0:128].rearrange("p (h d) -> p h d", h=2)
                    headnorm(kv_, 2, 64, gains["gk"], nk, True)
                    rope(nk, 2, 64, cosg[:, i, :], sing[:, i, :], kb_[:, :].rearrange("p (h d) -> p h d", h=2))
                    fw.tr([(psTb[:, 3, :], kb_[:], identb[:])])
                    fw.copy(kTs[:], psTb[:, 3, :])
                    fw.dma('pool', (kTg[:, tsl], ("kTg", i)), kTs[:])
                    fw.copy(vb_[:, :, 0:64], pr[:, 2072:2200].rearrange("p (h d) -> p h d", h=2), e='pool')
                    fw.dma('pool', (vg[tsl, :], ("vg", i)), vb_[:, :, :].rearrange("p h d -> p (h d)"))
                    cq = pr[:, 2200:2392].rearrange("p (h d) -> p h d", h=1)
                    headnorm(cq, 1, 192, None, cqn[:, :].rearrange("p (h d) -> p h d", h=1), True)
                    ckv = pr[:, 2392:2520].rearrange("p (h d) -> p h d", h=1)
                    headnorm(ckv, 1, 128, None, ckvn[:, :].rearrange("p (h d) -> p h d", h=1), True)
                    fw.tr([(psTb[:, 4, :], cqn[:, 0:128], identb[:]),
                           (psTb[0:64, 5, :], cqn[:, 128:192], identb[:]),
                           (psTb[:, 6, :], ckvn[:], identb[:])])
                    fw.copy(cqT[:, 0, :], psTb[:, 4, :])
                    fw.copy(cqT[0:64, 1, :], psTb[0:64, 5, :])
                    fw.copy(ckvT[:], psTb[:, 6, :])
                    fw.mm([(psU[:, 0:384], cqT[:, 0, :], wuq[:, 0, :], True, False),
                           (psU[:, 0:384], cqT[0:64, 1, :], wuq[0:64, 1, :], False, True)])
                    fw.copy(qup[:], psU[:, 0:384])
                    fw.mm([(psU[:], ckvT[:], wukv[:], True, True)])
                    fw.copy(kvup[:], psU[:])
                    qu = qup[:, :].rearrange("p (h d) -> p h d", h=4)
                    qmv = qm[:, :, :]
                    n4 = nrm[:, 0:256].rearrange("p (h d) -> p h d", h=4)
                    headnorm(qu[:, :, 0:64], 4, 64, gains["mqn"], qmv[:, :, 0:64], True)
                    n4r = nrm[:, 256:384].rearrange("p (h d) -> p h d", h=4)
                    headnorm(qu[:, :, 64:96], 4, 32, gains["mqr"], n4r, True)
                    rope(n4r, 4, 32, cosm[:, i, :], sinm[:, i, :], qmv[:, :, 64:96])
                    ku = kvup[:, :].rearrange("p (h d) -> p h d", h=4)
                    headnorm(ku[:, :, 0:64], 4, 64, gains["mkn"], km[:, :, 0:64], True)
                    krv = pr[:, 2520:2552].rearrange("p (h d) -> p h d", h=1)
                    n1r = nrm[:, 384:416].rearrange("p (h d) -> p h d", h=1)
                    headnorm(krv, 1, 32, gains["mkr"], n1r, True)
                    rope(n1r, 1, 32, cosm[:, i, :], sinm[:, i, :], krr[:, :].rearrange("p (h d) -> p h d", h=1))
                    fw.copy(km[:, :, 64:96], bc(krr[:, :], 1, 4), e='pool')
                    fw.copy(vmb[:, :, 0:64], ku[:, :, 64:128], e='pool')
                    fw.dma('pool', (vm[tsl, :], ("vm", i)), vmb[:, :, :].rearrange("p h d -> p (h d)"))
                    fw.tr([(psTb[0:96, hh_, :], qm[:, hh_, :], identb[:]) for hh_ in range(4)])
                    fw.copy(qTms[:], psTb[0:96, 0:4, :])
                    fw.dma('pool', (qTm[:, :, tsl].rearrange("h p t -> p h t"), ("qTm", i)), qTms[:])
                    fw.tr([(psTb[0:96, 4 + hh_, :], km[:, hh_, :], identb[:]) for hh_ in range(4)])
                    fw.copy(kTms[:], psTb[0:96, 4:8, :])
                    fw.dma('pool', (kTm[:, :, tsl].rearrange("h p t -> p h t"), ("kTm", i)), kTms[:])
                frontA(0)
                for i in range(NT):
                    if i + 1 < NT:
                        frontA(i + 1)
                    backA(i)
                fw.barrier()
            if stop_after == 'A':
                break

            with ExitStack() as stp:
                kT_all = sb(stp, "kT_all", [128, S], BF16)
                v_all = sb(stp, "v_all", [128, NT, 130], BF16)
                vm_all = sb(stp, "vm_all", [128, NT, 260], BF16)
                kTm_h = [sb(stp, f"kTm_h{i}", [96, S], BF16) for i in range(2)]
                qTt = [sb(stp, f"qTt{i}", [128, 512], BF16) for i in range(2)]
                pT = [sb(stp, f"pT{i}", [128, 2, 512], BF16) for i in range(2)]
                oTs = sb(stp, "oTs", [65, 512], F32)
                rcp = sb(stp, "rcp", [128, 4], F32)
                ob = [sb(stp, f"ob{i}", [128, 4, 64], BF16) for i in range(2)]
                psS = [ps(stp, f"psS{i}", [128, 2, 512]) for i in range(2)]
                acc = [ps(stp, f"acc{i}", [65, 512]) for i in range(2)]
                psO = ps(stp, "psO", [128, 4, 128])
                allq = [("qTg", i) for i in range(NT)]
                fw.dma('sp', kT_all[:], kTg, extra_reads=[("kTg", i) for i in range(NT)])
                fw.dma('sp', v_all[:], vg.rearrange("(t p) c -> p t c", p=128), extra_reads=[("vg", i) for i in range(NT)])
                fw.dma('sp', vm_all[:], vm.rearrange("(t p) c -> p t c", p=128), extra_reads=[("vm", i) for i in range(NT)])
                ucount = [0]

                pend = [None]

                def unit(kT_ap, qT_ap, vfn, scale, qb, col):
                    u = ucount[0]
                    ucount[0] += 1
                    ac = acc[u % 2]
                    NP = NT // 2

                    def Smm(kp):
                        pss = psS[kp % 2]
                        fw.mm([(pss[:, 0, :], kT_ap[:, (2 * kp) * 128:(2 * kp + 1) * 128], qT_ap, True, True),
                               (pss[:, 1, :], kT_ap[:, (2 * kp + 1) * 128:(2 * kp + 2) * 128], qT_ap, True, True)])
                    Smm(0)
                    if pend[0] is not None:
                        pend[0]()
                        pend[0] = None
                    for kp in range(NP):
                        if kp + 1 < NP:
                            Smm(kp + 1)
                        pt = pT[kp % 2]
                        fw.act(pt[:], psS[kp % 2][:], AF.Exp, scale=scale)
                        fw.mm([(ac[:], vfn(2 * kp), pt[:, 0, :], kp == 0, False),
                               (ac[:], vfn(2 * kp + 1), pt[:, 1, :], False, kp == NP - 1)])

                    def tail():
                        fw.copy(oTs[:], ac[:], e='dve')
                        fw.tr([(psO[:, s_, 0:65], oTs[:, s_ * 128:(s_ + 1) * 128], identf[0:65, 0:65]) for s_ in range(4)])
                        fw.recip(rcp[:], psO[:, :, 64])
                        o_ = ob[u % 2]
                        fw.tt(o_[:], psO[:, :, 0:64], bc(rcp[:, :], 2, 64), ALU.mult)
                        fw.dma('pool', (omix[qb * 512:(qb + 1) * 512, col:col + 64].rearrange("(s p) d -> p s d", p=128),
                                        ("omix", qb, col)), o_[:])
                    pend[0] = tail

                qcnt = 0
                for qb in range(NQB):
                    for j in range(3):
                        qt = qTt[qcnt % 2]
                        qcnt += 1
                        fw.dma('sp', qt[:], qTg[j, :, qb * 512:(qb + 1) * 512], extra_reads=allq)
                        for half in range(2):
                            head = j + 3 * half
                            rs = slice(half * 64, (half + 1) * 64)
                            unit(kT_all[rs, :], qt[rs, :],
                                 (lambda kt, half=half: v_all[:, kt, half * 65:(half + 1) * 65]),
                                 0.125, qb, 384 + head * 64)
                allqm = [("qTm", i) for i in range(NT)]
                for h in range(4):
                    kh = kTm_h[h % 2]
                    fw.dma('sp', kh[:], kTm[h], extra_reads=[("kTm", i) for i in range(NT)])
                    for qb in range(NQB):
                        qt = qTt[qcnt % 2]
                        qcnt += 1
                        fw.dma('sp', qt[0:96, :], qTm[h, :, qb * 512:(qb + 1) * 512], extra_reads=allqm)
                        unit(kh[:, :], qt[0:96, :], (lambda kt, h=h: vm_all[:, kt, h * 65:(h + 1) * 65]),
                             96 ** -0.5, qb, 768 + h * 64)
                if pend[0] is not None:
                    pend[0]()
                    pend[0] = None
                fw.barrier()
            if stop_after == 'B':
                break
            with ExitStack() as stp:
                wcv = sb(stp, "wcv", [128, 5, 1152], F32)
                fw.dma('sp', wcv[:], wconv_d[l].rearrange("p (j c) -> p j c", j=5))
                shf1 = [sb(stp, f"shf_{j}", [128, 1152], F32) for j in range(5)]
                shf = [shf1, shf1]
                cacc = sb(stp, "cacc", [128, 1152], F32)
                ctmp = sb(stp, "ctmp", [128, 1152], F32)
                qkvs = [sb(stp, f"qkv{i}", [128, 1152], F32) for i in range(2)]
                s12 = sb(stp, "s12", [128, 12], F32)
                qnb = sb(stp, "qnb", [128, 384], BF16)
                knb = sb(stp, "knb", [128, 384], BF16)
                kn32s = [sb(stp, f"kn32{i}", [128, 384], F32) for i in range(2)]
                qkTs = [sb(stp, f"qkT{i}", [64, 12, 128], BF16) for i in range(2)]
                gcss = [sb(stp, f"gcs{i}", [128, 24], F32) for i in range(2)]
                exs = [sb(stp, f"ex{i}", [128, 36], F32) for i in range(2)]
                Xd = sb(stp, "Xd", [128, 12, 128], F32)
                dec = sb(stp, "dec", [128, 12, 128], F32)
                Lt = sb(stp, "Lt", [128, 12, 128], F32)
                Pb = [sb(stp, f"Pb{i}", [128, 12, 128], F32) for i in range(2)]
                Qb = [sb(stp, f"Qb{i}", [128, 12, 128], F32) for i in range(2)]
                aqk = sb(stp, "aqk", [128, 12, 128], BF16)
                aqkT = sb(stp, "aqkT", [128, 12, 128], BF16)
                X32 = sb(stp, "X32", [128, 12, 128], F32)
                Wb = sb(stp, "Wb", [128, 12, 64], BF16)
                wT = sb(stp, "wT", [64, 12, 128], BF16)
                ktl = sb(stp, "ktl", [128, 12, 64], BF16)
                coef = sb(stp, "coef", [128, 12], F32)
                psA3 = ps(stp, "psA3", [128, 12, 128])
                psB3 = ps(stp, "psB3", [128, 12, 128])
                psT12f = ps(stp, "psT12", [128, 16, 128], BF16)
                psT12 = psT12f[:, 0:12, :]
                UTm, LTm = masks[:, 0, :], masks[:, 1, :]
                negm = masks[:, 2:4, :]
                strm = masks[:, 4:6, :]
                m0, m1s, m2s = masks[:, 6, :], masks[:, 7, :], masks[:, 8, :]
                bfn = {}
                for nm_ in ('L1Tb', 'L2Tb', 'T32b', 'T32Tb', 'Wz', 'T64b', 'T64Tb', 'Rb', 'Yb'):
                    bfn[nm_] = sb(stp, nm_, [128, 12, 128], BF16)

                def v4(t, w=128):
                    return t[:, :, :].rearrange("p (a h) d -> p a h d", a=2)

                def load_shift(i):
                    for j in range(5):
                        fw.dma('sp', shf[i % 2][j][:], dnpre[i * 128 + j:i * 128 + j + 128, :])
                load_shift(0)

                def frontC(i):
                    qkv, kn32, qkT, gcs, ex = qkvs[i % 2], kn32s[i % 2], qkTs[i % 2], gcss[i % 2], exs[i % 2]
                    sh = shf[i % 2]
                    tsl = slice(i * 128, (i + 1) * 128)
                    fw.tt(cacc[:], sh[0][:], wcv[:, 0, :], ALU.mult, e='pool')
                    for j in range(1, 5):
                        fw.tt(ctmp[:], sh[j][:], wcv[:, j, :], ALU.mult, e='pool')
                        fw.tt(cacc[:], cacc[:], ctmp[:], ALU.add)
                    if i + 1 < NT:
                        load_shift(i + 1)
                    fw.act(ctmp[:], cacc[:], AF.Exp, scale=-1.0)
                    fw.act(ctmp[:], ctmp[:], AF.Ln, bias=oneT[:], scale=1.0)
                    fw.act(ctmp[:], ctmp[:], AF.Exp, scale=-1.0)
                    fw.tt(qkv[:], cacc[:], ctmp[:], ALU.mult)
                    qk12 = qkv[:, 0:768].rearrange("p (h d) -> p h d", h=12)
                    c12 = cacc[:, 0:768].rearrange("p (h d) -> p h d", h=12)
                    fw.tt(c12, qk12, qk12, ALU.mult, e='pool')
                    fw.red(s12[:], c12)
                    fw.act(s12[:], s12[:], AF.Ln, bias=epsT[:], scale=1.0)
                    fw.act(s12[:], s12[:], AF.Exp, scale=-0.5)
                    fw.ts(s12[:, 0:6], s12[:, 0:6], 0.125, None, ALU.mult)
                    fw.tt(qnb[:, :].rearrange("p (h d) -> p h d", h=6), qk12[:, 0:6, :], bc(s12[:, 0:6], 2, 64), ALU.mult)
                    fw.tt(kn32[:, :].rearrange("p (h d) -> p h d", h=6), qk12[:, 6:12, :], bc(s12[:, 6:12], 2, 64), ALU.mult)
                    fw.copy(knb[:], kn32[:], e='pool')
                    fw.tr([(psT12[0:64, h, :], qnb[:, h * 64:(h + 1) * 64], identb[:]) for h in range(6)] +
                          [(psT12[0:64, 6 + h, :], knb[:, h * 64:(h + 1) * 64], identb[:]) for h in range(6)])
                    fw.copy(qkT[:], psT12[0:64, :, :], e='act')
                    fw.dma('pool', dQT[i].rearrange("p (h t) -> p h t", h=6), qkT[:, 0:6, :])
                    g_ = gT[:, i, :]
                    psG = psB3[:, 11, 0:24]
                    fw.mm([(psB3[:, 11, 0:6], UTm, g_[:, 0:6], True, True),
                           (psB3[:, 11, 6:12], LTm, g_[:, 6:12], True, True),
                           (psB3[:, 11, 12:24], onesf[:], g_, True, True)])
                    fw.copy(gcs[:], psG)
                    fw.act(ex[:, 0:24], gcs[:], AF.Exp)
                    fw.tt(ex[:, 24:36], gcs[:, 12:24], gcs[:, 0:12], ALU.subtract)
                    fw.act(ex[:, 24:36], ex[:, 24:36], AF.Exp)
                    fw.dma('pool', dE[tsl, :], ex[:, 0:24])

                def backC(i):
                    qkv, kn32, qkT, gcs, ex = qkvs[i % 2], kn32s[i % 2], qkTs[i % 2], gcss[i % 2], exs[i % 2]
                    tsl = slice(i * 128, (i + 1) * 128)
                    fw.tt(Xd[:], bc(identf[:, :], 1, 12), bc(gcs[:, 0:12], 2, 128), ALU.mult, e='pool')
                    Xf = Xd[:, :, :].rearrange("p u j -> p (u j)")
                    Af = psA3[:, :, :].rearrange("p u j -> p (u j)")
                    fw.mm([(Af[:, c * 512:(c + 1) * 512], onesf[:], Xf[:, c * 512:(c + 1) * 512], True, True) for c in range(3)])
                    fw.tt(dec[:], bc(gcs[:, 0:12], 2, 128), psA3[:], ALU.subtract)
                    fw.tt(v4(dec), v4(dec), bc(negm, 2, 6), ALU.add, e='pool')
                    fw.act(dec[:], dec[:], AF.Exp)
                    fw.mm([(psB3[:, h, :], qkT[:, 6 + h, :], qkT[:, 6 + h, :], True, True) for h in range(6)] +
                          [(psB3[:, 6 + h, :], qkT[:, h, :], qkT[:, 6 + h, :], True, True) for h in range(6)])
                    fw.tt(v4(Lt), v4(dec), bc(psB3[:, 0:6, :], 1, 2), ALU.mult)
                    fw.tt(v4(aqk), v4(dec), bc(psB3[:, 6:12, :], 1, 2), ALU.mult)
                    fw.stt(Xd[:], Lt[:], -1.0, bc(betaT[:, i, :], 2, 128), ALU.mult, ALU.mult)
                    fw.tr([(psB3[:, u, :], Xd[:, u, :], identf[:]) for u in range(12)])
                    fw.tt(Pb[0][:], Xd[:], bc(m0, 1, 12), ALU.mult, e='pool')
                    fw.tt(Qb[0][:], psB3[:], bc(m0, 1, 12), ALU.mult)
                    fw.tt(bfn['L1Tb'][:], psB3[:], bc(m1s, 1, 12), ALU.mult)
                    fw.tt(bfn['L2Tb'][:], psB3[:], bc(m2s, 1, 12), ALU.mult)
                    fw.tr([(psT12[:, u, :], aqk[:, u, :], identb[:]) for u in range(12)])
                    fw.copy(aqkT[:], psT12, e='act')
                    fw.dma('pool', dAT[i].rearrange("p (u t) -> p u t", u=12), aqkT[:])
                    fw.tt(coef[:], betaT[:, i, :], ex[:, 0:12], ALU.mult)
                    X4 = v4(bfn['Rb'])
                    v6 = qkv[:, 768:1152].rearrange("p (h d) -> p h d", h=6)
                    k6 = kn32[:, :].rearrange("p (h d) -> p h d", h=6)
                    b4 = betaT[:, i, :].rearrange("p (a h) -> p a h", a=2)
                    c4 = coef[:, :].rearrange("p (a h) -> p a h", a=2)
                    e4 = ex[:, 24:36].rearrange("p (a h) -> p a h", a=2)
                    fw.tt(X4[:, :, :, 0:64], bc(v6, 1, 2), bc(b4, 3, 64), ALU.mult)
                    fw.tt(X4[:, :, :, 64:128], bc(k6, 1, 2), bc(c4, 3, 64), ALU.mult, e='pool')
                    fw.tt(ktl[:, :, :].rearrange("p (a h) d -> p a h d", a=2), bc(k6, 1, 2), bc(e4, 3, 64), ALU.mult, e='pool')
                    fw.dma('pool', dKT[tsl, :], ktl[:, :, :].rearrange("p u d -> p (u d)"))
                    fw.tt(Lt[:], Pb[0][:], bc(identf[:, :], 1, 12), ALU.add, e='pool')
                    for k in range(4):
                        P_, Q_ = Pb[k % 2], Qb[k % 2]
                        Pn, Qn = Pb[(k + 1) % 2], Qb[(k + 1) % 2]
                        fw.mm([(psA3[:, u, :], P_[:, u, :], Q_[:, u, :], True, True) for u in range(12)])
                        fw.copy(Qn[:], psA3[:], e='act')
                        if k < 3:
                            fw.mm([(psB3[:, u, :], Q_[:, u, :], P_[:, u, :], True, True) for u in range(12)])
                            fw.copy(Pn[:], psB3[:], e='act')
                        fw.mm([(psA3[:, u, :], Qn[:, u, :], Lt[:, u, :], True, True) for u in range(12)])
                        if k < 3:
                            fw.tt(Lt[:], Lt[:], psA3[:], ALU.add)
                        else:
                            fw.tt(bfn['T32b'][:], Lt[:], psA3[:], ALU.add)
                    fw.tr([(psT12[:, u, :], bfn['T32b'][:, u, :], identb[:]) for u in range(12)])
                    fw.copy(bfn['T32Tb'][:], psT12, e='act')
                    fw.mm([(psA3[:, u, :], bfn['L1Tb'][:, u, :], bfn['T32b'][:, u, :], True, True) for u in range(12)])
                    fw.copy(bfn['Wz'][:], psA3[:], e='act')
                    fw.mm([(psB3[:, u, :], bfn['T32Tb'][:, u, :], bfn['Wz'][:, u, :], True, True) for u in range(12)])
                    fw.tt(bfn['T64b'][:], bfn['T32b'][:], psB3[:], ALU.subtract)
                    fw.tr([(psT12[:, u, :], bfn['T64b'][:, u, :], identb[:]) for u in range(12)])
                    fw.copy(bfn['T64Tb'][:], psT12, e='act')
                    fw.mm([(psA3[:, u, :], bfn['T64Tb'][:, u, :], bfn['Rb'][:, u, :], True, True) for u in range(12)])
                    fw.copy(dec[:], psA3[:], e='act')
                    fw.copy(bfn['Yb'][:], dec[:], e='act')
                    fw.mm([(psB3[:, u, :], bfn['L2Tb'][:, u, :], bfn['Yb'][:, u, :], True, True) for u in range(12)])
                    fw.copy(bfn['Wz'][:], psB3[:], e='act')
                    fw.mm([(psA3[:, u, :], bfn['T64Tb'][:, u, :], bfn['Wz'][:, u, :], True, True) for u in range(12)])
                    fw.tt(X32[:], dec[:], psA3[:], ALU.subtract)
                    fw.dma('pool', dU[tsl, :].rearrange("p (u d) -> p u d", u=12), X32[:, :, 0:64])
                    fw.copy(Wb[:], X32[:, :, 64:128], e='act')
                    fw.tr([(psT12[0:64, u, :], Wb[:, u, :], identb[:]) for u in range(12)])
                    fw.copy(wT[:], psT12[0:64, :, :], e='act')
                    fw.dma('pool', dWT[i].rearrange("p (u t) -> p u t", u=12), wT[:])
                frontC(0)
                for i in range(NT):
                    if i + 1 < NT:
                        frontC(i + 1)
                    backC(i)
                fw.barrier()

            with ExitStack() as stp:
                S32 = [sb(stp, f"S32_{d}", [64, 6, 64], F32) for d in range(2)]
                Sb = [sb(stp, f"Sb_{d}", [64, 6, 64], BF16) for d in range(2)]
                for d in range(2):
                    fw.memset(S32[d][:], 0.0)
                    fw.memset(Sb[d][:], 0.0)
                Ud = [[sb(stp, f"Ud{d}{b}", [128, 6, 64], F32) for b in range(2)] for d in range(2)]
                WTd = [[sb(stp, f"WTd{d}{b}", [64, 6, 128], BF16) for b in range(2)] for d in range(2)]
                ATd = [[sb(stp, f"ATd{d}{b}", [128, 6, 128], BF16) for b in range(2)] for d in range(2)]
                KTd = [[sb(stp, f"KTd{d}{b}", [128, 6, 64], BF16) for b in range(2)] for d in range(2)]
                QTd = [[sb(stp, f"QTd{d}{b}", [64, 6, 128], BF16) for b in range(2)] for d in range(2)]
                Ed = [[sb(stp, f"Ed{d}{b}", [128, 24], F32) for b in range(2)] for d in range(2)]
                vnew = [sb(stp, f"vnew{d}", [128, 6, 64], BF16) for d in range(2)]
                ot = [sb(stp, f"ot{d}", [128, 6, 64], F32) for d in range(2)]
                od = [sb(stp, f"od{d}", [128, 6, 64], F32) for d in range(2)]
                stmp = [sb(stp, f"stmp{d}", [64, 6, 64], F32) for d in range(2)]
                psV = [ps(stp, f"psV{d}", [128, 8, 64])[:, 0:6, :] for d in range(2)]
                psO1 = [ps(stp, f"psO1{d}", [128, 8, 64])[:, 0:6, :] for d in range(2)]
                psO2 = [ps(stp, f"psO2{d}", [128, 8, 64])[:, 0:6, :] for d in range(2)]
                psS = [ps(stp, f"psS{d}", [64, 8, 64])[:, 0:6, :] for d in range(2)]

                def load_scan(s_):
                    for d in range(2):
                        n = s_ if d == 0 else NT - 1 - s_
                        b = s_ % 2
                        rows = slice(n * 128, (n + 1) * 128)
                        fw.dma('sp', Ud[d][b][:], dU[rows, d * 384:(d + 1) * 384].rearrange("p (h v) -> p h v", h=6))
                        fw.dma('sp', WTd[d][b][:], dWT[n][:, d * 768:(d + 1) * 768].rearrange("p (h t) -> p h t", h=6))
                        fw.dma('sp', ATd[d][b][:], dAT[n][:, d * 768:(d + 1) * 768].rearrange("p (h t) -> p h t", h=6))
                        fw.dma('sp', KTd[d][b][:], dKT[rows, d * 384:(d + 1) * 384].rearrange("p (h v) -> p h v", h=6))
                        fw.dma('sp', QTd[d][b][:], dQT[n].rearrange("p (h t) -> p h t", h=6))
                        fw.dma('sp', Ed[d][b][:], dE[rows, :])
                load_scan(0)
                for s_ in range(NT):
                    if s_ + 1 < NT:
                        load_scan(s_ + 1)
                    b = s_ % 2
                    ns = [s_, NT - 1 - s_]
                    for d in range(2):
                        fw.tt(stmp[d][:], S32[d][:], bc(Ed[d][b][0:64, 12 + d * 6:18 + d * 6], 2, 64), ALU.mult)
                    for d in range(2):
                        fw.mm([(psV[d][:, h, :], WTd[d][b][:, h, :], Sb[d][:, h, :], True, True) for h in range(6)] +
                              [(psO1[d][:, h, :], QTd[d][b][:, h, :], Sb[d][:, h, :], True, True) for h in range(6)])
                    for d in range(2):
                        fw.tt(vnew[d][:], Ud[d][b][:], psV[d][:], ALU.subtract)
                    for d in range(2):
                        fw.mm([(psS[d][:, h, :], KTd[d][b][:, h, :], vnew[d][:, h, :], True, True) for h in range(6)] +
                              [(psO2[d][:, h, :], ATd[d][b][:, h, :], vnew[d][:, h, :], True, True) for h in range(6)])
                    for d in range(2):
                        fw.tt(S32[d][:], stmp[d][:], psS[d][:], ALU.add)
                        fw.copy(Sb[d][:], S32[d][:], e='act')
                    for d in range(2):
                        fw.tt(ot[d][:], psO1[d][:], bc(Ed[d][b][:, d * 6:d * 6 + 6], 2, 64), ALU.mult)
                        fw.tt(od[d][:], ot[d][:], psO2[d][:], ALU.add)
                        fw.dma('pool', dO[d, ns[d] * 128:(ns[d] + 1) * 128, :], od[d][:, :, :].rearrange("p h v -> p (h v)"))
                fw.barrier()

            with ExitStack() as stp:
                gdn = sb(stp, "gdn", [128, 64], F32)
                fw.dma('sp', gdn[:], dng_d[l])
                o0 = [sb(stp, f"o0_{b}", [128, 384], F32) for b in range(2)]
                o1 = [sb(stp, f"o1_{b}", [128, 384], F32) for b in range(2)]
                zt_ = [sb(stp, f"zt_{b}", [128, 384], F32) for b in range(2)]
                osum = sb(stp, "osum", [128, 384], F32)
                otmp = sb(stp, "otmp", [128, 384], F32)
                ze = sb(stp, "ze", [128, 384], F32)
                s6c = sb(stp, "s6c", [128, 6], F32)
                oab = sb(stp, "oab", [128, 384], BF16)

                def load_c3(i):
                    fw.dma('sp', o0[i % 2][:], dO[0, i * 128:(i + 1) * 128, :])
                    fw.dma('sp', o1[i % 2][:], dO[1, i * 128:(i + 1) * 128, :])
                    fw.dma('sp', zt_[i % 2][:], dnz[i * 128:(i + 1) * 128, :])
                load_c3(0)
                for i in range(NT):
                    if i + 1 < NT:
                        load_c3(i + 1)
                    b = i % 2
                    fw.tt(osum[:], o0[b][:], o1[b][:], ALU.add)
                    o6 = osum[:, :].rearrange("p (h d) -> p h d", h=6)
                    t6 = otmp[:, :].rearrange("p (h d) -> p h d", h=6)
                    fw.tt(t6, o6, o6, ALU.mult, e='pool')
                    fw.red(s6c[:], t6)
                    fw.act(s6c[:], s6c[:], AF.Ln, bias=epsT[:], scale=1.0 / 64)
                    fw.act(s6c[:], s6c[:], AF.Exp, scale=-0.5)
                    fw.tt(t6, o6, bc(s6c[:, :], 2, 64), ALU.mult)
                    fw.tt(t6, t6, bc(gdn[:, :], 1, 6), ALU.mult, e='pool')
                    fw.act(ze[:], zt_[b][:], AF.Exp, scale=-1.0)
                    fw.ts(ze[:], ze[:], 1.0, None, ALU.add, e='pool')
                    fw.recip(ze[:], ze[:])
                    fw.tt(ze[:], ze[:], zt_[b][:], ALU.mult)
                    fw.tt(oab[:], otmp[:], ze[:], ALU.mult)
                    fw.dma('pool', omix[i * 128:(i + 1) * 128, 0:384], oab[:])
                fw.barrier()
            if stop_after == 'C':
                break

            with ExitStack() as stp:
                wo = sb(stp, "wo", [128, 8, D], BF16)
                wos = [sb(stp, f"wos{i}", [128, D], F32) for i in range(2)]
                for k in range(8):
                    fw.dma('sp', wos[k % 2][:], w_out[l][k * 128:(k + 1) * 128, :])
                    fw.copy(wo[:, k, :], wos[k % 2][:], e=('pool' if k % 2 else 'dve'))
                wr = sb(stp, "wr", [128, 8, 36], F32)
                br = sb(stp, "br", [128, 36], F32)
                fw.dma('sp', wr[:], w_r[l].rearrange("(k p) n -> p k n", p=128))
                fw.dma('sp', br[:], b_r[l])
                om = [sb(stp, f"om{i}", [128, D], BF16) for i in range(2)]
                xd = [sb(stp, f"xd{i}", [128, D], F32) for i in range(2)]
                oT = sb(stp, "oT", [128, 8, 128], BF16)
                x1 = sb(stp, "x1", [128, D], F32)
                dtmp = sb(stp, "dtmp", [128, D], F32)
                h2 = sb(stp, "h2", [128, D], F32)
                h2T32 = sb(stp, "h2T32", [128, 8, 128], F32)
                h2Tb = sb(stp, "h2Tb", [128, 8, 128], BF16)
                ssd = sb(stp, "ssd", [128, 1], F32)
                lg = sb(stp, "lg", [128, 36], F32)
                r1 = [sb(stp, f"r1_{i}", [128, 1], F32) for i in range(8)]
                ohg = sb(stp, "ohg", [128, 4], F32)
                eg4 = sb(stp, "eg4", [128, 4], F32)
                t32 = sb(stp, "t32", [128, 32], F32)
                esel = sb(stp, "esel", [128, 8], F32)
                es2 = sb(stp, "es2", [128, 8], F32)
                mk1 = sb(stp, "mk1", [128, 8], F32)
                mk2 = sb(stp, "mk2", [128, 8], F32)
                ge = sb(stp, "ge", [128, 8], F32)
                Gt = sb(stp, "Gt", [128, 32], F32)
                psTb2 = ps(stp, "psTb2", [128, 8, 128], BF16)
                psY2 = ps(stp, "psY2", [128, D])
                psW2 = ps(stp, "psW2", [128, 8, 128])
                psR = ps(stp, "psR", [128, 512])[:, 0:36]

                def load_d(i):
                    fw.dma('sp', om[i % 2][:], omix[i * 128:(i + 1) * 128, :])
                    fw.dma('sp', xd[i % 2][:], x_src[i * 128:(i + 1) * 128, :])
                load_d(0)
                for i in range(NT):
                    if i + 1 < NT:
                        load_d(i + 1)
                    tsl = slice(i * 128, (i + 1) * 128)
                    o_, x_ = om[i % 2], xd[i % 2]
                    fw.tr([(psTb2[:, k, :], o_[:, k * 128:(k + 1) * 128], identb[:]) for k in range(8)])
                    fw.copy(oT[:], psTb2[:], e='act')
                    fw.mm([(psY2[:, 0:512], oT[:, k, :], wo[:, k, 0:512], k == 0, k == 7) for k in range(8)] +
                          [(psY2[:, 512:1024], oT[:, k, :], wo[:, k, 512:1024], k == 0, k == 7) for k in range(8)])
                    fw.tt(dtmp[:], psY2[:], gt1, ALU.mult)
                    fw.tt(x1[:], dtmp[:], x_[:], ALU.add, e='pool')
                    if not os.environ.get('SKIP_XS0'):
                        fw.dma('pool', xs[0][tsl, :], x1[:])
                    fw.act(dtmp[:], x1[:], AF.Square, accum_out=ssd[:])
                    fw.act(ssd[:], ssd[:], AF.Ln, bias=epsT[:], scale=1.0 / D)
                    fw.act(ssd[:], ssd[:], AF.Exp, scale=-0.5)
                    fw.stt(h2[:], x1[:], ssd[:, 0:1], A2[:], ALU.mult, ALU.mult)
                    fw.tt(h2[:], h2[:], sh2, ALU.add, e='pool')
                    fw.tr([(psW2[:, k, :], h2[:, k * 128:(k + 1) * 128], identf[:]) for k in range(8)])
                    fw.copy(h2T32[:], psW2[:])
                    fw.copy(h2Tb[:], psW2[:], e='act')
                    if not os.environ.get('SKIP_H2TD'):
                        fw.dma('pool', h2Td[:, :, tsl], h2Tb[:])
                    if os.environ.get("SKIP_ROUTER"):
                        continue
                    fw.mmk(psR[:], [(h2T32[:, k, :], wr[:, k, :]) for k in range(8)])
                    fw.tt(lg[:], psR[:], br[:], ALU.add)
                    gm, ngm, sume, gtp, m1, m2, dd, w1_ = r1
                    fw.red(gm[:], lg[:, 0:4], op=ALU.max)
                    fw.ts(ohg[:], lg[:, 0:4], gm[:, 0:1], None, ALU.is_equal)
                    fw.ts(ngm[:], gm[:], -1.0, None, ALU.mult)
                    fw.act(eg4[:], lg[:, 0:4], AF.Exp, bias=ngm[:], scale=1.0, accum_out=sume[:])
                    fw.recip(gtp[:], sume[:])
                    fw.tt(t32[:, :].rearrange("p (g e) -> p g e", g=4), lg[:, 4:36].rearrange("p (g e) -> p g e", g=4),
                          bc(ohg[:, :], 2, 8), ALU.mult)
                    fw.red(esel[:], t32[:, :].rearrange("p (g e) -> p e g", g=4))
                    fw.red(m1[:], esel[:], op=ALU.max)
                    fw.ts(mk1[:], esel[:], m1[:, 0:1], None, ALU.is_equal)
                    fw.stt(es2[:], mk1[:], -1e30, esel[:], ALU.mult, ALU.add)
                    fw.red(m2[:], es2[:], op=ALU.max)
                    fw.ts(mk2[:], es2[:], m2[:, 0:1], None, ALU.is_equal)
                    fw.tt(dd[:], m2[:], m1[:], ALU.subtract)
                    fw.act(dd[:], dd[:], AF.Exp)
                    fw.ts(w1_[:], dd[:], 1.0, None, ALU.add)
                    fw.recip(w1_[:], w1_[:])
                    fw.tt(dd[:], dd[:], w1_[:], ALU.mult)
                    fw.tt(w1_[:], w1_[:], gtp[:], ALU.mult)
                    fw.tt(dd[:], dd[:], gtp[:], ALU.mult)
                    fw.ts(ge[:], mk1[:], w1_[:, 0:1], None, ALU.mult)
                    fw.stt(ge[:], mk2[:], dd[:, 0:1], ge[:], ALU.mult, ALU.add)
                    fw.tt(Gt[:, :].rearrange("p (g e) -> p g e", g=4), bc(ohg[:, :], 2, 8), bc(ge[:, :], 1, 4), ALU.mult)
                    fw.dma('pool', Gd[tsl, :], Gt[:])
                fw.barrier()

            if stop_after == 'D':
                break
            SBK = min(S, 2048)
            TPB = SBK // 128
            NB = SBK // 512
            with ExitStack() as stp:
                h2Ts = sb(stp, "h2Ts", [128, 8, SBK], BF16)
                yacc = sb(stp, "yacc", [128, TPB, D], F32)
                Gs = sb(stp, "Gs", [128, TPB, 32], F32)
                w1b = [sb(stp, f"w1b_{i}", [128, 8, 256], BF16) for i in range(2)]
                w3b = [sb(stp, f"w3b_{i}", [128, 8, 256], BF16) for i in range(2)]
                w2b = [sb(stp, f"w2b_{i}", [128, 2, D], BF16) for i in range(2)]
                st1 = sb(stp, "st1", [128, 8, 256], F32)
                st3 = sb(stp, "st3", [128, 8, 256], F32)
                st2 = sb(stp, "st2", [128, 2, D], F32)
                e1_ = [sb(stp, f"e1_{i}", [128, 512], F32) for i in range(2)]
                p_ = [sb(stp, f"p_{i}", [128, 512], F32) for i in range(2)]
                hidT = [sb(stp, f"hidT{i}", [128, 2, 512], BF16) for i in range(2)]
                xe1 = st1[:, 0:4, :].rearrange("p k n -> p (k n)")
                xo1 = st3[:, 0:4, :].rearrange("p k n -> p (k n)")
                psH1 = [ps(stp, f"psH1{i}", [128, 512]) for i in range(2)]
                psH3 = [ps(stp, f"psH3{i}", [128, 512]) for i in range(2)]
                psY = [ps(stp, f"psY{i}", [128, D]) for i in range(2)]
                bcount = 0
                ycount = [0]
                pendE = [None]
                for sbk in range(S // SBK):
                    t0 = sbk * SBK
                    fw.dma('sp', h2Ts[:], h2Td[:, :, t0:t0 + SBK])
                    fw.dma('sp', Gs[:], Gd[t0:t0 + SBK, :].rearrange("(t p) e -> p t e", p=128))
                    for e_ in range(32):
                        wa1, wa3, wb2 = w1b[e_ % 2], w3b[e_ % 2], w2b[e_ % 2]
                        fw.dma('sp', st1[:], moe_w1[l, e_].rearrange("(k p) n -> p k n", p=128))
                        fw.dma('sp', st3[:], moe_w3[l, e_].rearrange("(k p) n -> p k n", p=128))
                        fw.dma('sp', st2[:], moe_w2[l, e_].rearrange("(k p) n -> p k n", p=128))
                        fw.copy(wa1[:], st1[:], e='pool')
                        fw.copy(wa3[:], st3[:], e='pool')
                        fw.copy(wb2[:], st2[:], e='pool')
                        for b in range(NB):
                            hT_ = hidT[bcount % 2]
                            bcount += 1
                            tk = slice(b * 512, (b + 1) * 512)
                            for c in range(2):
                                fs = slice(c * 128, (c + 1) * 128)
                                fw.mm([(psH1[c][:], wa1[:, k, fs], h2Ts[:, k, tk], k == 0, k == 7) for k in range(8)] +
                                      [(psH3[c][:], wa3[:, k, fs], h2Ts[:, k, tk], k == 0, k == 7) for k in range(8)])
                                fw.act(e1_[c][:], psH1[c][:], AF.Exp, scale=-1.0)
                                fw.act(e1_[c][:], e1_[c][:], AF.Ln, bias=oneT[:], scale=1.0)
                                fw.act(e1_[c][:], e1_[c][:], AF.Exp, scale=-1.0)
                                fw.tt(p_[c][:], psH1[c][:], e1_[c][:], ALU.mult)
                                fw.tt(hT_[:, c, :], p_[c][:], psH3[c][:], ALU.mult)
                                if pendE[0] is not None:
                                    pendE[0](c)
                                    if c == 1:
                                        pendE[0] = None

                            def mk(bb=b, hh=hT_, ee=e_, wb=wb2):
                                def f(half):
                                    for t4 in ((0, 1, 2, 3) if half is None else (2 * half, 2 * half + 1)):
                                        t = bb * 4 + t4
                                        py = psY[ycount[0] % 2]
                                        ycount[0] += 1
                                        ts4 = slice(t4 * 128, (t4 + 1) * 128)
                                        fw.mm([(py[:, 0:512], hh[:, j, ts4], wb[:, j, 0:512], j == 0, j == 1) for j in range(2)] +
                                              [(py[:, 512:1024], hh[:, j, ts4], wb[:, j, 512:1024], j == 0, j == 1) for j in range(2)])
                                        if ee == 0:
                                            fw.ts(yacc[:, t, :], py[:], Gs[:, t, ee:ee + 1], None, ALU.mult)
                                        else:
                                            fw.stt(yacc[:, t, :], py[:], Gs[:, t, ee:ee + 1], yacc[:, t, :], ALU.mult, ALU.add)
                                return f
                            pendE[0] = mk()
                    if pendE[0] is not None:
                        pendE[0](None)
                        pendE[0] = None
                    for t in range(TPB):
                        rows = slice(t0 + t * 128, t0 + (t + 1) * 128)
                        fw.dma('sp', xe1, xs[0][rows, :])
                        fw.tt(xo1, yacc[:, t, :], gt2, ALU.mult, e='pool')
                        fw.tt(xo1, xo1, xe1, ALU.add)
                        fw.dma('pool', x_dst[rows, :], xo1)
                fw.barrier()

        if dbg and os.environ.get("DBG_DN"):
            with ExitStack() as stp:
                t_f = sb(stp, "dbgdn_f", [128, D], F32)
                fw.memset(t_f[:], 0.0)
                for i in range(NT):
                    rows = slice(i * 128, (i + 1) * 128)
                    if os.environ.get("DBG_DN") == "U":
                        fw.dma('sp', t_f[:, 0:768], dU[rows, :])
                    else:
                        fw.dma('sp', t_f[:, 0:384], dO[0, rows, :])
                        fw.dma('sp', t_f[:, 384:768], dO[1, rows, :])
                    fw.dma('sp', t_f[:, 768:792], dE[rows, :])
                    fw.dma('sp', t_f[:, 800:812], gT[:, i, :])
                    fw.dma('sp', t_f[:, 812:824], betaT[:, i, :])
                    fw.dma('sp', dbg_out[rows, :], t_f[:])
                fw.barrier()
        elif dbg and stop_after in ('B', 'C'):
            with ExitStack() as stp:
                t_b = sb(stp, "dbg_b", [128, D], BF16)
                t_f = sb(stp, "dbg_f", [128, D], F32)
                for i in range(NT):
                    fw.dma('sp', t_b[:], omix[i * 128:(i + 1) * 128, :],
                           extra_reads=[("omix", qb, col) for qb in range(NQB) for col in range(384, 1024, 64)])
                    fw.copy(t_f[:], t_b[:])
                    fw.dma('sp', dbg_out[i * 128:(i + 1) * 128, :], t_f[:])
                fw.barrier()
        fw.barrier()
        print("instructions emitted:", fw.ninst)
    return nc


def rope_tables(S, rot_dim, grid_w=64):
    t = np.arange(S)
    row = (t // grid_w).astype(np.float32)
    col = (t % grid_w).astype(np.float32)
    n_freq = rot_dim // 4
    inv = (10000.0 ** (-np.arange(n_freq, dtype=np.float32) / n_freq)).astype(np.float32)
    ang = np.concatenate([row[:, None] * inv, col[:, None] * inv], axis=-1).astype(np.float32)
    return np.cos(ang).astype(np.float32), np.sin(ang).astype(np.float32)


def rep(v, n=128):
    v = np.asarray(v, np.float32)
    return np.ascontiguousarray(np.broadcast_to(v[..., None, :], v.shape[:-1] + (n, v.shape[-1])))


def prep_inputs(inp, S, depth):
    NT = S // 128
    f = lambda a: np.ascontiguousarray(np.asarray(a, np.float32))
    perm = np.arange(INC)
    base = 1560
    order = [0, 3, 1, 4, 2, 5]
    perm[base:base + 384] = np.concatenate([base + h * 64 + np.arange(64) for h in order])
    com = {}
    com["ada_w"] = f(inp["ada_w"][:depth])
    com["ada_b"] = rep(inp["ada_b"][:depth])
    com["g1"] = rep(inp["norm1_g"][:depth])
    com["g2"] = rep(inp["norm2_g"][:depth])
    com["w_in"] = f(np.asarray(inp["w_in"])[:depth][:, :, perm])
    com["w_out"] = f(inp["w_out"][:depth])
    com["gq_g"] = rep(inp["gqa_q_g"][:depth])
    com["gk_g"] = rep(inp["gqa_k_g"][:depth])
    com["mql_g"] = f(np.asarray(inp["mla_q_lat_g"])[:depth, :, None])
    com["mkvl_g"] = f(np.asarray(inp["mla_kv_lat_g"])[:depth, :, None])
    com["w_uq"] = f(inp["mla_w_uq"][:depth])
    com["w_ukv"] = f(inp["mla_w_ukv"][:depth])
    com["mqn_g"] = rep(inp["mla_qn_g"][:depth])
    com["mqr_g"] = rep(inp["mla_qr_g"][:depth])
    com["mkn_g"] = rep(inp["mla_kn_g"][:depth])
    com["mkr_g"] = rep(inp["mla_kr_g"][:depth])
    cg, sg = rope_tables(S, 64)
    cm, sm = rope_tables(S, 32)
    tm = lambda a: np.ascontiguousarray(a.reshape(NT, 128, -1).transpose(1, 0, 2))
    com["cosg"], com["sing"], com["cosm"], com["sinm"] = tm(cg), tm(sg), tm(cm), tm(sm)
    com["identf"] = np.eye(128, dtype=np.float32)
    com["w_r"] = f(np.concatenate([np.asarray(inp["moe_w_group"])[:depth], np.asarray(inp["moe_w_router"])[:depth]], axis=-1))
    com["b_r"] = rep(np.concatenate([np.asarray(inp["moe_b_group"])[:depth], np.asarray(inp["moe_b_router"])[:depth]], axis=-1))
    com["moe_w1"] = f(inp["moe_w1"][:depth])
    com["moe_w3"] = f(inp["moe_w3"][:depth])
    com["moe_w2"] = f(inp["moe_w2"][:depth])
    com["wconv"] = rep(np.asarray(inp["dn_conv"])[:depth].reshape(depth, 5 * 1152))
    com["alog"] = rep(np.asarray(inp["dn_a_log"])[:depth].reshape(depth, 12))
    com["dtb"] = rep(np.asarray(inp["dn_dt_bias"])[:depth].reshape(depth, 12))
    com["dng"] = rep(inp["dn_out_g"][:depth])
    ii = np.arange(128)[:, None]
    jj = np.arange(128)[None, :]
    NEG = -30000.0
    mk = np.stack([(ii <= jj), (ii >= jj),
                   np.where(jj <= ii, 0.0, NEG), np.where(jj >= ii, 0.0, NEG),
                   (jj < ii), (jj > ii),
                   (ii // 32 == jj // 32) & (ii != jj), (ii // 64 == jj // 64) & (ii // 32 != jj // 32), (ii // 64 != jj // 64)],
                  axis=1).astype(np.float32)
    mk[:, 7:9, :] *= -1.0
    com["masks"] = np.ascontiguousarray(mk)
    return com


def kernel(**inp):
    S, depth = 4096, 4
    com = prep_inputs(inp, S, depth)
    x = np.asarray(inp["x"], np.float32)
    c = np.asarray(inp["c"], np.float32)
    nc = build(S, depth)
    maps = []
    for core in range(8):
        b = core % 4
        m = dict(com)
        m["x"] = np.ascontiguousarray(x[b])
        m["c_pk"] = np.ascontiguousarray(c[b].reshape(8, 128).T)
        maps.append(m)
    res = run_bass_kernel_spmd(nc, maps, core_ids=list(range(8)))
    return np.stack([res.results[b]["y"] for b in range(4)], axis=0).astype(np.float32)
```

```python
import math
import os
import numpy as np
from contextlib import ExitStack
import concourse.bass as bass
import concourse.mybir as mybir
from concourse.bass_utils import run_bass_kernel_spmd

F32 = mybir.dt.float32
BF16 = mybir.dt.bfloat16
I32 = mybir.dt.int32
AF = mybir.ActivationFunctionType
ALU = mybir.AluOpType
AX = mybir.AxisListType
NDS = 20
import re
PSUM_RE = re.compile(r'p\d+_')

D = 1024
INC = 2552
EPS = 1e-6


class FW:
    def __init__(self, nc, stack):
        self.nc = nc
        self.stack = stack
        self.eng = {'pe': nc.tensor, 'act': nc.scalar, 'dve': nc.vector, 'pool': nc.gpsimd, 'sp': nc.sync}
        self.sem = {}
        self.cnt = {}
        for e in self.eng:
            self.sem[('c', e)] = stack.enter_context(nc.semaphore('s_' + e))
            self.cnt[('c', e)] = 0
        self.dring = {}
        self.dnext = {}
        for q in ('sp', 'act', 'pool'):
            self.dring[q] = []
            for i in range(NDS):
                k = ('d', q, i)
                self.sem[k] = stack.enter_context(nc.semaphore(f'd_{q}_{i}'))
                self.cnt[k] = 0
                self.dring[q].append(k)
            self.dnext[q] = 0
        self.waited = {e: {} for e in self.eng}
        self.W = {}
        self.R = {}
        self.ninst = 0

    @staticmethod
    def _res(x):
        if isinstance(x, tuple):
            if len(x) == 2 and not isinstance(x[0], (str, int)):
                return x[1]
            return x
        if isinstance(x, str):
            return x
        return x.name

    @staticmethod
    def ap(x):
        if isinstance(x, tuple):
            return x[0]
        return x

    def _wait(self, e, toks):
        eng = self.eng[e]
        for k, v in toks.items():
            if self.waited[e].get(k, 0) < v:
                eng.wait_ge(self.sem[k], v)
                self.waited[e][k] = v
                self.ninst += 1

    def _deps(self, reads, writes):
        toks = {}

        def add(d):
            for k, v in d.items():
                if toks.get(k, 0) < v:
                    toks[k] = v
        for r in reads:
            add(self.W.get(r, {}))
        for w in writes:
            add(self.W.get(w, {}))
            add(self.R.get(w, {}))
        return toks

    def _commit(self, tok, reads, writes):
        k, v = tok
        for r in reads:
            d = self.R.setdefault(r, {})
            d[k] = max(d.get(k, 0), v)
        for w in writes:
            self.W[w] = {k: v}
            self.R[w] = {}

    def op(self, e, fn, reads, writes):
        reads = [self._res(r) for r in reads if r is not None and not isinstance(r, (int, float))]
        writes = [self._res(w) for w in writes]
        writes = writes + [r for r in reads if isinstance(r, str) and PSUM_RE.match(r)]
        toks = self._deps(reads, writes)
        if e == 'pe':
            toks.pop(('c', 'pe'), None)
        self._wait(e, toks)
        ins = fn()
        k = ('c', e)
        self.cnt[k] += 1
        ins.then_inc(self.sem[k], 1)
        self.ninst += 1
        self._commit((k, self.cnt[k]), reads, writes)

    def dma(self, q, out, in_, extra_reads=(), extra_writes=()):
        reads = [self._res(in_)] + [self._res(r) for r in extra_reads]
        writes = [self._res(out)] + [self._res(w) for w in extra_writes]
        toks = self._deps(reads, writes)
        k = self.dring[q][self.dnext[q]]
        self.dnext[q] = (self.dnext[q] + 1) % NDS
        if self.cnt[k] > 0:
            toks[k] = max(toks.get(k, 0), self.cnt[k])
        self._wait(q, toks)
        ins = self.eng[q].dma_start(out=self.ap(out), in_=self.ap(in_))
        self.cnt[k] += 16
        ins.then_inc(self.sem[k], 16)
        self.ninst += 1
        self._commit((k, self.cnt[k]), reads, writes)

    def barrier(self, engines=('pe', 'act', 'dve', 'pool', 'sp')):
        toks = {k: v for k, v in self.cnt.items() if v > 0}
        for e in engines:
            self._wait(e, dict(toks))

    def act(self, out, in_, func, bias=None, scale=1.0, accum_out=None, e='act'):
        kw = {}
        if bias is not None:
            kw['bias'] = self.ap(bias)
        if accum_out is not None:
            kw['accum_out'] = self.ap(accum_out)
        sc = self.ap(scale) if not isinstance(scale, (int, float)) else scale
        wr = [out] + ([accum_out] if accum_out is not None else [])
        self.op(e, lambda: self.eng[e].activation(out=self.ap(out), in_=self.ap(in_), func=func, scale=sc, **kw),
                [in_, bias, scale], wr)

    def tt(self, out, in0, in1, op, e='dve'):
        self.op(e, lambda: self.eng[e].tensor_tensor(out=self.ap(out), in0=self.ap(in0), in1=self.ap(in1), op=op),
                [in0, in1], [out])

    def ts(self, out, in0, s1, s2, op0, op1=None, e='dve'):
        a1 = self.ap(s1) if not isinstance(s1, (int, float)) else s1
        a2 = (self.ap(s2) if not isinstance(s2, (int, float)) else s2) if s2 is not None else None
        kw = {}
        if op1 is not None:
            kw['op1'] = op1
        self.op(e, lambda: self.eng[e].tensor_scalar(out=self.ap(out), in0=self.ap(in0), scalar1=a1, scalar2=a2,
                                                     op0=op0, **kw), [in0, s1, s2], [out])

    def stt(self, out, in0, scalar, in1, op0, op1, e='dve'):
        a = self.ap(scalar) if not isinstance(scalar, (int, float)) else scalar
        self.op(e, lambda: self.eng[e].scalar_tensor_tensor(out=self.ap(out), in0=self.ap(in0), scalar=a,
                                                            in1=self.ap(in1), op0=op0, op1=op1),
                [in0, scalar, in1], [out])

    def red(self, out, in_, op=ALU.add, e='dve'):
        self.op(e, lambda: self.eng[e].tensor_reduce(out=self.ap(out), in_=self.ap(in_), axis=AX.X, op=op),
                [in_], [out])

    def recip(self, out, in_):
        self.op('dve', lambda: self.nc.vector.reciprocal(out=self.ap(out), in_=self.ap(in_)), [in_], [out])

    def copy(self, out, in_, e='dve'):
        if e == 'act':
            self.op(e, lambda: self.eng[e].copy(out=self.ap(out), in_=self.ap(in_)), [in_], [out])
        else:
            self.op(e, lambda: self.eng[e].tensor_copy(out=self.ap(out), in_=self.ap(in_)), [in_], [out])

    def memset(self, out, val, e='pool'):
        self.op(e, lambda: self.eng[e].memset(self.ap(out), val), [], [out])

    def mm(self, items, extra_reads=()):
        rd = list(extra_reads)
        wr = []
        for o, l, r, _, _ in items:
            rd += [l, r]
            wr.append(o)

        def fn():
            ins = None
            for o, l, r, st, sp in items:
                ins = self.nc.tensor.matmul(self.ap(o), self.ap(l), self.ap(r), start=st, stop=sp)
            return ins
        self.op('pe', fn, rd, wr)

    def mmk(self, out, pairs):
        n = len(pairs)
        self.mm([(out, l, r, i == 0, i == n - 1) for i, (l, r) in enumerate(pairs)])

    def tr(self, items):
        rd = []
        wr = []
        for o, i, idt in items:
            rd += [i, idt]
            wr.append(o)

        def fn():
            ins = None
            for o, i, idt in items:
                ins = self.nc.tensor.transpose(self.ap(o), self.ap(i), self.ap(idt))
            return ins
        self.op('pe', fn, rd, wr)


def bc(ap, axis, n):
    a = ap.unsqueeze(axis)
    shp = list(a.shape)
    shp[axis] = n
    return a.to_broadcast(shp)


def build(S=4096, depth=4, stop_after=None, dbg=False):
    NT = S // 128
    NQB = S // 512
    nc = bass.Bass("TRN2", target_bir_lowering=False)

    def din(name, shape, dt=F32):
        return nc.dram_tensor(name, list(shape), dt, kind="ExternalInput").ap()

    def scr(name, shape, dt):
        return nc.dram_tensor(name, list(shape), dt, kind="Internal").ap()

    x_in = din("x", [S, D])
    c_pk = din("c_pk", [128, 8])
    ada_w = din("ada_w", [depth, D, 6 * D])
    ada_b = din("ada_b", [depth, 128, 6 * D])
    g1_d = din("g1", [depth, 128, D])
    g2_d = din("g2", [depth, 128, D])
    w_in = din("w_in", [depth, D, INC])
    w_out = din("w_out", [depth, D, D])
    gq_g = din("gq_g", [depth, 128, 64])
    gk_g = din("gk_g", [depth, 128, 64])
    mql_g = din("mql_g", [depth, 192, 1])
    mkvl_g = din("mkvl_g", [depth, 128, 1])
    w_uq = din("w_uq", [depth, 192, 384])
    w_ukv = din("w_ukv", [depth, 128, 512])
    mqn_g = din("mqn_g", [depth, 128, 64])
    mqr_g = din("mqr_g", [depth, 128, 32])
    mkn_g = din("mkn_g", [depth, 128, 64])
    mkr_g = din("mkr_g", [depth, 128, 32])
    cosg_d = din("cosg", [128, NT, 32])
    sing_d = din("sing", [128, NT, 32])
    cosm_d = din("cosm", [128, NT, 16])
    sinm_d = din("sinm", [128, NT, 16])
    identf_d = din("identf", [128, 128])
    w_r = din("w_r", [depth, D, 36])
    b_r = din("b_r", [depth, 128, 36])
    moe_w1 = din("moe_w1", [depth, 32, D, 256])
    moe_w3 = din("moe_w3", [depth, 32, D, 256])
    moe_w2 = din("moe_w2", [depth, 32, 256, D])
    wconv_d = din("wconv", [depth, 128, 5 * 1152])
    alog_d = din("alog", [depth, 128, 12])
    dtb_d = din("dtb", [depth, 128, 12])
    dng_d = din("dng", [depth, 128, 64])
    masks_d = din("masks", [128, 9, 128])
    y_out = nc.dram_tensor("y", [S, D], F32, kind="ExternalOutput").ap()

    qTg = scr("qTg", [3, 128, S], BF16)
    kTg = scr("kTg", [128, S], BF16)
    vg = scr("vg", [S, 130], BF16)
    qTm = scr("qTm", [4, 96, S], BF16)
    kTm = scr("kTm", [4, 96, S], BF16)
    vm = scr("vm", [S, 260], BF16)
    omix = scr("omix", [S, D], BF16)
    xs = [scr("xs0", [S, D], F32), scr("xs1", [S, D], F32), scr("xs2", [S, D], F32)]
    h2Td = scr("h2Td", [128, 8, S], BF16)
    Gd = scr("Gd", [S, 32], F32)
    dnpre = scr("dnpre", [S + 4, 1152], F32)
    dnz = scr("dnz", [S, 384], F32)
    dU = scr("dU", [S, 768], F32)
    dWT = scr("dWT", [NT, 64, 12 * 128], BF16)
    dAT = scr("dAT", [NT, 128, 12 * 128], BF16)
    dKT = scr("dKT", [S, 768], BF16)
    dQT = scr("dQT", [NT, 64, 6 * 128], BF16)
    dE = scr("dE", [S, 24], F32)
    dO = scr("dO", [2, S, 384], F32)
    dbg_out = None
    if dbg:
        dbg_out = nc.dram_tensor("dbg", [S, D], F32, kind="ExternalOutput").ap()

    with ExitStack() as st0:
        fw = FW(nc, st0)

        uid = [0]

        def sb(stk, name, shape, dt):
            uid[0] += 1
            return stk.enter_context(nc.sbuf_tensor(f"s{uid[0]}_{name}", list(shape), dt))

        def ps(stk, name, shape, dt=F32):
            uid[0] += 1
            return stk.enter_context(nc.psum_tensor(f"p{uid[0]}_{name}", list(shape), dt))

        identf = sb(st0, "identf", [128, 128], F32)
        identb = sb(st0, "identb", [128, 128], BF16)
        epsT = sb(st0, "epsT", [128, 1], F32)
        cactB = sb(st0, "cactB", [128, 8, 128], F32)
        mod = sb(st0, "mod", [128, 6 * D], F32)
        A1 = sb(st0, "A1", [128, D], F32)
        A2 = sb(st0, "A2", [128, D], F32)
        fw.dma('sp', identf[:], identf_d)
        fw.memset(epsT[:], EPS)
        oneT = sb(st0, "oneT", [128, 1], F32)
        fw.memset(oneT[:], 1.0)
        onesf = sb(st0, "onesf", [128, 128], F32)
        fw.memset(onesf[:], 1.0)
        masks = sb(st0, "masks", [128, 9, 128], F32)
        fw.dma('sp', masks[:], masks_d)
        gT = sb(st0, "gT", [128, NT, 12], F32)
        betaT = sb(st0, "betaT", [128, NT, 12], F32)
        with ExitStack() as stp:
            zt = sb(stp, "zt", [128, 1152], F32)
            fw.memset(zt[:], 0.0)
            fw.dma('sp', dnpre[0:2, :], zt[0:2, :])
            fw.dma('sp', dnpre[S + 2:S + 4, :], zt[0:2, :])
            fw.barrier()
        fw.copy(identb[:], identf[:], e='dve')
        with ExitStack() as stp:
            ct = sb(stp, "ct", [128, 8], F32)
            ce = sb(stp, "ce", [128, 8], F32)
            fw.dma('sp', ct[:], c_pk)
            fw.act(ce[:], ct[:], AF.Exp, scale=-1.0)
            fw.ts(ce[:], ce[:], 1.0, None, ALU.add)
            fw.recip(ce[:], ce[:])
            fw.tt(ce[:], ce[:], ct[:], ALU.mult)
            fw.copy(cactB[:], bc(ce[:, :], 2, 128))
            fw.barrier()

        for l in range(depth):
            x_src = x_in if l == 0 else xs[1 + (l - 1) % 2]
            x_dst = y_out if l == depth - 1 else xs[1 + l % 2]
            last = (l == depth - 1)
            with ExitStack() as stp:
                psM = [ps(stp, f"psM{i}", [128, 512]) for i in range(2)]
                awt = [sb(stp, f"awt{i}", [128, 8, 512], F32) for i in range(2)]
                abt = sb(stp, "abt", [128, 6 * D], F32)
                g1t = sb(stp, "g1t", [128, D], F32)
                g2t = sb(stp, "g2t", [128, D], F32)
                fw.dma('sp', abt[:], ada_b[l])
                fw.dma('sp', g1t[:], g1_d[l])
                fw.dma('sp', g2t[:], g2_d[l])
                for cb in range(12):
                    a = awt[cb % 2]
                    fw.dma('sp', a[:], ada_w[l][:, cb * 512:(cb + 1) * 512].rearrange("(k p) n -> p k n", p=128))
                    fw.mmk(psM[cb % 2][:], [(cactB[:, k, :], a[:, k, :]) for k in range(8)])
                    fw.tt(mod[:, cb * 512:(cb + 1) * 512], psM[cb % 2][:], abt[:, cb * 512:(cb + 1) * 512], ALU.add)
                fw.stt(A1[:], mod[:, D:2 * D], 1.0, g1t[:], ALU.add, ALU.mult)
                fw.stt(A2[:], mod[:, 4 * D:5 * D], 1.0, g2t[:], ALU.add, ALU.mult)
                fw.barrier()
            sh1 = mod[:, 0:D]
            gt1 = mod[:, 2 * D:3 * D]
            sh2 = mod[:, 3 * D:4 * D]
            gt2 = mod[:, 5 * D:6 * D]

            with ExitStack() as stp:
                win = sb(stp, "win", [128, 8, INC], BF16)
                cosg = sb(stp, "cosg", [128, NT, 32], F32)
                sing = sb(stp, "sing", [128, NT, 32], F32)
                cosm = sb(stp, "cosm", [128, NT, 16], F32)
                sinm = sb(stp, "sinm", [128, NT, 16], F32)
                fw.dma('sp', cosg[:], cosg_d)
                fw.dma('sp', sing[:], sing_d)
                fw.dma('sp', cosm[:], cosm_d)
                fw.dma('sp', sinm[:], sinm_d)
                wst = [sb(stp, f"wst{i}", [128, INC], F32) for i in range(2)]
                for k in range(8):
                    fw.dma('sp', wst[k % 2][:], w_in[l][k * 128:(k + 1) * 128, :])
                    fw.copy(win[:, k, :], wst[k % 2][:], e=('pool' if k % 2 else 'dve'))
                wuq = sb(stp, "wuq", [128, 2, 384], BF16)
                wukv = sb(stp, "wukv", [128, 512], BF16)
                wuqs = sb(stp, "wuqs", [128, 2, 384], F32)
                wukvs = sb(stp, "wukvs", [128, 512], F32)
                gql = sb(stp, "gql", [128, 2], F32)
                gkvl = sb(stp, "gkvl", [128, 1], F32)
                fw.dma('sp', wuqs[:, 0, :], w_uq[l][0:128, :])
                fw.dma('sp', wuqs[0:64, 1, :], w_uq[l][128:192, :])
                fw.dma('sp', wukvs[:], w_ukv[l])
                fw.dma('sp', gql[:, 0:1], mql_g[l][0:128, :])
                fw.dma('sp', gql[0:64, 1:2], mql_g[l][128:192, :])
                fw.dma('sp', gkvl[:], mkvl_g[l])
                fw.ts(wuq[:, 0, :], wuqs[:, 0, :], gql[:, 0:1], None, ALU.mult)
                fw.ts(wuq[0:64, 1, :], wuqs[0:64, 1, :], gql[0:64, 1:2], None, ALU.mult)
                fw.ts(wukv[:], wukvs[:], gkvl[:, 0:1], None, ALU.mult)
                gains = {}
                for nm, dd, w_ in (("gq", gq_g, 64), ("gk", gk_g, 64), ("mqn", mqn_g, 64), ("mqr", mqr_g, 32),
                                   ("mkn", mkn_g, 64), ("mkr", mkr_g, 32)):
                    gains[nm] = sb(stp, "gn_" + nm, [128, w_], F32)
                    fw.dma('sp', gains[nm][:], dd[l])

                xt = [sb(stp, f"xt{i}", [128, D], F32) for i in range(2)]
                junk = sb(stp, "junk", [128, D], F32)
                ss = sb(stp, "ss", [128, 1], F32)
                hh = sb(stp, "hh", [128, D], F32)
                hT = sb(stp, "hT", [128, 8, 128], BF16)
                prs = [sb(stp, f"pr{i}", [128, INC], F32) for i in range(2)]
                tmpa = sb(stp, "tmpa", [128, 512], F32)
                tmpb = sb(stp, "tmpb", [128, 512], F32)
                nrm = sb(stp, "nrm", [128, 512], F32)
                s6 = sb(stp, "s6", [128, 16], F32)
                qb_ = sb(stp, "qb_", [128, 384], BF16)
                kb_ = sb(stp, "kb_", [128, 128], BF16)
                vb_ = sb(stp, "vb_", [128, 2, 65], BF16)
                qTs = sb(stp, "qTs", [128, 3, 128], BF16)
                kTs = sb(stp, "kTs", [128, 128], BF16)
                cqn = sb(stp, "cqn", [128, 192], BF16)
                ckvn = sb(stp, "ckvn", [128, 128], BF16)
                cqT = sb(stp, "cqT", [128, 2, 128], BF16)
                ckvT = sb(stp, "ckvT", [128, 128], BF16)
                qup = sb(stp, "qup", [128, 384], F32)
                kvup = sb(stp, "kvup", [128, 512], F32)
                qm = sb(stp, "qm", [128, 4, 96], BF16)
                km = sb(stp, "km", [128, 4, 96], BF16)
                vmb = sb(stp, "vmb", [128, 4, 65], BF16)
                krr = sb(stp, "krr", [128, 32], F32)
                qTms = sb(stp, "qTms", [96, 4, 128], BF16)
                kTms = sb(stp, "kTms", [96, 4, 128], BF16)
                psW = ps(stp, "psW", [128, 8, 128])
                psP = [ps(stp, f"psP{i}", [128, 512]) for i in range(2)]
                psTb = ps(stp, "psTb", [128, 8, 128], BF16)
                psU = ps(stp, "psU", [128, 512])
                fw.memset(vb_[:], 1.0)
                fw.memset(vmb[:], 1.0)
                negA = sb(stp, "negA", [128, 12], F32)
                dtb = sb(stp, "dtb", [128, 12], F32)
                t12 = [sb(stp, f"t12_{i}", [128, 12], F32) for i in range(3)]
                fw.dma('sp', negA[:], alog_d[l])
                fw.dma('sp', dtb[:], dtb_d[l])
                fw.act(negA[:], negA[:], AF.Exp)
                fw.ts(negA[:], negA[:], -1.0, None, ALU.mult)

                def headnorm(src, H, Dh, gain, out, mean):
                    t = tmpa[:, 0:H * Dh].rearrange("p (h d) -> p h d", h=H)
                    fw.tt(t, src, src, ALU.mult)
                    fw.red(s6[:, 0:H], t)
                    fw.act(s6[:, 0:H], s6[:, 0:H], AF.Ln, bias=epsT[:], scale=(1.0 / Dh if mean else 1.0))
                    fw.act(s6[:, 0:H], s6[:, 0:H], AF.Exp, scale=-0.5)
                    fw.tt(t, src, bc(s6[:, 0:H], 2, Dh), ALU.mult)
                    if gain is not None:
                        fw.tt(out, t, bc(gain[:, :], 1, H), ALU.mult)
                    else:
                        fw.copy(out, t)

                def rope(src, H, R, cs, sn, out):
                    h2 = R // 2
                    x1 = src[:, :, 0:h2]
                    x2 = src[:, :, h2:R]
                    c_ = bc(cs, 1, H)
                    s_ = bc(sn, 1, H)
                    ta = tmpa[:, 0:H * h2].rearrange("p (h d) -> p h d", h=H)
                    tb = tmpb[:, 0:H * h2].rearrange("p (h d) -> p h d", h=H)
                    fw.tt(ta, x1, c_, ALU.mult)
                    fw.tt(tb, x2, s_, ALU.mult)
                    fw.tt(out[:, :, 0:h2], ta, tb, ALU.subtract)
                    fw.tt(ta, x1, s_, ALU.mult)
                    fw.tt(tb, x2, c_, ALU.mult)
                    fw.tt(out[:, :, h2:R], ta, tb, ALU.add)

                fw.dma('sp', xt[0][:], x_src[0:128, :])

                def frontA(i):
                    xc = xt[i % 2]
                    pr = prs[i % 2]
                    if i + 1 < NT:
                        fw.dma('sp', xt[(i + 1) % 2][:], x_src[(i + 1) * 128:(i + 2) * 128, :])
                    fw.act(junk[:], xc[:], AF.Square, accum_out=ss[:])
                    fw.act(ss[:], ss[:], AF.Ln, bias=epsT[:], scale=1.0 / D)
                    fw.act(ss[:], ss[:], AF.Exp, scale=-0.5)
                    fw.stt(hh[:], xc[:], ss[:, 0:1], A1[:], ALU.mult, ALU.mult)
                    fw.tt(hh[:], hh[:], sh1, ALU.add)
                    fw.tr([(psW[:, k, :], hh[:, k * 128:(k + 1) * 128], identf[:]) for k in range(8)])
                    fw.copy(hT[:], psW[:], e='act')
                    for cb in range(5):
                        c0 = cb * 512
                        c1 = min(INC, c0 + 512)
                        pp = psP[cb % 2]
                        fw.mmk(pp[:, 0:c1 - c0], [(hT[:, k, :], win[:, k, c0:c1]) for k in range(8)])
                        fw.copy(pr[:, c0:c1], pp[:, 0:c1 - c0], e=('dve' if cb % 2 == 0 else 'act'))

                def backA(i):
                    pr = prs[i % 2]
                    tsl = slice(i * 128, (i + 1) * 128)
                    fw.dma('pool', (dnpre[2 + i * 128:2 + (i + 1) * 128, :], ("dnpre", i)), pr[:, 0:1152])
                    fw.dma('pool', (dnz[tsl, :], ("dnz", i)), pr[:, 1152:1536])
                    fw.act(t12[0][:], pr[:, 1536:1548], AF.Exp, scale=-1.0)
                    fw.ts(t12[0][:], t12[0][:], 1.0, None, ALU.add)
                    fw.recip(betaT[:, i, :], t12[0][:])
                    fw.tt(t12[1][:], pr[:, 1548:1560], dtb[:], ALU.add)
                    fw.ts(t12[2][:], t12[1][:], -1.0, None, ALU.mult)
                    fw.tt(t12[2][:], t12[2][:], t12[1][:], ALU.max)
                    fw.act(t12[2][:], t12[2][:], AF.Exp, scale=-1.0)
                    fw.act(t12[2][:], t12[2][:], AF.Ln, bias=oneT[:], scale=1.0)
                    fw.ts(t12[1][:], t12[1][:], 0.0, None, ALU.max)
                    fw.tt(t12[1][:], t12[1][:], t12[2][:], ALU.add)
                    fw.tt(gT[:, i, :], t12[1][:], negA[:], ALU.mult)
                    qv = pr[:, 1560:1944].rearrange("p (h d) -> p h d", h=6)
                    nq = nrm[:, 0:384].rearrange("p (h d) -> p h d", h=6)
                    headnorm(qv, 6, 64, gains["gq"], nq, True)
                    rope(nq, 6, 64, cosg[:, i, :], sing[:, i, :], qb_[:, :].rearrange("p (h d) -> p h d", h=6))
                    fw.tr([(psTb[:, j, :], qb_[:, j * 128:(j + 1) * 128], identb[:]) for j in range(3)])
                    fw.copy(qTs[:], psTb[:, 0:3, :])
                    fw.dma('pool', (qTg[:, :, tsl].rearrange("j p t -> p j t"), ("qTg", i)), qTs[:])
                    kv_ = pr[:, 1944:2072].rearrange("p (h d) -> p h d", h=2)
                    nk = nrm[:, 0:128].rearrange("p (h d) -> p h d", h=2)
                    headnorm(kv_, 2, 64, gains["gk"], nk, True)
                    rope(nk, 2, 64, cosg[:, i, :], sing[:, i, :], kb_[:, :].rearrange("p (h d) -> p h d", h=2))
                    fw.tr([(psTb[:, 3, :], kb_[:], identb[:])])
                    fw.copy(kTs[:], psTb[:, 3, :])
                    fw.dma('pool', (kTg[:, tsl], ("kTg", i)), kTs[:])
                    fw.copy(vb_[:, :, 0:64], pr[:, 2072:2200].rearrange("p (h d) -> p h d", h=2), e='pool')
                    fw.dma('pool', (vg[tsl, :], ("vg", i)), vb_[:, :, :].rearrange("p h d -> p (h d)"))
                    cq = pr[:, 2200:2392].rearrange("p (h d) -> p h d", h=1)
                    headnorm(cq, 1, 192, None, cqn[:, :].rearrange("p (h d) -> p h d", h=1), True)
                    ckv = pr[:, 2392:2520].rearrange("p (h d) -> p h d", h=1)
                    headnorm(ckv, 1, 128, None, ckvn[:, :].rearrange("p (h d) -> p h d", h=1), True)
                    fw.tr([(psTb[:, 4, :], cqn[:, 0:128], identb[:]),
                           (psTb[0:64, 5, :], cqn[:, 128:192], identb[:]),
                           (psTb[:, 6, :], ckvn[:], identb[:])])
                    fw.copy(cqT[:, 0, :], psTb[:, 4, :])
                    fw.copy(cqT[0:64, 1, :], psTb[0:64, 5, :])
                    fw.copy(ckvT[:], psTb[:, 6, :])
                    fw.mm([(psU[:, 0:384], cqT[:, 0, :], wuq[:, 0, :], True, False),
                           (psU[:, 0:384], cqT[0:64, 1, :], wuq[0:64, 1, :], False, True)])
                    fw.copy(qup[:], psU[:, 0:384])
                    fw.mm([(psU[:], ckvT[:], wukv[:], True, True)])
                    fw.copy(kvup[:], psU[:])
                    qu = qup[:, :].rearrange("p (h d) -> p h d", h=4)
                    qmv = qm[:, :, :]
                    n4 = nrm[:, 0:256].rearrange("p (h d) -> p h d", h=4)
                    headnorm(qu[:, :, 0:64], 4, 64, gains["mqn"], qmv[:, :, 0:64], True)
                    n4r = nrm[:, 256:384].rearrange("p (h d) -> p h d", h=4)
                    headnorm(qu[:, :, 64:96], 4, 32, gains["mqr"], n4r, True)
                    rope(n4r, 4, 32, cosm[:, i, :], sinm[:, i, :], qmv[:, :, 64:96])
                    ku = kvup[:, :].rearrange("p (h d) -> p h d", h=4)
                    headnorm(ku[:, :, 0:64], 4, 64, gains["mkn"], km[:, :, 0:64], True)
                    krv = pr[:, 2520:2552].rearrange("p (h d) -> p h d", h=1)
                    n1r = nrm[:, 384:416].rearrange("p (h d) -> p h d", h=1)
                    headnorm(krv, 1, 32, gains["mkr"], n1r, True)
                    rope(n1r, 1, 32, cosm[:, i, :], sinm[:, i, :], krr[:, :].rearrange("p (h d) -> p h d", h=1))
                    fw.copy(km[:, :, 64:96], bc(krr[:, :], 1, 4), e='pool')
                    fw.copy(vmb[:, :, 0:64], ku[:, :, 64:128], e='pool')
                    fw.dma('pool', (vm[tsl, :], ("vm", i)), vmb[:, :, :].rearrange("p h d -> p (h d)"))
                    fw.tr([(psTb[0:96, hh_, :], qm[:, hh_, :], identb[:]) for hh_ in range(4)])
                    fw.copy(qTms[:], psTb[0:96, 0:4, :])
                    fw.dma('pool', (qTm[:, :, tsl].rearrange("h p t -> p h t"), ("qTm", i)), qTms[:])
                    fw.tr([(psTb[0:96, 4 + hh_, :], km[:, hh_, :], identb[:]) for hh_ in range(4)])
                    fw.copy(kTms[:], psTb[0:96, 4:8, :])
                    fw.dma('pool', (kTm[:, :, tsl].rearrange("h p t -> p h t"), ("kTm", i)), kTms[:])
                frontA(0)
                for i in range(NT):
                    if i + 1 < NT:
                        frontA(i + 1)
                    backA(i)
                fw.barrier()
            if stop_after == 'A':
                break

            with ExitStack() as stp:
                kT_all = sb(stp, "kT_all", [128, S], BF16)
                v_all = sb(stp, "v_all", [128, NT, 130], BF16)
                vm_all = sb(stp, "vm_all", [128, NT, 260], BF16)
                kTm_h = [sb(stp, f"kTm_h{i}", [96, S], BF16) for i in range(2)]
                qTt = [sb(stp, f"qTt{i}", [128, 512], BF16) for i in range(2)]
                pT = [sb(stp, f"pT{i}", [128, 2, 512], BF16) for i in range(2)]
                oTs = sb(stp, "oTs", [65, 512], F32)
                rcp = sb(stp, "rcp", [128, 4], F32)
                ob = [sb(stp, f"ob{i}", [128, 4, 64], BF16) for i in range(2)]
                psS = [ps(stp, f"psS{i}", [128, 2, 512]) for i in range(2)]
                acc = [ps(stp, f"acc{i}", [65, 512]) for i in range(2)]
                psO = ps(stp, "psO", [128, 4, 128])
                allq = [("qTg", i) for i in range(NT)]
                fw.dma('sp', kT_all[:], kTg, extra_reads=[("kTg", i) for i in range(NT)])
                fw.dma('sp', v_all[:], vg.rearrange("(t p) c -> p t c", p=128), extra_reads=[("vg", i) for i in range(NT)])
                fw.dma('sp', vm_all[:], vm.rearrange("(t p) c -> p t c", p=128), extra_reads=[("vm", i) for i in range(NT)])
                ucount = [0]

                pend = [None]

                def unit(kT_ap, qT_ap, vfn, scale, qb, col):
                    u = ucount[0]
                    ucount[0] += 1
                    ac = acc[u % 2]
                    NP = NT // 2

                    def Smm(kp):
                        pss = psS[kp % 2]
                        fw.mm([(pss[:, 0, :], kT_ap[:, (2 * kp) * 128:(2 * kp + 1) * 128], qT_ap, True, True),
                               (pss[:, 1, :], kT_ap[:, (2 * kp + 1) * 128:(2 * kp + 2) * 128], qT_ap, True, True)])
                    Smm(0)
                    if pend[0] is not None:
                        pend[0]()
                        pend[0] = None
                    for kp in range(NP):
                        if kp + 1 < NP:
                            Smm(kp + 1)
                        pt = pT[kp % 2]
                        fw.act(pt[:], psS[kp % 2][:], AF.Exp, scale=scale)
                        fw.mm([(ac[:], vfn(2 * kp), pt[:, 0, :], kp == 0, False),
                               (ac[:], vfn(2 * kp + 1), pt[:, 1, :], False, kp == NP - 1)])

                    def tail():
                        fw.copy(oTs[:], ac[:], e='dve')
                        fw.tr([(psO[:, s_, 0:65], oTs[:, s_ * 128:(s_ + 1) * 128], identf[0:65, 0:65]) for s_ in range(4)])
                        fw.recip(rcp[:], psO[:, :, 64])
                        o_ = ob[u % 2]
                        fw.tt(o_[:], psO[:, :, 0:64], bc(rcp[:, :], 2, 64), ALU.mult)
                        fw.dma('pool', (omix[qb * 512:(qb + 1) * 512, col:col + 64].rearrange("(s p) d -> p s d", p=128),
                                        ("omix", qb, col)), o_[:])
                    pend[0] = tail

                qcnt = 0
                for qb in range(NQB):
                    for j in range(3):
                        qt = qTt[qcnt % 2]
                        qcnt += 1
                        fw.dma('sp', qt[:], qTg[j, :, qb * 512:(qb + 1) * 512], extra_reads=allq)
                        for half in range(2):
                            head = j + 3 * half
                            rs = slice(half * 64, (half + 1) * 64)
                            unit(kT_all[rs, :], qt[rs, :],
                                 (lambda kt, half=half: v_all[:, kt, half * 65:(half + 1) * 65]),
                                 0.125, qb, 384 + head * 64)
                allqm = [("qTm", i) for i in range(NT)]
                for h in range(4):
                    kh = kTm_h[h % 2]
                    fw.dma('sp', kh[:], kTm[h], extra_reads=[("kTm", i) for i in range(NT)])
                    for qb in range(NQB):
                        qt = qTt[qcnt % 2]
                        qcnt += 1
                        fw.dma('sp', qt[0:96, :], qTm[h, :, qb * 512:(qb + 1) * 512], extra_reads=allqm)
                        unit(kh[:, :], qt[0:96, :], (lambda kt, h=h: vm_all[:, kt, h * 65:(h + 1) * 65]),
                             96 ** -0.5, qb, 768 + h * 64)
                if pend[0] is not None:
                    pend[0]()
                    pend[0] = None
                fw.barrier()
            if stop_after == 'B':
                break
            with ExitStack() as stp:
                wcv = sb(stp, "wcv", [128, 5, 1152], F32)
                fw.dma('sp', wcv[:], wconv_d[l].rearrange("p (j c) -> p j c", j=5))
                shf1 = [sb(stp, f"shf_{j}", [128, 1152], F32) for j in range(5)]
                shf = [shf1, shf1]
                cacc = sb(stp, "cacc", [128, 1152], F32)
                ctmp = sb(stp, "ctmp", [128, 1152], F32)
                qkvs = [sb(stp, f"qkv{i}", [128, 1152], F32) for i in range(2)]
                s12 = sb(stp, "s12", [128, 12], F32)
                qnb = sb(stp, "qnb", [128, 384], BF16)
                knb = sb(stp, "knb", [128, 384], BF16)
                kn32s = [sb(stp, f"kn32{i}", [128, 384], F32) for i in range(2)]
                qkTs = [sb(stp, f"qkT{i}", [64, 12, 128], BF16) for i in range(2)]
                gcss = [sb(stp, f"gcs{i}", [128, 24], F32) for i in range(2)]
                exs = [sb(stp, f"ex{i}", [128, 36], F32) for i in range(2)]
                Xd = sb(stp, "Xd", [128, 12, 128], F32)
                dec = sb(stp, "dec", [128, 12, 128], F32)
                Lt = sb(stp, "Lt", [128, 12, 128], F32)
                Pb = [sb(stp, f"Pb{i}", [128, 12, 128], F32) for i in range(2)]
                Qb = [sb(stp, f"Qb{i}", [128, 12, 128], F32) for i in range(2)]
                aqk = sb(stp, "aqk", [128, 12, 128], BF16)
                aqkT = sb(stp, "aqkT", [128, 12, 128], BF16)
                X32 = sb(stp, "X32", [128, 12, 128], F32)
                Wb = sb(stp, "Wb", [128, 12, 64], BF16)
                wT = sb(stp, "wT", [64, 12, 128], BF16)
                ktl = sb(stp, "ktl", [128, 12, 64], BF16)
                coef = sb(stp, "coef", [128, 12], F32)
                psA3 = ps(stp, "psA3", [128, 12, 128])
                psB3 = ps(stp, "psB3", [128, 12, 128])
                psT12f = ps(stp, "psT12", [128, 16, 128], BF16)
                psT12 = psT12f[:, 0:12, :]
                UTm, LTm = masks[:, 0, :], masks[:, 1, :]
                negm = masks[:, 2:4, :]
                strm = masks[:, 4:6, :]
                m0, m1s, m2s = masks[:, 6, :], masks[:, 7, :], masks[:, 8, :]
                bfn = {}
                for nm_ in ('L1Tb', 'L2Tb', 'T32b', 'T32Tb', 'Wz', 'T64b', 'T64Tb', 'Rb', 'Yb'):
                    bfn[nm_] = sb(stp, nm_, [128, 12, 128], BF16)

                def v4(t, w=128):
                    return t[:, :, :].rearrange("p (a h) d -> p a h d", a=2)

                def load_shift(i):
                    for j in range(5):
                        fw.dma('sp', shf[i % 2][j][:], dnpre[i * 128 + j:i * 128 + j + 128, :])
                load_shift(0)

                def frontC(i):
                    qkv, kn32, qkT, gcs, ex = qkvs[i % 2], kn32s[i % 2], qkTs[i % 2], gcss[i % 2], exs[i % 2]
                    sh = shf[i % 2]
                    tsl = slice(i * 128, (i + 1) * 128)
                    fw.tt(cacc[:], sh[0][:], wcv[:, 0, :], ALU.mult, e='pool')
                    for j in range(1, 5):
                        fw.tt(ctmp[:], sh[j][:], wcv[:, j, :], ALU.mult, e='pool')
                        fw.tt(cacc[:], cacc[:], ctmp[:], ALU.add)
                    if i + 1 < NT:
                        load_shift(i + 1)
                    fw.act(ctmp[:], cacc[:], AF.Exp, scale=-1.0)
                    fw.act(ctmp[:], ctmp[:], AF.Ln, bias=oneT[:], scale=1.0)
                    fw.act(ctmp[:], ctmp[:], AF.Exp, scale=-1.0)
                    fw.tt(qkv[:], cacc[:], ctmp[:], ALU.mult)
                    qk12 = qkv[:, 0:768].rearrange("p (h d) -> p h d", h=12)
                    c12 = cacc[:, 0:768].rearrange("p (h d) -> p h d", h=12)
                    fw.tt(c12, qk12, qk12, ALU.mult, e='pool')
                    fw.red(s12[:], c12)
                    fw.act(s12[:], s12[:], AF.Ln, bias=epsT[:], scale=1.0)
                    fw.act(s12[:], s12[:], AF.Exp, scale=-0.5)
                    fw.ts(s12[:, 0:6], s12[:, 0:6], 0.125, None, ALU.mult)
                    fw.tt(qnb[:, :].rearrange("p (h d) -> p h d", h=6), qk12[:, 0:6, :], bc(s12[:, 0:6], 2, 64), ALU.mult)
                    fw.tt(kn32[:, :].rearrange("p (h d) -> p h d", h=6), qk12[:, 6:12, :], bc(s12[:, 6:12], 2, 64), ALU.mult)
                    fw.copy(knb[:], kn32[:], e='pool')
                    fw.tr([(psT12[0:64, h, :], qnb[:, h * 64:(h + 1) * 64], identb[:]) for h in range(6)] +
                          [(psT12[0:64, 6 + h, :], knb[:, h * 64:(h + 1) * 64], identb[:]) for h in range(6)])
                    fw.copy(qkT[:], psT12[0:64, :, :], e='act')
                    fw.dma('pool', dQT[i].rearrange("p (h t) -> p h t", h=6), qkT[:, 0:6, :])
                    g_ = gT[:, i, :]
                    psG = psB3[:, 11, 0:24]
                    fw.mm([(psB3[:, 11, 0:6], UTm, g_[:, 0:6], True, True),
                           (psB3[:, 11, 6:12], LTm, g_[:, 6:12], True, True),
                           (psB3[:, 11, 12:24], onesf[:], g_, True, True)])
                    fw.copy(gcs[:], psG)
                    fw.act(ex[:, 0:24], gcs[:], AF.Exp)
                    fw.tt(ex[:, 24:36], gcs[:, 12:24], gcs[:, 0:12], ALU.subtract)
                    fw.act(ex[:, 24:36], ex[:, 24:36], AF.Exp)
                    fw.dma('pool', dE[tsl, :], ex[:, 0:24])

                def backC(i):
                    qkv, kn32, qkT, gcs, ex = qkvs[i % 2], kn32s[i % 2], qkTs[i % 2], gcss[i % 2], exs[i % 2]
                    tsl = slice(i * 128, (i + 1) * 128)
                    fw.tt(Xd[:], bc(identf[:, :], 1, 12), bc(gcs[:, 0:12], 2, 128), ALU.mult)
                    Xf = Xd[:, :, :].rearrange("p u j -> p (u j)")
                    Af = psA3[:, :, :].rearrange("p u j -> p (u j)")
                    fw.mm([(Af[:, c * 512:(c + 1) * 512], onesf[:], Xf[:, c * 512:(c + 1) * 512], True, True) for c in range(3)])
                    fw.tt(dec[:], bc(gcs[:, 0:12], 2, 128), psA3[:], ALU.subtract)
                    fw.tt(v4(dec), v4(dec), bc(negm, 2, 6), ALU.add)
                    fw.act(dec[:], dec[:], AF.Exp)
                    fw.mm([(psB3[:, h, :], qkT[:, 6 + h, :], qkT[:, 6 + h, :], True, True) for h in range(6)] +
                          [(psB3[:, 6 + h, :], qkT[:, h, :], qkT[:, 6 + h, :], True, True) for h in range(6)])
                    fw.tt(v4(Lt), v4(dec), bc(psB3[:, 0:6, :], 1, 2), ALU.mult)
                    fw.tt(v4(aqk), v4(dec), bc(psB3[:, 6:12, :], 1, 2), ALU.mult)
                    fw.stt(Xd[:], Lt[:], -1.0, bc(betaT[:, i, :], 2, 128), ALU.mult, ALU.mult)
                    fw.tr([(psB3[:, u, :], Xd[:, u, :], identf[:]) for u in range(12)])
                    fw.tt(Pb[0][:], Xd[:], bc(m0, 1, 12), ALU.mult, e='pool')
                    fw.tt(Qb[0][:], psB3[:], bc(m0, 1, 12), ALU.mult)
                    fw.tt(bfn['L1Tb'][:], psB3[:], bc(m1s, 1, 12), ALU.mult)
                    fw.tt(bfn['L2Tb'][:], psB3[:], bc(m2s, 1, 12), ALU.mult)
                    fw.tr([(psT12[:, u, :], aqk[:, u, :], identb[:]) for u in range(12)])
                    fw.copy(aqkT[:], psT12, e='act')
                    fw.dma('pool', dAT[i].rearrange("p (u t) -> p u t", u=12), aqkT[:])
                    fw.tt(coef[:], betaT[:, i, :], ex[:, 0:12], ALU.mult)
                    X4 = v4(bfn['Rb'])
                    v6 = qkv[:, 768:1152].rearrange("p (h d) -> p h d", h=6)
                    k6 = kn32[:, :].rearrange("p (h d) -> p h d", h=6)
                    b4 = betaT[:, i, :].rearrange("p (a h) -> p a h", a=2)
                    c4 = coef[:, :].rearrange("p (a h) -> p a h", a=2)
                    e4 = ex[:, 24:36].rearrange("p (a h) -> p a h", a=2)
                    fw.tt(X4[:, :, :, 0:64], bc(v6, 1, 2), bc(b4, 3, 64), ALU.mult)
                    fw.tt(X4[:, :, :, 64:128], bc(k6, 1, 2), bc(c4, 3, 64), ALU.mult, e='pool')
                    fw.tt(ktl[:, :, :].rearrange("p (a h) d -> p a h d", a=2), bc(k6, 1, 2), bc(e4, 3, 64), ALU.mult, e='pool')
                    fw.dma('pool', dKT[tsl, :], ktl[:, :, :].rearrange("p u d -> p (u d)"))
                    fw.tt(Lt[:], Pb[0][:], bc(identf[:, :], 1, 12), ALU.add)
                    for k in range(4):
                        P_, Q_ = Pb[k % 2], Qb[k % 2]
                        Pn, Qn = Pb[(k + 1) % 2], Qb[(k + 1) % 2]
                        fw.mm([(psA3[:, u, :], P_[:, u, :], Q_[:, u, :], True, True) for u in range(12)])
                        fw.copy(Qn[:], psA3[:], e='act')
                        if k < 3:
                            fw.mm([(psB3[:, u, :], Q_[:, u, :], P_[:, u, :], True, True) for u in range(12)])
                            fw.copy(Pn[:], psB3[:], e='act')
                        fw.mm([(psA3[:, u, :], Qn[:, u, :], Lt[:, u, :], True, True) for u in range(12)])
                        if k < 3:
                            fw.tt(Lt[:], Lt[:], psA3[:], ALU.add)
                        else:
                            fw.tt(bfn['T32b'][:], Lt[:], psA3[:], ALU.add)
                    fw.tr([(psT12[:, u, :], bfn['T32b'][:, u, :], identb[:]) for u in range(12)])
                    fw.copy(bfn['T32Tb'][:], psT12, e='act')
                    fw.mm([(psA3[:, u, :], bfn['L1Tb'][:, u, :], bfn['T32b'][:, u, :], True, True) for u in range(12)])
                    fw.copy(bfn['Wz'][:], psA3[:], e='act')
                    fw.mm([(psB3[:, u, :], bfn['T32Tb'][:, u, :], bfn['Wz'][:, u, :], True, True) for u in range(12)])
                    fw.tt(bfn['T64b'][:], bfn['T32b'][:], psB3[:], ALU.subtract)
                    fw.tr([(psT12[:, u, :], bfn['T64b'][:, u, :], identb[:]) for u in range(12)])
                    fw.copy(bfn['T64Tb'][:], psT12, e='act')
                    fw.mm([(psA3[:, u, :], bfn['T64Tb'][:, u, :], bfn['Rb'][:, u, :], True, True) for u in range(12)])
                    fw.copy(dec[:], psA3[:], e='act')
                    fw.copy(bfn['Yb'][:], dec[:], e='act')
                    fw.mm([(psB3[:, u, :], bfn['L2Tb'][:, u, :], bfn['Yb'][:, u, :], True, True) for u in range(12)])
                    fw.copy(bfn['Wz'][:], psB3[:], e='act')
                    fw.mm([(psA3[:, u, :], bfn['T64Tb'][:, u, :], bfn['Wz'][:, u, :], True, True) for u in range(12)])
                    fw.tt(X32[:], dec[:], psA3[:], ALU.subtract)
                    fw.dma('pool', dU[tsl, :].rearrange("p (u d) -> p u d", u=12), X32[:, :, 0:64])
                    fw.copy(Wb[:], X32[:, :, 64:128], e='act')
                    fw.tr([(psT12[0:64, u, :], Wb[:, u, :], identb[:]) for u in range(12)])
                    fw.copy(wT[:], psT12[0:64, :, :], e='act')
                    fw.dma('pool', dWT[i].rearrange("p (u t) -> p u t", u=12), wT[:])
                frontC(0)
                for i in range(NT):
                    if i + 1 < NT:
                        frontC(i + 1)
                    backC(i)
                fw.barrier()

            with ExitStack() as stp:
                S32 = [sb(stp, f"S32_{d}", [64, 6, 64], F32) for d in range(2)]
                Sb = [sb(stp, f"Sb_{d}", [64, 6, 64], BF16) for d in range(2)]
                for d in range(2):
                    fw.memset(S32[d][:], 0.0)
                    fw.memset(Sb[d][:], 0.0)
                Ud = [[sb(stp, f"Ud{d}{b}", [128, 6, 64], F32) for b in range(2)] for d in range(2)]
                WTd = [[sb(stp, f"WTd{d}{b}", [64, 6, 128], BF16) for b in range(2)] for d in range(2)]
                ATd = [[sb(stp, f"ATd{d}{b}", [128, 6, 128], BF16) for b in range(2)] for d in range(2)]
                KTd = [[sb(stp, f"KTd{d}{b}", [128, 6, 64], BF16) for b in range(2)] for d in range(2)]
                QTd = [[sb(stp, f"QTd{d}{b}", [64, 6, 128], BF16) for b in range(2)] for d in range(2)]
                Ed = [[sb(stp, f"Ed{d}{b}", [128, 24], F32) for b in range(2)] for d in range(2)]
                vnew = [sb(stp, f"vnew{d}", [128, 6, 64], BF16) for d in range(2)]
                ot = [sb(stp, f"ot{d}", [128, 6, 64], F32) for d in range(2)]
                od = [sb(stp, f"od{d}", [128, 6, 64], F32) for d in range(2)]
                stmp = [sb(stp, f"stmp{d}", [64, 6, 64], F32) for d in range(2)]
                psV = [ps(stp, f"psV{d}", [128, 8, 64])[:, 0:6, :] for d in range(2)]
                psO1 = [ps(stp, f"psO1{d}", [128, 8, 64])[:, 0:6, :] for d in range(2)]
                psO2 = [ps(stp, f"psO2{d}", [128, 8, 64])[:, 0:6, :] for d in range(2)]
                psS = [ps(stp, f"psS{d}", [64, 8, 64])[:, 0:6, :] for d in range(2)]

                def load_scan(s_):
                    for d in range(2):
                        n = s_ if d == 0 else NT - 1 - s_
                        b = s_ % 2
                        rows = slice(n * 128, (n + 1) * 128)
                        fw.dma('sp', Ud[d][b][:], dU[rows, d * 384:(d + 1) * 384].rearrange("p (h v) -> p h v", h=6))
                        fw.dma('sp', WTd[d][b][:], dWT[n][:, d * 768:(d + 1) * 768].rearrange("p (h t) -> p h t", h=6))
                        fw.dma('sp', ATd[d][b][:], dAT[n][:, d * 768:(d + 1) * 768].rearrange("p (h t) -> p h t", h=6))
                        fw.dma('sp', KTd[d][b][:], dKT[rows, d * 384:(d + 1) * 384].rearrange("p (h v) -> p h v", h=6))
                        fw.dma('sp', QTd[d][b][:], dQT[n].rearrange("p (h t) -> p h t", h=6))
                        fw.dma('sp', Ed[d][b][:], dE[rows, :])
                load_scan(0)
                for s_ in range(NT):
                    if s_ + 1 < NT:
                        load_scan(s_ + 1)
                    b = s_ % 2
                    ns = [s_, NT - 1 - s_]
                    for d in range(2):
                        fw.tt(stmp[d][:], S32[d][:], bc(Ed[d][b][0:64, 12 + d * 6:18 + d * 6], 2, 64), ALU.mult)
                    for d in range(2):
                        fw.mm([(psV[d][:, h, :], WTd[d][b][:, h, :], Sb[d][:, h, :], True, True) for h in range(6)] +
                              [(psO1[d][:, h, :], QTd[d][b][:, h, :], Sb[d][:, h, :], True, True) for h in range(6)])
                    for d in range(2):
                        fw.tt(vnew[d][:], Ud[d][b][:], psV[d][:], ALU.subtract)
                    for d in range(2):
                        fw.mm([(psS[d][:, h, :], KTd[d][b][:, h, :], vnew[d][:, h, :], True, True) for h in range(6)] +
                              [(psO2[d][:, h, :], ATd[d][b][:, h, :], vnew[d][:, h, :], True, True) for h in range(6)])
                    for d in range(2):
                        fw.tt(S32[d][:], stmp[d][:], psS[d][:], ALU.add)
                        fw.copy(Sb[d][:], S32[d][:], e='act')
                    for d in range(2):
                        fw.tt(ot[d][:], psO1[d][:], bc(Ed[d][b][:, d * 6:d * 6 + 6], 2, 64), ALU.mult)
                        fw.tt(od[d][:], ot[d][:], psO2[d][:], ALU.add)
                        fw.dma('pool', dO[d, ns[d] * 128:(ns[d] + 1) * 128, :], od[d][:, :, :].rearrange("p h v -> p (h v)"))
                fw.barrier()

            with ExitStack() as stp:
                gdn = sb(stp, "gdn", [128, 64], F32)
                fw.dma('sp', gdn[:], dng_d[l])
                o0 = [sb(stp, f"o0_{b}", [128, 384], F32) for b in range(2)]
                o1 = [sb(stp, f"o1_{b}", [128, 384], F32) for b in range(2)]
                zt_ = [sb(stp, f"zt_{b}", [128, 384], F32) for b in range(2)]
                osum = sb(stp, "osum", [128, 384], F32)
                otmp = sb(stp, "otmp", [128, 384], F32)
                ze = sb(stp, "ze", [128, 384], F32)
                s6c = sb(stp, "s6c", [128, 6], F32)
                oab = sb(stp, "oab", [128, 384], BF16)

                def load_c3(i):
                    fw.dma('sp', o0[i % 2][:], dO[0, i * 128:(i + 1) * 128, :])
                    fw.dma('sp', o1[i % 2][:], dO[1, i * 128:(i + 1) * 128, :])
                    fw.dma('sp', zt_[i % 2][:], dnz[i * 128:(i + 1) * 128, :])
                load_c3(0)
                for i in range(NT):
                    if i + 1 < NT:
                        load_c3(i + 1)
                    b = i % 2
                    fw.tt(osum[:], o0[b][:], o1[b][:], ALU.add)
                    o6 = osum[:, :].rearrange("p (h d) -> p h d", h=6)
                    t6 = otmp[:, :].rearrange("p (h d) -> p h d", h=6)
                    fw.tt(t6, o6, o6, ALU.mult)
                    fw.red(s6c[:], t6)
                    fw.act(s6c[:], s6c[:], AF.Ln, bias=epsT[:], scale=1.0 / 64)
                    fw.act(s6c[:], s6c[:], AF.Exp, scale=-0.5)
                    fw.tt(t6, o6, bc(s6c[:, :], 2, 64), ALU.mult)
                    fw.tt(t6, t6, bc(gdn[:, :], 1, 6), ALU.mult)
                    fw.act(ze[:], zt_[b][:], AF.Exp, scale=-1.0)
                    fw.ts(ze[:], ze[:], 1.0, None, ALU.add, e='pool')
                    fw.recip(ze[:], ze[:])
                    fw.tt(ze[:], ze[:], zt_[b][:], ALU.mult)
                    fw.tt(oab[:], otmp[:], ze[:], ALU.mult)
                    fw.dma('pool', omix[i * 128:(i + 1) * 128, 0:384], oab[:])
                fw.barrier()
            if stop_after == 'C':
                break

            with ExitStack() as stp:
                wo = sb(stp, "wo", [128, 8, D], BF16)
                wos = [sb(stp, f"wos{i}", [128, D], F32) for i in range(2)]
                for k in range(8):
                    fw.dma('sp', wos[k % 2][:], w_out[l][k * 128:(k + 1) * 128, :])
                    fw.copy(wo[:, k, :], wos[k % 2][:], e=('pool' if k % 2 else 'dve'))
                wr = sb(stp, "wr", [128, 8, 36], F32)
                br = sb(stp, "br", [128, 36], F32)
                fw.dma('sp', wr[:], w_r[l].rearrange("(k p) n -> p k n", p=128))
                fw.dma('sp', br[:], b_r[l])
                om = [sb(stp, f"om{i}", [128, D], BF16) for i in range(2)]
                xd = [sb(stp, f"xd{i}", [128, D], F32) for i in range(2)]
                oT = sb(stp, "oT", [128, 8, 128], BF16)
                x1 = sb(stp, "x1", [128, D], F32)
                dtmp = sb(stp, "dtmp", [128, D], F32)
                h2 = sb(stp, "h2", [128, D], F32)
                h2T32 = sb(stp, "h2T32", [128, 8, 128], F32)
                h2Tb = sb(stp, "h2Tb", [128, 8, 128], BF16)
                ssd = sb(stp, "ssd", [128, 1], F32)
                lg = sb(stp, "lg", [128, 36], F32)
                r1 = [sb(stp, f"r1_{i}", [128, 1], F32) for i in range(8)]
                ohg = sb(stp, "ohg", [128, 4], F32)
                eg4 = sb(stp, "eg4", [128, 4], F32)
                t32 = sb(stp, "t32", [128, 32], F32)
                esel = sb(stp, "esel", [128, 8], F32)
                es2 = sb(stp, "es2", [128, 8], F32)
                mk1 = sb(stp, "mk1", [128, 8], F32)
                mk2 = sb(stp, "mk2", [128, 8], F32)
                ge = sb(stp, "ge", [128, 8], F32)
                Gt = sb(stp, "Gt", [128, 32], F32)
                psTb2 = ps(stp, "psTb2", [128, 8, 128], BF16)
                psY2 = ps(stp, "psY2", [128, D])
                psW2 = ps(stp, "psW2", [128, 8, 128])
                psR = ps(stp, "psR", [128, 512])[:, 0:36]

                def load_d(i):
                    fw.dma('sp', om[i % 2][:], omix[i * 128:(i + 1) * 128, :])
                    fw.dma('sp', xd[i % 2][:], x_src[i * 128:(i + 1) * 128, :])
                load_d(0)
                for i in range(NT):
                    if i + 1 < NT:
                        load_d(i + 1)
                    tsl = slice(i * 128, (i + 1) * 128)
                    o_, x_ = om[i % 2], xd[i % 2]
                    fw.tr([(psTb2[:, k, :], o_[:, k * 128:(k + 1) * 128], identb[:]) for k in range(8)])
                    fw.copy(oT[:], psTb2[:], e='act')
                    fw.mm([(psY2[:, 0:512], oT[:, k, :], wo[:, k, 0:512], k == 0, k == 7) for k in range(8)] +
                          [(psY2[:, 512:1024], oT[:, k, :], wo[:, k, 512:1024], k == 0, k == 7) for k in range(8)])
                    fw.tt(dtmp[:], psY2[:], gt1, ALU.mult)
                    fw.tt(x1[:], dtmp[:], x_[:], ALU.add)
                    if not os.environ.get('SKIP_XS0'):
                        fw.dma('pool', xs[0][tsl, :], x1[:])
                    fw.act(dtmp[:], x1[:], AF.Square, accum_out=ssd[:])
                    fw.act(ssd[:], ssd[:], AF.Ln, bias=epsT[:], scale=1.0 / D)
                    fw.act(ssd[:], ssd[:], AF.Exp, scale=-0.5)
                    fw.stt(h2[:], x1[:], ssd[:, 0:1], A2[:], ALU.mult, ALU.mult)
                    fw.tt(h2[:], h2[:], sh2, ALU.add)
                    fw.tr([(psW2[:, k, :], h2[:, k * 128:(k + 1) * 128], identf[:]) for k in range(8)])
                    fw.copy(h2T32[:], psW2[:])
                    fw.copy(h2Tb[:], psW2[:], e='act')
                    if not os.environ.get('SKIP_H2TD'):
                        fw.dma('pool', h2Td[:, :, tsl], h2Tb[:])
                    if os.environ.get("SKIP_ROUTER"):
                        continue
                    fw.mmk(psR[:], [(h2T32[:, k, :], wr[:, k, :]) for k in range(8)])
                    fw.tt(lg[:], psR[:], br[:], ALU.add)
                    gm, ngm, sume, gtp, m1, m2, dd, w1_ = r1
                    fw.red(gm[:], lg[:, 0:4], op=ALU.max)
                    fw.ts(ohg[:], lg[:, 0:4], gm[:, 0:1], None, ALU.is_equal)
                    fw.ts(ngm[:], gm[:], -1.0, None, ALU.mult)
                    fw.act(eg4[:], lg[:, 0:4], AF.Exp, bias=ngm[:], scale=1.0, accum_out=sume[:])
                    fw.recip(gtp[:], sume[:])
                    fw.tt(t32[:, :].rearrange("p (g e) -> p g e", g=4), lg[:, 4:36].rearrange("p (g e) -> p g e", g=4),
                          bc(ohg[:, :], 2, 8), ALU.mult)
                    fw.red(esel[:], t32[:, :].rearrange("p (g e) -> p e g", g=4))
                    fw.red(m1[:], esel[:], op=ALU.max)
                    fw.ts(mk1[:], esel[:], m1[:, 0:1], None, ALU.is_equal)
                    fw.stt(es2[:], mk1[:], -1e30, esel[:], ALU.mult, ALU.add)
                    fw.red(m2[:], es2[:], op=ALU.max)
                    fw.ts(mk2[:], es2[:], m2[:, 0:1], None, ALU.is_equal)
                    fw.tt(dd[:], m2[:], m1[:], ALU.subtract)
                    fw.act(dd[:], dd[:], AF.Exp)
                    fw.ts(w1_[:], dd[:], 1.0, None, ALU.add)
                    fw.recip(w1_[:], w1_[:])
                    fw.tt(dd[:], dd[:], w1_[:], ALU.mult)
                    fw.tt(w1_[:], w1_[:], gtp[:], ALU.mult)
                    fw.tt(dd[:], dd[:], gtp[:], ALU.mult)
                    fw.ts(ge[:], mk1[:], w1_[:, 0:1], None, ALU.mult)
                    fw.stt(ge[:], mk2[:], dd[:, 0:1], ge[:], ALU.mult, ALU.add)
                    fw.tt(Gt[:, :].rearrange("p (g e) -> p g e", g=4), bc(ohg[:, :], 2, 8), bc(ge[:, :], 1, 4), ALU.mult)
                    fw.dma('pool', Gd[tsl, :], Gt[:])
                fw.barrier()

            if stop_after == 'D':
                break
            SBK = min(S, 2048)
            TPB = SBK // 128
            NB = SBK // 512
            with ExitStack() as stp:
                h2Ts = sb(stp, "h2Ts", [128, 8, SBK], BF16)
                yacc = sb(stp, "yacc", [128, TPB, D], F32)
                Gs = sb(stp, "Gs", [128, TPB, 32], F32)
                w1b = [sb(stp, f"w1b_{i}", [128, 8, 256], BF16) for i in range(2)]
                w3b = [sb(stp, f"w3b_{i}", [128, 8, 256], BF16) for i in range(2)]
                w2b = [sb(stp, f"w2b_{i}", [128, 2, D], BF16) for i in range(2)]
                st1 = sb(stp, "st1", [128, 8, 256], F32)
                st3 = sb(stp, "st3", [128, 8, 256], F32)
                st2 = sb(stp, "st2", [128, 2, D], F32)
                e1_ = [sb(stp, f"e1_{i}", [128, 512], F32) for i in range(2)]
                p_ = [sb(stp, f"p_{i}", [128, 512], F32) for i in range(2)]
                hidT = [sb(stp, f"hidT{i}", [128, 2, 512], BF16) for i in range(2)]
                xe1 = st1[:, 0:4, :].rearrange("p k n -> p (k n)")
                xo1 = st3[:, 0:4, :].rearrange("p k n -> p (k n)")
                psH1 = [ps(stp, f"psH1{i}", [128, 512]) for i in range(2)]
                psH3 = [ps(stp, f"psH3{i}", [128, 512]) for i in range(2)]
                psY = [ps(stp, f"psY{i}", [128, D]) for i in range(2)]
                bcount = 0
                ycount = [0]
                pendE = [None]
                for sbk in range(S // SBK):
                    t0 = sbk * SBK
                    fw.dma('sp', h2Ts[:], h2Td[:, :, t0:t0 + SBK])
                    fw.dma('sp', Gs[:], Gd[t0:t0 + SBK, :].rearrange("(t p) e -> p t e", p=128))
                    for e_ in range(32):
                        wa1, wa3, wb2 = w1b[e_ % 2], w3b[e_ % 2], w2b[e_ % 2]
                        fw.dma('sp', st1[:], moe_w1[l, e_].rearrange("(k p) n -> p k n", p=128))
                        fw.dma('sp', st3[:], moe_w3[l, e_].rearrange("(k p) n -> p k n", p=128))
                        fw.dma('sp', st2[:], moe_w2[l, e_].rearrange("(k p) n -> p k n", p=128))
                        fw.copy(wa1[:], st1[:], e='pool')
                        fw.copy(wa3[:], st3[:], e='pool')
                        fw.copy(wb2[:], st2[:], e='pool')
                        for b in range(NB):
                            hT_ = hidT[bcount % 2]
                            bcount += 1
                            tk = slice(b * 512, (b + 1) * 512)
                            for c in range(2):
                                fs = slice(c * 128, (c + 1) * 128)
                                fw.mm([(psH1[c][:], wa1[:, k, fs], h2Ts[:, k, tk], k == 0, k == 7) for k in range(8)] +
                                      [(psH3[c][:], wa3[:, k, fs], h2Ts[:, k, tk], k == 0, k == 7) for k in range(8)])
                                fw.act(e1_[c][:], psH1[c][:], AF.Exp, scale=-1.0)
                                fw.act(e1_[c][:], e1_[c][:], AF.Ln, bias=oneT[:], scale=1.0)
                                fw.act(e1_[c][:], e1_[c][:], AF.Exp, scale=-1.0)
                                fw.tt(p_[c][:], psH1[c][:], e1_[c][:], ALU.mult)
                                fw.tt(hT_[:, c, :], p_[c][:], psH3[c][:], ALU.mult)
                                if pendE[0] is not None:
                                    pendE[0](c)
                                    if c == 1:
                                        pendE[0] = None

                            def mk(bb=b, hh=hT_, ee=e_, wb=wb2):
                                def f(half):
                                    for t4 in ((0, 1, 2, 3) if half is None else (2 * half, 2 * half + 1)):
                                        t = bb * 4 + t4
                                        py = psY[ycount[0] % 2]
                                        ycount[0] += 1
                                        ts4 = slice(t4 * 128, (t4 + 1) * 128)
                                        fw.mm([(py[:, 0:512], hh[:, j, ts4], wb[:, j, 0:512], j == 0, j == 1) for j in range(2)] +
                                              [(py[:, 512:1024], hh[:, j, ts4], wb[:, j, 512:1024], j == 0, j == 1) for j in range(2)])
                                        if ee == 0:
                                            fw.ts(yacc[:, t, :], py[:], Gs[:, t, ee:ee + 1], None, ALU.mult)
                                        else:
                                            fw.stt(yacc[:, t, :], py[:], Gs[:, t, ee:ee + 1], yacc[:, t, :], ALU.mult, ALU.add)
                                return f
                            pendE[0] = mk()
                    if pendE[0] is not None:
                        pendE[0](None)
                        pendE[0] = None
                    for t in range(TPB):
                        rows = slice(t0 + t * 128, t0 + (t + 1) * 128)
                        fw.dma('sp', xe1, xs[0][rows, :])
                        fw.tt(xo1, yacc[:, t, :], gt2, ALU.mult, e='pool')
                        fw.tt(xo1, xo1, xe1, ALU.add)
                        fw.dma('pool', x_dst[rows, :], xo1)
                fw.barrier()

        if dbg and os.environ.get("DBG_DN"):
            with ExitStack() as stp:
                t_f = sb(stp, "dbgdn_f", [128, D], F32)
                fw.memset(t_f[:], 0.0)
                for i in range(NT):
                    rows = slice(i * 128, (i + 1) * 128)
                    if os.environ.get("DBG_DN") == "U":
                        fw.dma('sp', t_f[:, 0:768], dU[rows, :])
                    else:
                        fw.dma('sp', t_f[:, 0:384], dO[0, rows, :])
                        fw.dma('sp', t_f[:, 384:768], dO[1, rows, :])
                    fw.dma('sp', t_f[:, 768:792], dE[rows, :])
                    fw.dma('sp', t_f[:, 800:812], gT[:, i, :])
                    fw.dma('sp', t_f[:, 812:824], betaT[:, i, :])
                    fw.dma('sp', dbg_out[rows, :], t_f[:])
                fw.barrier()
        elif dbg and stop_after in ('B', 'C'):
            with ExitStack() as stp:
                t_b = sb(stp, "dbg_b", [128, D], BF16)
                t_f = sb(stp, "dbg_f", [128, D], F32)
                for i in range(NT):
                    fw.dma('sp', t_b[:], omix[i * 128:(i + 1) * 128, :],
                           extra_reads=[("omix", qb, col) for qb in range(NQB) for col in range(384, 1024, 64)])
                    fw.copy(t_f[:], t_b[:])
                    fw.dma('sp', dbg_out[i * 128:(i + 1) * 128, :], t_f[:])
                fw.barrier()
        fw.barrier()
        print("instructions emitted:", fw.ninst)
    return nc


def rope_tables(S, rot_dim, grid_w=64):
    t = np.arange(S)
    row = (t // grid_w).astype(np.float32)
    col = (t % grid_w).astype(np.float32)
    n_freq = rot_dim // 4
    inv = (10000.0 ** (-np.arange(n_freq, dtype=np.float32) / n_freq)).astype(np.float32)
    ang = np.concatenate([row[:, None] * inv, col[:, None] * inv], axis=-1).astype(np.float32)
    return np.cos(ang).astype(np.float32), np.sin(ang).astype(np.float32)


def rep(v, n=128):
    v = np.asarray(v, np.float32)
    return np.ascontiguousarray(np.broadcast_to(v[..., None, :], v.shape[:-1] + (n, v.shape[-1])))


def prep_inputs(inp, S, depth):
    NT = S // 128
    f = lambda a: np.ascontiguousarray(np.asarray(a, np.float32))
    perm = np.arange(INC)
    base = 1560
    order = [0, 3, 1, 4, 2, 5]
    perm[base:base + 384] = np.concatenate([base + h * 64 + np.arange(64) for h in order])
    com = {}
    com["ada_w"] = f(inp["ada_w"][:depth])
    com["ada_b"] = rep(inp["ada_b"][:depth])
    com["g1"] = rep(inp["norm1_g"][:depth])
    com["g2"] = rep(inp["norm2_g"][:depth])
    com["w_in"] = f(np.asarray(inp["w_in"])[:depth][:, :, perm])
    com["w_out"] = f(inp["w_out"][:depth])
    com["gq_g"] = rep(inp["gqa_q_g"][:depth])
    com["gk_g"] = rep(inp["gqa_k_g"][:depth])
    com["mql_g"] = f(np.asarray(inp["mla_q_lat_g"])[:depth, :, None])
    com["mkvl_g"] = f(np.asarray(inp["mla_kv_lat_g"])[:depth, :, None])
    com["w_uq"] = f(inp["mla_w_uq"][:depth])
    com["w_ukv"] = f(inp["mla_w_ukv"][:depth])
    com["mqn_g"] = rep(inp["mla_qn_g"][:depth])
    com["mqr_g"] = rep(inp["mla_qr_g"][:depth])
    com["mkn_g"] = rep(inp["mla_kn_g"][:depth])
    com["mkr_g"] = rep(inp["mla_kr_g"][:depth])
    cg, sg = rope_tables(S, 64)
    cm, sm = rope_tables(S, 32)
    tm = lambda a: np.ascontiguousarray(a.reshape(NT, 128, -1).transpose(1, 0, 2))
    com["cosg"], com["sing"], com["cosm"], com["sinm"] = tm(cg), tm(sg), tm(cm), tm(sm)
    com["identf"] = np.eye(128, dtype=np.float32)
    com["w_r"] = f(np.concatenate([np.asarray(inp["moe_w_group"])[:depth], np.asarray(inp["moe_w_router"])[:depth]], axis=-1))
    com["b_r"] = rep(np.concatenate([np.asarray(inp["moe_b_group"])[:depth], np.asarray(inp["moe_b_router"])[:depth]], axis=-1))
    com["moe_w1"] = f(inp["moe_w1"][:depth])
    com["moe_w3"] = f(inp["moe_w3"][:depth])
    com["moe_w2"] = f(inp["moe_w2"][:depth])
    com["wconv"] = rep(np.asarray(inp["dn_conv"])[:depth].reshape(depth, 5 * 1152))
    com["alog"] = rep(np.asarray(inp["dn_a_log"])[:depth].reshape(depth, 12))
    com["dtb"] = rep(np.asarray(inp["dn_dt_bias"])[:depth].reshape(depth, 12))
    com["dng"] = rep(inp["dn_out_g"][:depth])
    ii = np.arange(128)[:, None]
    jj = np.arange(128)[None, :]
    NEG = -30000.0
    mk = np.stack([(ii <= jj), (ii >= jj),
                   np.where(jj <= ii, 0.0, NEG), np.where(jj >= ii, 0.0, NEG),
                   (jj < ii), (jj > ii),
                   (ii // 32 == jj // 32) & (ii != jj), (ii // 64 == jj // 64) & (ii // 32 != jj // 32), (ii // 64 != jj // 64)],
                  axis=1).astype(np.float32)
    mk[:, 7:9, :] *= -1.0
    com["masks"] = np.ascontiguousarray(mk)
    return com


def kernel(**inp):
    S, depth = 4096, 4
    com = prep_inputs(inp, S, depth)
    x = np.asarray(inp["x"], np.float32)
    c = np.asarray(inp["c"], np.float32)
    nc = build(S, depth)
    maps = []
    for core in range(8):
        b = core % 4
        m = dict(com)
        m["x"] = np.ascontiguousarray(x[b])
        m["c_pk"] = np.ascontiguousarray(c[b].reshape(8, 128).T)
        maps.append(m)
    res = run_bass_kernel_spmd(nc, maps, core_ids=list(range(8)))
    return np.stack([res.results[b]["y"] for b in range(4)], axis=0).astype(np.float32)
```

```python
import math
import os
import numpy as np
from contextlib import ExitStack
import concourse.bass as bass
import concourse.mybir as mybir
from concourse.bass_utils import run_bass_kernel_spmd

F32 = mybir.dt.float32
BF16 = mybir.dt.bfloat16
I32 = mybir.dt.int32
AF = mybir.ActivationFunctionType
ALU = mybir.AluOpType
AX = mybir.AxisListType
NDS = 20
import re
PSUM_RE = re.compile(r'p\d+_')

D = 1024
INC = 2552
EPS = 1e-6


class FW:
    def __init__(self, nc, stack):
        self.nc = nc
        self.stack = stack
        self.eng = {'pe': nc.tensor, 'act': nc.scalar, 'dve': nc.vector, 'pool': nc.gpsimd, 'sp': nc.sync}
        self.sem = {}
        self.cnt = {}
        for e in self.eng:
            self.sem[('c', e)] = stack.enter_context(nc.semaphore('s_' + e))
            self.cnt[('c', e)] = 0
        self.dring = {}
        self.dnext = {}
        for q in ('sp', 'act', 'pool'):
            self.dring[q] = []
            for i in range(NDS):
                k = ('d', q, i)
                self.sem[k] = stack.enter_context(nc.semaphore(f'd_{q}_{i}'))
                self.cnt[k] = 0
                self.dring[q].append(k)
            self.dnext[q] = 0
        self.waited = {e: {} for e in self.eng}
        self.W = {}
        self.R = {}
        self.ninst = 0

    @staticmethod
    def _res(x):
        if isinstance(x, tuple):
            if len(x) == 2 and not isinstance(x[0], (str, int)):
                return x[1]
            return x
        if isinstance(x, str):
            return x
        return x.name

    @staticmethod
    def ap(x):
        if isinstance(x, tuple):
            return x[0]
        return x

    def _wait(self, e, toks):
        eng = self.eng[e]
        for k, v in toks.items():
            if self.waited[e].get(k, 0) < v:
                eng.wait_ge(self.sem[k], v)
                self.waited[e][k] = v
                self.ninst += 1

    def _deps(self, reads, writes):
        toks = {}

        def add(d):
            for k, v in d.items():
                if toks.get(k, 0) < v:
                    toks[k] = v
        for r in reads:
            add(self.W.get(r, {}))
        for w in writes:
            add(self.W.get(w, {}))
            add(self.R.get(w, {}))
        return toks

    def _commit(self, tok, reads, writes):
        k, v = tok
        for r in reads:
            d = self.R.setdefault(r, {})
            d[k] = max(d.get(k, 0), v)
        for w in writes:
            self.W[w] = {k: v}
            self.R[w] = {}

    def op(self, e, fn, reads, writes):
        reads = [self._res(r) for r in reads if r is not None and not isinstance(r, (int, float))]
        writes = [self._res(w) for w in writes]
        writes = writes + [r for r in reads if isinstance(r, str) and PSUM_RE.match(r)]
        toks = self._deps(reads, writes)
        if e == 'pe':
            toks.pop(('c', 'pe'), None)
        self._wait(e, toks)
        ins = fn()
        k = ('c', e)
        self.cnt[k] += 1
        ins.then_inc(self.sem[k], 1)
        self.ninst += 1
        self._commit((k, self.cnt[k]), reads, writes)

    def dma(self, q, out, in_, extra_reads=(), extra_writes=()):
        reads = [self._res(in_)] + [self._res(r) for r in extra_reads]
        writes = [self._res(out)] + [self._res(w) for w in extra_writes]
        toks = self._deps(reads, writes)
        k = self.dring[q][self.dnext[q]]
        self.dnext[q] = (self.dnext[q] + 1) % NDS
        if self.cnt[k] > 0:
            toks[k] = max(toks.get(k, 0), self.cnt[k])
        self._wait(q, toks)
        ins = self.eng[q].dma_start(out=self.ap(out), in_=self.ap(in_))
        self.cnt[k] += 16
        ins.then_inc(self.sem[k], 16)
        self.ninst += 1
        self._commit((k, self.cnt[k]), reads, writes)

    def barrier(self, engines=('pe', 'act', 'dve', 'pool', 'sp')):
        toks = {k: v for k, v in self.cnt.items() if v > 0}
        for e in engines:
            self._wait(e, dict(toks))

    def act(self, out, in_, func, bias=None, scale=1.0, accum_out=None, e='act'):
        kw = {}
        if bias is not None:
            kw['bias'] = self.ap(bias)
        if accum_out is not None:
            kw['accum_out'] = self.ap(accum_out)
        sc = self.ap(scale) if not isinstance(scale, (int, float)) else scale
        wr = [out] + ([accum_out] if accum_out is not None else [])
        self.op(e, lambda: self.eng[e].activation(out=self.ap(out), in_=self.ap(in_), func=func, scale=sc, **kw),
                [in_, bias, scale], wr)

    def tt(self, out, in0, in1, op, e='dve'):
        self.op(e, lambda: self.eng[e].tensor_tensor(out=self.ap(out), in0=self.ap(in0), in1=self.ap(in1), op=op),
                [in0, in1], [out])

    def ts(self, out, in0, s1, s2, op0, op1=None, e='dve'):
        a1 = self.ap(s1) if not isinstance(s1, (int, float)) else s1
        a2 = (self.ap(s2) if not isinstance(s2, (int, float)) else s2) if s2 is not None else None
        kw = {}
        if op1 is not None:
            kw['op1'] = op1
        self.op(e, lambda: self.eng[e].tensor_scalar(out=self.ap(out), in0=self.ap(in0), scalar1=a1, scalar2=a2,
                                                     op0=op0, **kw), [in0, s1, s2], [out])

    def stt(self, out, in0, scalar, in1, op0, op1, e='dve'):
        a = self.ap(scalar) if not isinstance(scalar, (int, float)) else scalar
        self.op(e, lambda: self.eng[e].scalar_tensor_tensor(out=self.ap(out), in0=self.ap(in0), scalar=a,
                                                            in1=self.ap(in1), op0=op0, op1=op1),
                [in0, scalar, in1], [out])

    def red(self, out, in_, op=ALU.add, e='dve'):
        self.op(e, lambda: self.eng[e].tensor_reduce(out=self.ap(out), in_=self.ap(in_), axis=AX.X, op=op),
                [in_], [out])

    def recip(self, out, in_):
        self.op('dve', lambda: self.nc.vector.reciprocal(out=self.ap(out), in_=self.ap(in_)), [in_], [out])

    def copy(self, out, in_, e='dve'):
        if e == 'act':
            self.op(e, lambda: self.eng[e].copy(out=self.ap(out), in_=self.ap(in_)), [in_], [out])
        else:
            self.op(e, lambda: self.eng[e].tensor_copy(out=self.ap(out), in_=self.ap(in_)), [in_], [out])

    def memset(self, out, val, e='pool'):
        self.op(e, lambda: self.eng[e].memset(self.ap(out), val), [], [out])

    def mm(self, items, extra_reads=()):
        rd = list(extra_reads)
        wr = []
        for o, l, r, _, _ in items:
            rd += [l, r]
            wr.append(o)

        def fn():
            ins = None
            for o, l, r, st, sp in items:
                ins = self.nc.tensor.matmul(self.ap(o), self.ap(l), self.ap(r), start=st, stop=sp)
            return ins
        self.op('pe', fn, rd, wr)

    def mmk(self, out, pairs):
        n = len(pairs)
        self.mm([(out, l, r, i == 0, i == n - 1) for i, (l, r) in enumerate(pairs)])

    def tr(self, items):
        rd = []
        wr = []
        for o, i, idt in items:
            rd += [i, idt]
            wr.append(o)

        def fn():
            ins = None
            for o, i, idt in items:
                ins = self.nc.tensor.transpose(self.ap(o), self.ap(i), self.ap(idt))
            return ins
        self.op('pe', fn, rd, wr)


def bc(ap, axis, n):
    a = ap.unsqueeze(axis)
    shp = list(a.shape)
    shp[axis] = n
    return a.to_broadcast(shp)


def build(S=4096, depth=4, stop_after=None, dbg=False):
    NT = S // 128
    NQB = S // 512
    nc = bass.Bass("TRN2", target_bir_lowering=False)

    def din(name, shape, dt=F32):
        return nc.dram_tensor(name, list(shape), dt, kind="ExternalInput").ap()

    def scr(name, shape, dt):
        return nc.dram_tensor(name, list(shape), dt, kind="Internal").ap()

    x_in = din("x", [S, D])
    c_pk = din("c_pk", [128, 8])
    ada_w = din("ada_w", [depth, D, 6 * D])
    ada_b = din("ada_b", [depth, 128, 6 * D])
    g1_d = din("g1", [depth, 128, D])
    g2_d = din("g2", [depth, 128, D])
    w_in = din("w_in", [depth, D, INC])
    w_out = din("w_out", [depth, D, D])
    gq_g = din("gq_g", [depth, 128, 64])
    gk_g = din("gk_g", [depth, 128, 64])
    mql_g = din("mql_g", [depth, 192, 1])
    mkvl_g = din("mkvl_g", [depth, 128, 1])
    w_uq = din("w_uq", [depth, 192, 384])
    w_ukv = din("w_ukv", [depth, 128, 512])
    mqn_g = din("mqn_g", [depth, 128, 64])
    mqr_g = din("mqr_g", [depth, 128, 32])
    mkn_g = din("mkn_g", [depth, 128, 64])
    mkr_g = din("mkr_g", [depth, 128, 32])
    cosg_d = din("cosg", [128, NT, 32])
    sing_d = din("sing", [128, NT, 32])
    cosm_d = din("cosm", [128, NT, 16])
    sinm_d = din("sinm", [128, NT, 16])
    identf_d = din("identf", [128, 128])
    w_r = din("w_r", [depth, D, 36])
    b_r = din("b_r", [depth, 128, 36])
    moe_w1 = din("moe_w1", [depth, 32, D, 256])
    moe_w3 = din("moe_w3", [depth, 32, D, 256])
    moe_w2 = din("moe_w2", [depth, 32, 256, D])
    wconv_d = din("wconv", [depth, 128, 5 * 1152])
    alog_d = din("alog", [depth, 128, 12])
    dtb_d = din("dtb", [depth, 128, 12])
    dng_d = din("dng", [depth, 128, 64])
    masks_d = din("masks", [128, 9, 128])
    y_out = nc.dram_tensor("y", [S, D], F32, kind="ExternalOutput").ap()

    qTg = scr("qTg", [3, 128, S], BF16)
    kTg = scr("kTg", [128, S], BF16)
    vg = scr("vg", [S, 130], BF16)
    qTm = scr("qTm", [4, 96, S], BF16)
    kTm = scr("kTm", [4, 96, S], BF16)
    vm = scr("vm", [S, 260], BF16)
    omix = scr("omix", [S, D], BF16)
    xs = [scr("xs0", [S, D], F32), scr("xs1", [S, D], F32), scr("xs2", [S, D], F32)]
    h2Td = scr("h2Td", [128, 8, S], BF16)
    Gd = scr("Gd", [S, 32], F32)
    dnpre = scr("dnpre", [S + 4, 1152], F32)
    dnz = scr("dnz", [S, 384], F32)
    dU = scr("dU", [S, 768], F32)
    dWT = scr("dWT", [NT, 64, 12 * 128], BF16)
    dAT = scr("dAT", [NT, 128, 12 * 128], BF16)
    dKT = scr("dKT", [S, 768], BF16)
    dQT = scr("dQT", [NT, 64, 6 * 128], BF16)
    dE = scr("dE", [S, 24], F32)
    dO = scr("dO", [2, S, 384], F32)
    dbg_out = None
    if dbg:
        dbg_out = nc.dram_tensor("dbg", [S, D], F32, kind="ExternalOutput").ap()

    with ExitStack() as st0:
        fw = FW(nc, st0)

        uid = [0]

        def sb(stk, name, shape, dt):
            uid[0] += 1
            return stk.enter_context(nc.sbuf_tensor(f"s{uid[0]}_{name}", list(shape), dt))

        def ps(stk, name, shape, dt=F32):
            uid[0] += 1
            return stk.enter_context(nc.psum_tensor(f"p{uid[0]}_{name}", list(shape), dt))

        identf = sb(st0, "identf", [128, 128], F32)
        identb = sb(st0, "identb", [128, 128], BF16)
        epsT = sb(st0, "epsT", [128, 1], F32)
        cactB = sb(st0, "cactB", [128, 8, 128], F32)
        mod = sb(st0, "mod", [128, 6 * D], F32)
        A1 = sb(st0, "A1", [128, D], F32)
        A2 = sb(st0, "A2", [128, D], F32)
        fw.dma('sp', identf[:], identf_d)
        fw.memset(epsT[:], EPS)
        oneT = sb(st0, "oneT", [128, 1], F32)
        fw.memset(oneT[:], 1.0)
        onesf = sb(st0, "onesf", [128, 128], F32)
        fw.memset(onesf[:], 1.0)
        masks = sb(st0, "masks", [128, 9, 128], F32)
        fw.dma('sp', masks[:], masks_d)
        gT = sb(st0, "gT", [128, NT, 12], F32)
        betaT = sb(st0, "betaT", [128, NT, 12], F32)
        with ExitStack() as stp:
            zt = sb(stp, "zt", [128, 1152], F32)
            fw.memset(zt[:], 0.0)
            fw.dma('sp', dnpre[0:2, :], zt[0:2, :])
            fw.dma('sp', dnpre[S + 2:S + 4, :], zt[0:2, :])
            fw.barrier()
        fw.copy(identb[:], identf[:], e='dve')
        with ExitStack() as stp:
            ct = sb(stp, "ct", [128, 8], F32)
            ce = sb(stp, "ce", [128, 8], F32)
            fw.dma('sp', ct[:], c_pk)
            fw.act(ce[:], ct[:], AF.Exp, scale=-1.0)
            fw.ts(ce[:], ce[:], 1.0, None, ALU.add)
            fw.recip(ce[:], ce[:])
            fw.tt(ce[:], ce[:], ct[:], ALU.mult)
            fw.copy(cactB[:], bc(ce[:, :], 2, 128))
            fw.barrier()

        for l in range(depth):
            x_src = x_in if l == 0 else xs[1 + (l - 1) % 2]
            x_dst = y_out if l == depth - 1 else xs[1 + l % 2]
            last = (l == depth - 1)
            with ExitStack() as stp:
                psM = [ps(stp, f"psM{i}", [128, 512]) for i in range(2)]
                awt = [sb(stp, f"awt{i}", [128, 8, 512], F32) for i in range(2)]
                abt = sb(stp, "abt", [128, 6 * D], F32)
                g1t = sb(stp, "g1t", [128, D], F32)
                g2t = sb(stp, "g2t", [128, D], F32)
                fw.dma('sp', abt[:], ada_b[l])
                fw.dma('sp', g1t[:], g1_d[l])
                fw.dma('sp', g2t[:], g2_d[l])
                for cb in range(12):
                    a = awt[cb % 2]
                    fw.dma('sp', a[:], ada_w[l][:, cb * 512:(cb + 1) * 512].rearrange("(k p) n -> p k n", p=128))
                    fw.mmk(psM[cb % 2][:], [(cactB[:, k, :], a[:, k, :]) for k in range(8)])
                    fw.tt(mod[:, cb * 512:(cb + 1) * 512], psM[cb % 2][:], abt[:, cb * 512:(cb + 1) * 512], ALU.add)
                fw.stt(A1[:], mod[:, D:2 * D], 1.0, g1t[:], ALU.add, ALU.mult)
                fw.stt(A2[:], mod[:, 4 * D:5 * D], 1.0, g2t[:], ALU.add, ALU.mult)
                fw.barrier()
            sh1 = mod[:, 0:D]
            gt1 = mod[:, 2 * D:3 * D]
            sh2 = mod[:, 3 * D:4 * D]
            gt2 = mod[:, 5 * D:6 * D]

            with ExitStack() as stp:
                win = sb(stp, "win", [128, 8, INC], BF16)
                cosg = sb(stp, "cosg", [128, NT, 32], F32)
                sing = sb(stp, "sing", [128, NT, 32], F32)
                cosm = sb(stp, "cosm", [128, NT, 16], F32)
                sinm = sb(stp, "sinm", [128, NT, 16], F32)
                fw.dma('sp', cosg[:], cosg_d)
                fw.dma('sp', sing[:], sing_d)
                fw.dma('sp', cosm[:], cosm_d)
                fw.dma('sp', sinm[:], sinm_d)
                wst = [sb(stp, f"wst{i}", [128, INC], F32) for i in range(2)]
                for k in range(8):
                    fw.dma('sp', wst[k % 2][:], w_in[l][k * 128:(k + 1) * 128, :])
                    fw.copy(win[:, k, :], wst[k % 2][:], e=('pool' if k % 2 else 'dve'))
                wuq = sb(stp, "wuq", [128, 2, 384], BF16)
                wukv = sb(stp, "wukv", [128, 512], BF16)
                wuqs = sb(stp, "wuqs", [128, 2, 384], F32)
                wukvs = sb(stp, "wukvs", [128, 512], F32)
                gql = sb(stp, "gql", [128, 2], F32)
                gkvl = sb(stp, "gkvl", [128, 1], F32)
                fw.dma('sp', wuqs[:, 0, :], w_uq[l][0:128, :])
                fw.dma('sp', wuqs[0:64, 1, :], w_uq[l][128:192, :])
                fw.dma('sp', wukvs[:], w_ukv[l])
                fw.dma('sp', gql[:, 0:1], mql_g[l][0:128, :])
                fw.dma('sp', gql[0:64, 1:2], mql_g[l][128:192, :])
                fw.dma('sp', gkvl[:], mkvl_g[l])
                fw.ts(wuq[:, 0, :], wuqs[:, 0, :], gql[:, 0:1], None, ALU.mult)
                fw.ts(wuq[0:64, 1, :], wuqs[0:64, 1, :], gql[0:64, 1:2], None, ALU.mult)
                fw.ts(wukv[:], wukvs[:], gkvl[:, 0:1], None, ALU.mult)
                gains = {}
                for nm, dd, w_ in (("gq", gq_g, 64), ("gk", gk_g, 64), ("mqn", mqn_g, 64), ("mqr", mqr_g, 32),
                                   ("mkn", mkn_g, 64), ("mkr", mkr_g, 32)):
                    gains[nm] = sb(stp, "gn_" + nm, [128, w_], F32)
                    fw.dma('sp', gains[nm][:], dd[l])

                xt = [sb(stp, f"xt{i}", [128, D], F32) for i in range(2)]
                junk = sb(stp, "junk", [128, D], F32)
                ss = sb(stp, "ss", [128, 1], F32)
                hh = sb(stp, "hh", [128, D], F32)
                hT = sb(stp, "hT", [128, 8, 128], BF16)
                prs = [sb(stp, f"pr{i}", [128, INC], F32) for i in range(2)]
                qb_ = sb(stp, "qb_", [128, 384], BF16)
                kb_ = sb(stp, "kb_", [128, 128], BF16)
                vb_ = sb(stp, "vb_", [128, 2, 65], BF16)
                qTs = sb(stp, "qTs", [128, 3, 128], BF16)
                kTs = sb(stp, "kTs", [128, 128], BF16)
                cqn = sb(stp, "cqn", [128, 192], BF16)
                ckvn = sb(stp, "ckvn", [128, 128], BF16)
                cqT = sb(stp, "cqT", [128, 2, 128], BF16)
                ckvT = sb(stp, "ckvT", [128, 128], BF16)
                qup = sb(stp, "qup", [128, 384], F32)
                kvup = sb(stp, "kvup", [128, 512], F32)
                qm = sb(stp, "qm", [128, 4, 96], BF16)
                km = sb(stp, "km", [128, 4, 96], BF16)
                vmb = sb(stp, "vmb", [128, 4, 65], BF16)
                krr = sb(stp, "krr", [128, 32], F32)
                qTms = sb(stp, "qTms", [96, 4, 128], BF16)
                kTms = sb(stp, "kTms", [96, 4, 128], BF16)
                psW = ps(stp, "psW", [128, 8, 128])
                psP = [ps(stp, f"psP{i}", [128, 512]) for i in range(2)]
                psTb = ps(stp, "psTb", [128, 8, 128], BF16)
                psU = ps(stp, "psU", [128, 512])
                fw.memset(vb_[:], 1.0)
                fw.memset(vmb[:], 1.0)
                negA = sb(stp, "negA", [128, 12], F32)
                dtb = sb(stp, "dtb", [128, 12], F32)
                t12 = [sb(stp, f"t12_{i}", [128, 12], F32) for i in range(3)]
                fw.dma('sp', negA[:], alog_d[l])
                fw.dma('sp', dtb[:], dtb_d[l])
                fw.act(negA[:], negA[:], AF.Exp)
                fw.ts(negA[:], negA[:], -1.0, None, ALU.mult)

                tmpas = [sb(stp, f"tmpa_{i}", [128, 512], F32) for i in range(4)]
                tmpbs = [sb(stp, f"tmpb_{i}", [128, 512], F32) for i in range(2)]
                s6s = [sb(stp, f"s6_{i}", [128, 16], F32) for i in range(4)]
                nrms = [sb(stp, f"nrm_{i}", [128, 512], F32) for i in range(4)]

                def headnorm(src, H, Dh, gain, out, mean, sl):
                    t = tmpas[sl][:, 0:H * Dh].rearrange("p (h d) -> p h d", h=H)
                    s_ = s6s[sl]
                    fw.tt(t, src, src, ALU.mult)
                    yield
                    fw.red(s_[:, 0:H], t)
                    yield
                    fw.act(s_[:, 0:H], s_[:, 0:H], AF.Ln, bias=epsT[:], scale=(1.0 / Dh if mean else 1.0))
                    yield
                    fw.act(s_[:, 0:H], s_[:, 0:H], AF.Exp, scale=-0.5)
                    yield
                    fw.tt(t, src, bc(s_[:, 0:H], 2, Dh), ALU.mult)
                    yield
                    if gain is not None:
                        fw.tt(out, t, bc(gain[:, :], 1, H), ALU.mult)
                    else:
                        fw.copy(out, t)

                def rope(src, H, R, cs, sn, out, sl):
                    h2 = R // 2
                    x1 = src[:, :, 0:h2]
                    x2 = src[:, :, h2:R]
                    c_ = bc(cs, 1, H)
                    s_ = bc(sn, 1, H)
                    ta = tmpas[sl][:, 0:H * h2].rearrange("p (h d) -> p h d", h=H)
                    tb = tmpbs[sl][:, 0:H * h2].rearrange("p (h d) -> p h d", h=H)
                    fw.tt(ta, x1, c_, ALU.mult)
                    fw.tt(tb, x2, s_, ALU.mult)
                    yield
                    fw.tt(out[:, :, 0:h2], ta, tb, ALU.subtract)
                    yield
                    fw.tt(ta, x1, s_, ALU.mult)
                    fw.tt(tb, x2, c_, ALU.mult)
                    yield
                    fw.tt(out[:, :, h2:R], ta, tb, ALU.add)

                def run_il(gens):
                    gens = list(gens)
                    while gens:
                        for g_ in list(gens):
                            try:
                                next(g_)
                            except StopIteration:
                                gens.remove(g_)

                fw.dma('sp', xt[0][:], x_src[0:128, :])

                def frontA(i):
                    xc = xt[i % 2]
                    pr = prs[i % 2]
                    if i + 1 < NT:
                        fw.dma('sp', xt[(i + 1) % 2][:], x_src[(i + 1) * 128:(i + 2) * 128, :])
                    fw.act(junk[:], xc[:], AF.Square, accum_out=ss[:])
                    fw.act(ss[:], ss[:], AF.Ln, bias=epsT[:], scale=1.0 / D)
                    fw.act(ss[:], ss[:], AF.Exp, scale=-0.5)
                    fw.stt(hh[:], xc[:], ss[:, 0:1], A1[:], ALU.mult, ALU.mult)
                    fw.tt(hh[:], hh[:], sh1, ALU.add)
                    fw.tr([(psW[:, k, :], hh[:, k * 128:(k + 1) * 128], identf[:]) for k in range(8)])
                    fw.copy(hT[:], psW[:], e='act')
                    for cb in range(5):
                        c0 = cb * 512
                        c1 = min(INC, c0 + 512)
                        pp = psP[cb % 2]
                        fw.mmk(pp[:, 0:c1 - c0], [(hT[:, k, :], win[:, k, c0:c1]) for k in range(8)])
                        fw.copy(pr[:, c0:c1], pp[:, 0:c1 - c0], e=('dve' if cb % 2 == 0 else 'act'))

                def backA(i):
                    pr = prs[i % 2]
                    tsl = slice(i * 128, (i + 1) * 128)
                    fw.dma('pool', (dnpre[2 + i * 128:2 + (i + 1) * 128, :], ("dnpre", i)), pr[:, 0:1152])
                    fw.dma('pool', (dnz[tsl, :], ("dnz", i)), pr[:, 1152:1536])
                    fw.act(t12[0][:], pr[:, 1536:1548], AF.Exp, scale=-1.0)
                    fw.ts(t12[0][:], t12[0][:], 1.0, None, ALU.add)
                    fw.recip(betaT[:, i, :], t12[0][:])
                    fw.tt(t12[1][:], pr[:, 1548:1560], dtb[:], ALU.add)
                    fw.ts(t12[2][:], t12[1][:], -1.0, None, ALU.mult)
                    fw.tt(t12[2][:], t12[2][:], t12[1][:], ALU.max)
                    fw.act(t12[2][:], t12[2][:], AF.Exp, scale=-1.0)
                    fw.act(t12[2][:], t12[2][:], AF.Ln, bias=oneT[:], scale=1.0)
                    fw.ts(t12[1][:], t12[1][:], 0.0, None, ALU.max)
                    fw.tt(t12[1][:], t12[1][:], t12[2][:], ALU.add)
                    fw.tt(gT[:, i, :], t12[1][:], negA[:], ALU.mult)
                    qv = pr[:, 1560:1944].rearrange("p (h d) -> p h d", h=6)
                    nq = nrms[0][:, 0:384].rearrange("p (h d) -> p h d", h=6)
                    kv_ = pr[:, 1944:2072].rearrange("p (h d) -> p h d", h=2)
                    nk = nrms[1][:, 0:128].rearrange("p (h d) -> p h d", h=2)
                    cq = pr[:, 2200:2392].rearrange("p (h d) -> p h d", h=1)
                    ckv = pr[:, 2392:2520].rearrange("p (h d) -> p h d", h=1)
                    run_il([headnorm(qv, 6, 64, gains["gq"], nq, True, 0),
                            headnorm(kv_, 2, 64, gains["gk"], nk, True, 1),
                            headnorm(cq, 1, 192, None, cqn[:, :].rearrange("p (h d) -> p h d", h=1), True, 2),
                            headnorm(ckv, 1, 128, None, ckvn[:, :].rearrange("p (h d) -> p h d", h=1), True, 3)])
                    run_il([rope(nq, 6, 64, cosg[:, i, :], sing[:, i, :], qb_[:, :].rearrange("p (h d) -> p h d", h=6), 0),
                            rope(nk, 2, 64, cosg[:, i, :], sing[:, i, :], kb_[:, :].rearrange("p (h d) -> p h d", h=2), 1)])
                    fw.tr([(psTb[:, j, :], qb_[:, j * 128:(j + 1) * 128], identb[:]) for j in range(3)] +
                          [(psTb[:, 3, :], kb_[:], identb[:])] +
                          [(psTb[:, 4, :], cqn[:, 0:128], identb[:]),
                           (psTb[0:64, 5, :], cqn[:, 128:192], identb[:]),
                           (psTb[:, 6, :], ckvn[:], identb[:])])
                    fw.copy(cqT[:, 0, :], psTb[:, 4, :])
                    fw.copy(cqT[0:64, 1, :], psTb[0:64, 5, :])
                    fw.copy(ckvT[:], psTb[:, 6, :])
                    fw.copy(qTs[:], psTb[:, 0:3, :])
                    fw.copy(kTs[:], psTb[:, 3, :])
                    fw.mm([(psU[:, 0:384], cqT[:, 0, :], wuq[:, 0, :], True, False),
                           (psU[:, 0:384], cqT[0:64, 1, :], wuq[0:64, 1, :], False, True)])
                    fw.copy(qup[:], psU[:, 0:384])
                    fw.mm([(psU[:], ckvT[:], wukv[:], True, True)])
                    fw.copy(kvup[:], psU[:])
                    fw.dma('pool', (qTg[:, :, tsl].rearrange("j p t -> p j t"), ("qTg", i)), qTs[:])
                    fw.dma('pool', (kTg[:, tsl], ("kTg", i)), kTs[:])
                    fw.copy(vb_[:, :, 0:64], pr[:, 2072:2200].rearrange("p (h d) -> p h d", h=2), e='pool')
                    fw.dma('pool', (vg[tsl, :], ("vg", i)), vb_[:, :, :].rearrange("p h d -> p (h d)"))
                    qu = qup[:, :].rearrange("p (h d) -> p h d", h=4)
                    qmv = qm[:, :, :]
                    n4r = nrms[2][:, 0:128].rearrange("p (h d) -> p h d", h=4)
                    ku = kvup[:, :].rearrange("p (h d) -> p h d", h=4)
                    krv = pr[:, 2520:2552].rearrange("p (h d) -> p h d", h=1)
                    n1r = nrms[3][:, 0:32].rearrange("p (h d) -> p h d", h=1)
                    run_il([headnorm(qu[:, :, 0:64], 4, 64, gains["mqn"], qmv[:, :, 0:64], True, 0),
                            headnorm(qu[:, :, 64:96], 4, 32, gains["mqr"], n4r, True, 1),
                            headnorm(ku[:, :, 0:64], 4, 64, gains["mkn"], km[:, :, 0:64], True, 2),
                            headnorm(krv, 1, 32, gains["mkr"], n1r, True, 3)])
                    run_il([rope(n4r, 4, 32, cosm[:, i, :], sinm[:, i, :], qmv[:, :, 64:96], 0),
                            rope(n1r, 1, 32, cosm[:, i, :], sinm[:, i, :], krr[:, :].rearrange("p (h d) -> p h d", h=1), 1)])
                    fw.copy(km[:, :, 64:96], bc(krr[:, :], 1, 4), e='pool')
                    fw.copy(vmb[:, :, 0:64], ku[:, :, 64:128], e='pool')
                    fw.dma('pool', (vm[tsl, :], ("vm", i)), vmb[:, :, :].rearrange("p h d -> p (h d)"))
                    fw.tr([(psTb[0:96, hh_, :], qm[:, hh_, :], identb[:]) for hh_ in range(4)])
                    fw.copy(qTms[:], psTb[0:96, 0:4, :])
                    fw.dma('pool', (qTm[:, :, tsl].rearrange("h p t -> p h t"), ("qTm", i)), qTms[:])
                    fw.tr([(psTb[0:96, 4 + hh_, :], km[:, hh_, :], identb[:]) for hh_ in range(4)])
                    fw.copy(kTms[:], psTb[0:96, 4:8, :])
                    fw.dma('pool', (kTm[:, :, tsl].rearrange("h p t -> p h t"), ("kTm", i)), kTms[:])
                frontA(0)
                for i in range(NT):
                    if i + 1 < NT:
                        frontA(i + 1)
                    backA(i)
                fw.barrier()
            if stop_after == 'A':
                break

            with ExitStack() as stp:
                kT_all = sb(stp, "kT_all", [128, S], BF16)
                v_all = sb(stp, "v_all", [128, NT, 130], BF16)
                vm_all = sb(stp, "vm_all", [128, NT, 260], BF16)
                kTm_h = [sb(stp, f"kTm_h{i}", [96, S], BF16) for i in range(2)]
                qTt = [sb(stp, f"qTt{i}", [128, 512], BF16) for i in range(2)]
                pT = [sb(stp, f"pT{i}", [128, 2, 512], BF16) for i in range(2)]
                oTs = sb(stp, "oTs", [65, 512], F32)
                rcp = sb(stp, "rcp", [128, 4], F32)
                ob = [sb(stp, f"ob{i}", [128, 4, 64], BF16) for i in range(2)]
                psS = [ps(stp, f"psS{i}", [128, 2, 512]) for i in range(2)]
                acc = [ps(stp, f"acc{i}", [65, 512]) for i in range(2)]
                psO = ps(stp, "psO", [128, 4, 128])
                allq = [("qTg", i) for i in range(NT)]
                fw.dma('sp', kT_all[:], kTg, extra_reads=[("kTg", i) for i in range(NT)])
                fw.dma('sp', v_all[:], vg.rearrange("(t p) c -> p t c", p=128), extra_reads=[("vg", i) for i in range(NT)])
                fw.dma('sp', vm_all[:], vm.rearrange("(t p) c -> p t c", p=128), extra_reads=[("vm", i) for i in range(NT)])
                ucount = [0]

                pend = [None]

                def unit(kT_ap, qT_ap, vfn, scale, qb, col):
                    u = ucount[0]
                    ucount[0] += 1
                    ac = acc[u % 2]
                    NP = NT // 2

                    def Smm(kp):
                        pss = psS[kp % 2]
                        fw.mm([(pss[:, 0, :], kT_ap[:, (2 * kp) * 128:(2 * kp + 1) * 128], qT_ap, True, True),
                               (pss[:, 1, :], kT_ap[:, (2 * kp + 1) * 128:(2 * kp + 2) * 128], qT_ap, True, True)])
                    Smm(0)
                    if pend[0] is not None:
                        pend[0]()
                        pend[0] = None
                    for kp in range(NP):
                        if kp + 1 < NP:
                            Smm(kp + 1)
                        pt = pT[kp % 2]
                        fw.act(pt[:], psS[kp % 2][:], AF.Exp, scale=scale)
                        fw.mm([(ac[:], vfn(2 * kp), pt[:, 0, :], kp == 0, False),
                               (ac[:], vfn(2 * kp + 1), pt[:, 1, :], False, kp == NP - 1)])

                    def tail():
                        fw.copy(oTs[:], ac[:], e='dve')
                        fw.tr([(psO[:, s_, 0:65], oTs[:, s_ * 128:(s_ + 1) * 128], identf[0:65, 0:65]) for s_ in range(4)])
                        fw.recip(rcp[:], psO[:, :, 64])
                        o_ = ob[u % 2]
                        fw.tt(o_[:], psO[:, :, 0:64], bc(rcp[:, :], 2, 64), ALU.mult)
                        fw.dma('pool', (omix[qb * 512:(qb + 1) * 512, col:col + 64].rearrange("(s p) d -> p s d", p=128),
                                        ("omix", qb, col)), o_[:])
                    pend[0] = tail

                qcnt = 0
                for qb in range(NQB):
                    for j in range(3):
                        qt = qTt[qcnt % 2]
                        qcnt += 1
                        fw.dma('sp', qt[:], qTg[j, :, qb * 512:(qb + 1) * 512], extra_reads=allq)
                        for half in range(2):
                            head = j + 3 * half
                            rs = slice(half * 64, (half + 1) * 64)
                            unit(kT_all[rs, :], qt[rs, :],
                                 (lambda kt, half=half: v_all[:, kt, half * 65:(half + 1) * 65]),
                                 0.125, qb, 384 + head * 64)
                allqm = [("qTm", i) for i in range(NT)]
                for h in range(4):
                    kh = kTm_h[h % 2]
                    fw.dma('sp', kh[:], kTm[h], extra_reads=[("kTm", i) for i in range(NT)])
                    for qb in range(NQB):
                        qt = qTt[qcnt % 2]
                        qcnt += 1
                        fw.dma('sp', qt[0:96, :], qTm[h, :, qb * 512:(qb + 1) * 512], extra_reads=allqm)
                        unit(kh[:, :], qt[0:96, :], (lambda kt, h=h: vm_all[:, kt, h * 65:(h + 1) * 65]),
                             96 ** -0.5, qb, 768 + h * 64)
                if pend[0] is not None:
                    pend[0]()
                    pend[0] = None
                fw.barrier()
            if stop_after == 'B':
                break
            with ExitStack() as stp:
                wcv = sb(stp, "wcv", [128, 5, 1152], F32)
                fw.dma('sp', wcv[:], wconv_d[l].rearrange("p (j c) -> p j c", j=5))
                shf1 = [sb(stp, f"shf_{j}", [128, 1152], F32) for j in range(5)]
                shf = [shf1, shf1]
                cacc = sb(stp, "cacc", [128, 1152], F32)
                ctmp = sb(stp, "ctmp", [128, 1152], F32)
                qkvs = [sb(stp, f"qkv{i}", [128, 1152], F32) for i in range(2)]
                s12 = sb(stp, "s12", [128, 12], F32)
                qnb = sb(stp, "qnb", [128, 384], BF16)
                knb = sb(stp, "knb", [128, 384], BF16)
                kn32s = [sb(stp, f"kn32{i}", [128, 384], F32) for i in range(2)]
                qkTs = [sb(stp, f"qkT{i}", [64, 12, 128], BF16) for i in range(2)]
                gcss = [sb(stp, f"gcs{i}", [128, 24], F32) for i in range(2)]
                exs = [sb(stp, f"ex{i}", [128, 36], F32) for i in range(2)]
                Xd = sb(stp, "Xd", [128, 12, 128], F32)
                dec = sb(stp, "dec", [128, 12, 128], F32)
                Lt = sb(stp, "Lt", [128, 12, 128], F32)
                Pb = [sb(stp, f"Pb{i}", [128, 12, 128], F32) for i in range(2)]
                Qb = [sb(stp, f"Qb{i}", [128, 12, 128], F32) for i in range(2)]
                aqk = sb(stp, "aqk", [128, 12, 128], BF16)
                aqkT = sb(stp, "aqkT", [128, 12, 128], BF16)
                X32 = sb(stp, "X32", [128, 12, 128], F32)
                Wb = sb(stp, "Wb", [128, 12, 64], BF16)
                wT = sb(stp, "wT", [64, 12, 128], BF16)
                ktl = sb(stp, "ktl", [128, 12, 64], BF16)
                coef = sb(stp, "coef", [128, 12], F32)
                psA3 = ps(stp, "psA3", [128, 12, 128])
                psB3 = ps(stp, "psB3", [128, 12, 128])
                psT12f = ps(stp, "psT12", [128, 16, 128], BF16)
                psT12 = psT12f[:, 0:12, :]
                UTm, LTm = masks[:, 0, :], masks[:, 1, :]
                negm = masks[:, 2:4, :]
                strm = masks[:, 4:6, :]
                m0, m1s, m2s = masks[:, 6, :], masks[:, 7, :], masks[:, 8, :]
                bfn = {}
                for nm_ in ('L1Tb', 'L2Tb', 'T32b', 'T32Tb', 'Wz', 'T64b', 'T64Tb', 'Rb', 'Yb'):
                    bfn[nm_] = sb(stp, nm_, [128, 12, 128], BF16)

                def v4(t, w=128):
                    return t[:, :, :].rearrange("p (a h) d -> p a h d", a=2)

                def load_shift(i):
                    for j in range(5):
                        fw.dma('sp', shf[i % 2][j][:], dnpre[i * 128 + j:i * 128 + j + 128, :])
                load_shift(0)

                def frontC(i):
                    qkv, kn32, qkT, gcs, ex = qkvs[i % 2], kn32s[i % 2], qkTs[i % 2], gcss[i % 2], exs[i % 2]
                    sh = shf[i % 2]
                    tsl = slice(i * 128, (i + 1) * 128)
                    fw.tt(cacc[:], sh[0][:], wcv[:, 0, :], ALU.mult, e='pool')
                    for j in range(1, 5):
                        fw.tt(ctmp[:], sh[j][:], wcv[:, j, :], ALU.mult, e='pool')
                        fw.tt(cacc[:], cacc[:], ctmp[:], ALU.add)
                    if i + 1 < NT:
                        load_shift(i + 1)
                    fw.act(ctmp[:], cacc[:], AF.Exp, scale=-1.0)
                    fw.act(ctmp[:], ctmp[:], AF.Ln, bias=oneT[:], scale=1.0)
                    fw.act(ctmp[:], ctmp[:], AF.Exp, scale=-1.0)
                    fw.tt(qkv[:], cacc[:], ctmp[:], ALU.mult)
                    qk12 = qkv[:, 0:768].rearrange("p (h d) -> p h d", h=12)
                    c12 = cacc[:, 0:768].rearrange("p (h d) -> p h d", h=12)
                    fw.tt(c12, qk12, qk12, ALU.mult, e='pool')
                    fw.red(s12[:], c12)
                    fw.act(s12[:], s12[:], AF.Ln, bias=epsT[:], scale=1.0)
                    fw.act(s12[:], s12[:], AF.Exp, scale=-0.5)
                    fw.ts(s12[:, 0:6], s12[:, 0:6], 0.125, None, ALU.mult)
                    fw.tt(qnb[:, :].rearrange("p (h d) -> p h d", h=6), qk12[:, 0:6, :], bc(s12[:, 0:6], 2, 64), ALU.mult)
                    fw.tt(kn32[:, :].rearrange("p (h d) -> p h d", h=6), qk12[:, 6:12, :], bc(s12[:, 6:12], 2, 64), ALU.mult)
                    fw.copy(knb[:], kn32[:], e='pool')
                    fw.tr([(psT12[0:64, h, :], qnb[:, h * 64:(h + 1) * 64], identb[:]) for h in range(6)] +
                          [(psT12[0:64, 6 + h, :], knb[:, h * 64:(h + 1) * 64], identb[:]) for h in range(6)])
                    fw.copy(qkT[:], psT12[0:64, :, :], e='act')
                    fw.dma('pool', dQT[i].rearrange("p (h t) -> p h t", h=6), qkT[:, 0:6, :])
                    g_ = gT[:, i, :]
                    psG = psB3[:, 11, 0:24]
                    fw.mm([(psB3[:, 11, 0:6], UTm, g_[:, 0:6], True, True),
                           (psB3[:, 11, 6:12], LTm, g_[:, 6:12], True, True),
                           (psB3[:, 11, 12:24], onesf[:], g_, True, True)])
                    fw.copy(gcs[:], psG)
                    fw.act(ex[:, 0:24], gcs[:], AF.Exp)
                    fw.tt(ex[:, 24:36], gcs[:, 12:24], gcs[:, 0:12], ALU.subtract)
                    fw.act(ex[:, 24:36], ex[:, 24:36], AF.Exp)
                    fw.dma('pool', dE[tsl, :], ex[:, 0:24])

                def backC(i):
                    qkv, kn32, qkT, gcs, ex = qkvs[i % 2], kn32s[i % 2], qkTs[i % 2], gcss[i % 2], exs[i % 2]
                    tsl = slice(i * 128, (i + 1) * 128)
                    fw.tt(Xd[:], bc(identf[:, :], 1, 12), bc(gcs[:, 0:12], 2, 128), ALU.mult)
                    Xf = Xd[:, :, :].rearrange("p u j -> p (u j)")
                    Af = psA3[:, :, :].rearrange("p u j -> p (u j)")
                    fw.mm([(Af[:, c * 512:(c + 1) * 512], onesf[:], Xf[:, c * 512:(c + 1) * 512], True, True) for c in range(3)])
                    fw.tt(dec[:], bc(gcs[:, 0:12], 2, 128), psA3[:], ALU.subtract)
                    fw.tt(v4(dec), v4(dec), bc(negm, 2, 6), ALU.add)
                    fw.act(dec[:], dec[:], AF.Exp)
                    fw.mm([(psB3[:, h, :], qkT[:, 6 + h, :], qkT[:, 6 + h, :], True, True) for h in range(6)] +
                          [(psB3[:, 6 + h, :], qkT[:, h, :], qkT[:, 6 + h, :], True, True) for h in range(6)])
                    fw.tt(v4(Lt), v4(dec), bc(psB3[:, 0:6, :], 1, 2), ALU.mult)
                    fw.tt(v4(aqk), v4(dec), bc(psB3[:, 6:12, :], 1, 2), ALU.mult)
                    fw.stt(Xd[:], Lt[:], -1.0, bc(betaT[:, i, :], 2, 128), ALU.mult, ALU.mult)
                    fw.tr([(psB3[:, u, :], Xd[:, u, :], identf[:]) for u in range(12)])
                    fw.tt(Pb[0][:], Xd[:], bc(m0, 1, 12), ALU.mult, e='pool')
                    fw.tt(Qb[0][:], psB3[:], bc(m0, 1, 12), ALU.mult)
                    fw.tt(bfn['L1Tb'][:], psB3[:], bc(m1s, 1, 12), ALU.mult)
                    fw.tt(bfn['L2Tb'][:], psB3[:], bc(m2s, 1, 12), ALU.mult)
                    fw.tr([(psT12[:, u, :], aqk[:, u, :], identb[:]) for u in range(12)])
                    fw.copy(aqkT[:], psT12, e='act')
                    fw.dma('pool', dAT[i].rearrange("p (u t) -> p u t", u=12), aqkT[:])
                    fw.tt(coef[:], betaT[:, i, :], ex[:, 0:12], ALU.mult)
                    X4 = v4(bfn['Rb'])
                    v6 = qkv[:, 768:1152].rearrange("p (h d) -> p h d", h=6)
                    k6 = kn32[:, :].rearrange("p (h d) -> p h d", h=6)
                    b4 = betaT[:, i, :].rearrange("p (a h) -> p a h", a=2)
                    c4 = coef[:, :].rearrange("p (a h) -> p a h", a=2)
                    e4 = ex[:, 24:36].rearrange("p (a h) -> p a h", a=2)
                    fw.tt(X4[:, :, :, 0:64], bc(v6, 1, 2), bc(b4, 3, 64), ALU.mult)
                    fw.tt(X4[:, :, :, 64:128], bc(k6, 1, 2), bc(c4, 3, 64), ALU.mult, e='pool')
                    fw.tt(ktl[:, :, :].rearrange("p (a h) d -> p a h d", a=2), bc(k6, 1, 2), bc(e4, 3, 64), ALU.mult, e='pool')
                    fw.dma('pool', dKT[tsl, :], ktl[:, :, :].rearrange("p u d -> p (u d)"))
                    fw.tt(Lt[:], Pb[0][:], bc(identf[:, :], 1, 12), ALU.add)
                    for k in range(4):
                        P_, Q_ = Pb[k % 2], Qb[k % 2]
                        Pn, Qn = Pb[(k + 1) % 2], Qb[(k + 1) % 2]
                        fw.mm([(psA3[:, u, :], P_[:, u, :], Q_[:, u, :], True, True) for u in range(12)])
                        fw.copy(Qn[:], psA3[:], e='act')
                        if k < 3:
                            fw.mm([(psB3[:, u, :], Q_[:, u, :], P_[:, u, :], True, True) for u in range(12)])
                            fw.copy(Pn[:], psB3[:], e='act')
                        fw.mm([(psA3[:, u, :], Qn[:, u, :], Lt[:, u, :], True, True) for u in range(12)])
                        if k < 3:
                            fw.tt(Lt[:], Lt[:], psA3[:], ALU.add)
                        else:
                            fw.tt(bfn['T32b'][:], Lt[:], psA3[:], ALU.add)
                    fw.tr([(psT12[:, u, :], bfn['T32b'][:, u, :], identb[:]) for u in range(12)])
                    fw.copy(bfn['T32Tb'][:], psT12, e='act')
                    fw.mm([(psA3[:, u, :], bfn['L1Tb'][:, u, :], bfn['T32b'][:, u, :], True, True) for u in range(12)])
                    fw.copy(bfn['Wz'][:], psA3[:], e='act')
                    fw.mm([(psB3[:, u, :], bfn['T32Tb'][:, u, :], bfn['Wz'][:, u, :], True, True) for u in range(12)])
                    fw.tt(bfn['T64b'][:], bfn['T32b'][:], psB3[:], ALU.subtract)
                    fw.tr([(psT12[:, u, :], bfn['T64b'][:, u, :], identb[:]) for u in range(12)])
                    fw.copy(bfn['T64Tb'][:], psT12, e='act')
                    fw.mm([(psA3[:, u, :], bfn['T64Tb'][:, u, :], bfn['Rb'][:, u, :], True, True) for u in range(12)])
                    fw.copy(dec[:], psA3[:], e='act')
                    fw.copy(bfn['Yb'][:], dec[:], e='act')
                    fw.mm([(psB3[:, u, :], bfn['L2Tb'][:, u, :], bfn['Yb'][:, u, :], True, True) for u in range(12)])
                    fw.copy(bfn['Wz'][:], psB3[:], e='act')
                    fw.mm([(psA3[:, u, :], bfn['T64Tb'][:, u, :], bfn['Wz'][:, u, :], True, True) for u in range(12)])
                    fw.tt(X32[:], dec[:], psA3[:], ALU.subtract)
                    fw.dma('pool', dU[tsl, :].rearrange("p (u d) -> p u d", u=12), X32[:, :, 0:64])
                    fw.copy(Wb[:], X32[:, :, 64:128], e='act')
                    fw.tr([(psT12[0:64, u, :], Wb[:, u, :], identb[:]) for u in range(12)])
                    fw.copy(wT[:], psT12[0:64, :, :], e='act')
                    fw.dma('pool', dWT[i].rearrange("p (u t) -> p u t", u=12), wT[:])
                frontC(0)
                for i in range(NT):
                    if i + 1 < NT:
                        frontC(i + 1)
                    backC(i)
                fw.barrier()

            with ExitStack() as stp:
                S32 = [sb(stp, f"S32_{d}", [64, 6, 64], F32) for d in range(2)]
                Sb = [sb(stp, f"Sb_{d}", [64, 6, 64], BF16) for d in range(2)]
                for d in range(2):
                    fw.memset(S32[d][:], 0.0)
                    fw.memset(Sb[d][:], 0.0)
                Ud = [[sb(stp, f"Ud{d}{b}", [128, 6, 64], F32) for b in range(2)] for d in range(2)]
                WTd = [[sb(stp, f"WTd{d}{b}", [64, 6, 128], BF16) for b in range(2)] for d in range(2)]
                ATd = [[sb(stp, f"ATd{d}{b}", [128, 6, 128], BF16) for b in range(2)] for d in range(2)]
                KTd = [[sb(stp, f"KTd{d}{b}", [128, 6, 64], BF16) for b in range(2)] for d in range(2)]
                QTd = [[sb(stp, f"QTd{d}{b}", [64, 6, 128], BF16) for b in range(2)] for d in range(2)]
                Ed = [[sb(stp, f"Ed{d}{b}", [128, 24], F32) for b in range(2)] for d in range(2)]
                vnew = [sb(stp, f"vnew{d}", [128, 6, 64], BF16) for d in range(2)]
                ot = [sb(stp, f"ot{d}", [128, 6, 64], F32) for d in range(2)]
                od = [sb(stp, f"od{d}", [128, 6, 64], F32) for d in range(2)]
                stmp = [sb(stp, f"stmp{d}", [64, 6, 64], F32) for d in range(2)]
                psV = [ps(stp, f"psV{d}", [128, 8, 64])[:, 0:6, :] for d in range(2)]
                psO1 = [ps(stp, f"psO1{d}", [128, 8, 64])[:, 0:6, :] for d in range(2)]
                psO2 = [ps(stp, f"psO2{d}", [128, 8, 64])[:, 0:6, :] for d in range(2)]
                psS = [ps(stp, f"psS{d}", [64, 8, 64])[:, 0:6, :] for d in range(2)]

                def load_scan(s_):
                    for d in range(2):
                        n = s_ if d == 0 else NT - 1 - s_
                        b = s_ % 2
                        rows = slice(n * 128, (n + 1) * 128)
                        fw.dma('sp', Ud[d][b][:], dU[rows, d * 384:(d + 1) * 384].rearrange("p (h v) -> p h v", h=6))
                        fw.dma('sp', WTd[d][b][:], dWT[n][:, d * 768:(d + 1) * 768].rearrange("p (h t) -> p h t", h=6))
                        fw.dma('sp', ATd[d][b][:], dAT[n][:, d * 768:(d + 1) * 768].rearrange("p (h t) -> p h t", h=6))
                        fw.dma('sp', KTd[d][b][:], dKT[rows, d * 384:(d + 1) * 384].rearrange("p (h v) -> p h v", h=6))
                        fw.dma('sp', QTd[d][b][:], dQT[n].rearrange("p (h t) -> p h t", h=6))
                        fw.dma('sp', Ed[d][b][:], dE[rows, :])
                load_scan(0)
                for s_ in range(NT):
                    if s_ + 1 < NT:
                        load_scan(s_ + 1)
                    b = s_ % 2
                    ns = [s_, NT - 1 - s_]
                    for d in range(2):
                        fw.tt(stmp[d][:], S32[d][:], bc(Ed[d][b][0:64, 12 + d * 6:18 + d * 6], 2, 64), ALU.mult)
                    for d in range(2):
                        fw.mm([(psV[d][:, h, :], WTd[d][b][:, h, :], Sb[d][:, h, :], True, True) for h in range(6)] +
                              [(psO1[d][:, h, :], QTd[d][b][:, h, :], Sb[d][:, h, :], True, True) for h in range(6)])
                    for d in range(2):
                        fw.tt(vnew[d][:], Ud[d][b][:], psV[d][:], ALU.subtract)
                    for d in range(2):
                        fw.mm([(psS[d][:, h, :], KTd[d][b][:, h, :], vnew[d][:, h, :], True, True) for h in range(6)] +
                              [(psO2[d][:, h, :], ATd[d][b][:, h, :], vnew[d][:, h, :], True, True) for h in range(6)])
                    for d in range(2):
                        fw.tt(S32[d][:], stmp[d][:], psS[d][:], ALU.add)
                        fw.copy(Sb[d][:], S32[d][:], e='act')
                    for d in range(2):
                        fw.tt(ot[d][:], psO1[d][:], bc(Ed[d][b][:, d * 6:d * 6 + 6], 2, 64), ALU.mult)
                        fw.tt(od[d][:], ot[d][:], psO2[d][:], ALU.add)
                        fw.dma('pool', dO[d, ns[d] * 128:(ns[d] + 1) * 128, :], od[d][:, :, :].rearrange("p h v -> p (h v)"))
                fw.barrier()

            with ExitStack() as stp:
                gdn = sb(stp, "gdn", [128, 64], F32)
                fw.dma('sp', gdn[:], dng_d[l])
                o0 = [sb(stp, f"o0_{b}", [128, 384], F32) for b in range(2)]
                o1 = [sb(stp, f"o1_{b}", [128, 384], F32) for b in range(2)]
                zt_ = [sb(stp, f"zt_{b}", [128, 384], F32) for b in range(2)]
                osum = sb(stp, "osum", [128, 384], F32)
                otmp = sb(stp, "otmp", [128, 384], F32)
                ze = sb(stp, "ze", [128, 384], F32)
                s6c = sb(stp, "s6c", [128, 6], F32)
                oab = sb(stp, "oab", [128, 384], BF16)

                def load_c3(i):
                    fw.dma('sp', o0[i % 2][:], dO[0, i * 128:(i + 1) * 128, :])
                    fw.dma('sp', o1[i % 2][:], dO[1, i * 128:(i + 1) * 128, :])
                    fw.dma('sp', zt_[i % 2][:], dnz[i * 128:(i + 1) * 128, :])
                load_c3(0)
                for i in range(NT):
                    if i + 1 < NT:
                        load_c3(i + 1)
                    b = i % 2
                    fw.tt(osum[:], o0[b][:], o1[b][:], ALU.add)
                    o6 = osum[:, :].rearrange("p (h d) -> p h d", h=6)
                    t6 = otmp[:, :].rearrange("p (h d) -> p h d", h=6)
                    fw.tt(t6, o6, o6, ALU.mult)
                    fw.red(s6c[:], t6)
                    fw.act(s6c[:], s6c[:], AF.Ln, bias=epsT[:], scale=1.0 / 64)
                    fw.act(s6c[:], s6c[:], AF.Exp, scale=-0.5)
                    fw.tt(t6, o6, bc(s6c[:, :], 2, 64), ALU.mult)
                    fw.tt(t6, t6, bc(gdn[:, :], 1, 6), ALU.mult)
                    fw.act(ze[:], zt_[b][:], AF.Exp, scale=-1.0)
                    fw.ts(ze[:], ze[:], 1.0, None, ALU.add, e='pool')
                    fw.recip(ze[:], ze[:])
                    fw.tt(ze[:], ze[:], zt_[b][:], ALU.mult)
                    fw.tt(oab[:], otmp[:], ze[:], ALU.mult)
                    fw.dma('pool', omix[i * 128:(i + 1) * 128, 0:384], oab[:])
                fw.barrier()
            if stop_after == 'C':
                break

            with ExitStack() as stp:
                wo = sb(stp, "wo", [128, 8, D], BF16)
                wos = [sb(stp, f"wos{i}", [128, D], F32) for i in range(2)]
                for k in range(8):
                    fw.dma('sp', wos[k % 2][:], w_out[l][k * 128:(k + 1) * 128, :])
                    fw.copy(wo[:, k, :], wos[k % 2][:], e=('pool' if k % 2 else 'dve'))
                wr = sb(stp, "wr", [128, 8, 36], F32)
                br = sb(stp, "br", [128, 36], F32)
                fw.dma('sp', wr[:], w_r[l].rearrange("(k p) n -> p k n", p=128))
                fw.dma('sp', br[:], b_r[l])
                om = [sb(stp, f"om{i}", [128, D], BF16) for i in range(2)]
                xd = [sb(stp, f"xd{i}", [128, D], F32) for i in range(2)]
                oT = sb(stp, "oT", [128, 8, 128], BF16)
                x1 = sb(stp, "x1", [128, D], F32)
                dtmp = sb(stp, "dtmp", [128, D], F32)
                h2 = sb(stp, "h2", [128, D], F32)
                h2T32 = sb(stp, "h2T32", [128, 8, 128], F32)
                h2Tb = sb(stp, "h2Tb", [128, 8, 128], BF16)
                ssd = sb(stp, "ssd", [128, 1], F32)
                lg = sb(stp, "lg", [128, 36], F32)
                r1 = [sb(stp, f"r1_{i}", [128, 1], F32) for i in range(8)]
                ohg = sb(stp, "ohg", [128, 4], F32)
                eg4 = sb(stp, "eg4", [128, 4], F32)
                t32 = sb(stp, "t32", [128, 32], F32)
                esel = sb(stp, "esel", [128, 8], F32)
                es2 = sb(stp, "es2", [128, 8], F32)
                mk1 = sb(stp, "mk1", [128, 8], F32)
                mk2 = sb(stp, "mk2", [128, 8], F32)
                ge = sb(stp, "ge", [128, 8], F32)
                Gt = sb(stp, "Gt", [128, 32], F32)
                psTb2 = ps(stp, "psTb2", [128, 8, 128], BF16)
                psY2 = ps(stp, "psY2", [128, D])
                psW2 = ps(stp, "psW2", [128, 8, 128])
                psR = ps(stp, "psR", [128, 512])[:, 0:36]

                def load_d(i):
                    fw.dma('sp', om[i % 2][:], omix[i * 128:(i + 1) * 128, :])
                    fw.dma('sp', xd[i % 2][:], x_src[i * 128:(i + 1) * 128, :])
                load_d(0)
                for i in range(NT):
                    if i + 1 < NT:
                        load_d(i + 1)
                    tsl = slice(i * 128, (i + 1) * 128)
                    o_, x_ = om[i % 2], xd[i % 2]
                    fw.tr([(psTb2[:, k, :], o_[:, k * 128:(k + 1) * 128], identb[:]) for k in range(8)])
                    fw.copy(oT[:], psTb2[:], e='act')
                    fw.mm([(psY2[:, 0:512], oT[:, k, :], wo[:, k, 0:512], k == 0, k == 7) for k in range(8)] +
                          [(psY2[:, 512:1024], oT[:, k, :], wo[:, k, 512:1024], k == 0, k == 7) for k in range(8)])
                    fw.tt(dtmp[:], psY2[:], gt1, ALU.mult)
                    fw.tt(x1[:], dtmp[:], x_[:], ALU.add)
                    if not os.environ.get('SKIP_XS0'):
                        fw.dma('pool', xs[0][tsl, :], x1[:])
                    fw.act(dtmp[:], x1[:], AF.Square, accum_out=ssd[:])
                    fw.act(ssd[:], ssd[:], AF.Ln, bias=epsT[:], scale=1.0 / D)
                    fw.act(ssd[:], ssd[:], AF.Exp, scale=-0.5)
                    fw.stt(h2[:], x1[:], ssd[:, 0:1], A2[:], ALU.mult, ALU.mult)
                    fw.tt(h2[:], h2[:], sh2, ALU.add)
                    fw.tr([(psW2[:, k, :], h2[:, k * 128:(k + 1) * 128], identf[:]) for k in range(8)])
                    fw.copy(h2T32[:], psW2[:])
                    fw.copy(h2Tb[:], psW2[:], e='act')
                    if not os.environ.get('SKIP_H2TD'):
                        fw.dma('pool', h2Td[:, :, tsl], h2Tb[:])
                    if os.environ.get("SKIP_ROUTER"):
                        continue
                    fw.mmk(psR[:], [(h2T32[:, k, :], wr[:, k, :]) for k in range(8)])
                    fw.tt(lg[:], psR[:], br[:], ALU.add)
                    gm, ngm, sume, gtp, m1, m2, dd, w1_ = r1
                    fw.red(gm[:], lg[:, 0:4], op=ALU.max)
                    fw.ts(ohg[:], lg[:, 0:4], gm[:, 0:1], None, ALU.is_equal)
                    fw.ts(ngm[:], gm[:], -1.0, None, ALU.mult)
                    fw.act(eg4[:], lg[:, 0:4], AF.Exp, bias=ngm[:], scale=1.0, accum_out=sume[:])
                    fw.recip(gtp[:], sume[:])
                    fw.tt(t32[:, :].rearrange("p (g e) -> p g e", g=4), lg[:, 4:36].rearrange("p (g e) -> p g e", g=4),
                          bc(ohg[:, :], 2, 8), ALU.mult)
                    fw.red(esel[:], t32[:, :].rearrange("p (g e) -> p e g", g=4))
                    fw.red(m1[:], esel[:], op=ALU.max)
                    fw.ts(mk1[:], esel[:], m1[:, 0:1], None, ALU.is_equal)
                    fw.stt(es2[:], mk1[:], -1e30, esel[:], ALU.mult, ALU.add)
                    fw.red(m2[:], es2[:], op=ALU.max)
                    fw.ts(mk2[:], es2[:], m2[:, 0:1], None, ALU.is_equal)
                    fw.tt(dd[:], m2[:], m1[:], ALU.subtract)
                    fw.act(dd[:], dd[:], AF.Exp)
                    fw.ts(w1_[:], dd[:], 1.0, None, ALU.add)
                    fw.recip(w1_[:], w1_[:])
                    fw.tt(dd[:], dd[:], w1_[:], ALU.mult)
                    fw.tt(w1_[:], w1_[:], gtp[:], ALU.mult)
                    fw.tt(dd[:], dd[:], gtp[:], ALU.mult)
                    fw.ts(ge[:], mk1[:], w1_[:, 0:1], None, ALU.mult)
                    fw.stt(ge[:], mk2[:], dd[:, 0:1], ge[:], ALU.mult, ALU.add)
                    fw.tt(Gt[:, :].rearrange("p (g e) -> p g e", g=4), bc(ohg[:, :], 2, 8), bc(ge[:, :], 1, 4), ALU.mult)
                    fw.dma('pool', Gd[tsl, :], Gt[:])
                fw.barrier()

            if stop_after == 'D':
                break
            SBK = min(S, 2048)
            TPB = SBK // 128
            NB = SBK // 512
            with ExitStack() as stp:
                h2Ts = sb(stp, "h2Ts", [128, 8, SBK], BF16)
                yacc = sb(stp, "yacc", [128, TPB, D], F32)
                Gs = sb(stp, "Gs", [128, TPB, 32], F32)
                w1b = [sb(stp, f"w1b_{i}", [128, 8, 256], BF16) for i in range(2)]
                w3b = [sb(stp, f"w3b_{i}", [128, 8, 256], BF16) for i in range(2)]
                w2b = [sb(stp, f"w2b_{i}", [128, 2, D], BF16) for i in range(2)]
                st1 = sb(stp, "st1", [128, 8, 256], F32)
                st3 = sb(stp, "st3", [128, 8, 256], F32)
                st2 = sb(stp, "st2", [128, 2, D], F32)
                e1_ = [sb(stp, f"e1_{i}", [128, 512], F32) for i in range(2)]
                p_ = [sb(stp, f"p_{i}", [128, 512], F32) for i in range(2)]
                hidT = [sb(stp, f"hidT{i}", [128, 2, 512], BF16) for i in range(2)]
                xe1 = st1[:, 0:4, :].rearrange("p k n -> p (k n)")
                xo1 = st3[:, 0:4, :].rearrange("p k n -> p (k n)")
                psH1 = [ps(stp, f"psH1{i}", [128, 512]) for i in range(2)]
                psH3 = [ps(stp, f"psH3{i}", [128, 512]) for i in range(2)]
                psY = [ps(stp, f"psY{i}", [128, D]) for i in range(2)]
                bcount = 0
                ycount = [0]
                pendE = [None]
                for sbk in range(S // SBK):
                    t0 = sbk * SBK
                    fw.dma('sp', h2Ts[:], h2Td[:, :, t0:t0 + SBK])
                    fw.dma('sp', Gs[:], Gd[t0:t0 + SBK, :].rearrange("(t p) e -> p t e", p=128))
                    for e_ in range(32):
                        wa1, wa3, wb2 = w1b[e_ % 2], w3b[e_ % 2], w2b[e_ % 2]
                        fw.dma('sp', st1[:], moe_w1[l, e_].rearrange("(k p) n -> p k n", p=128))
                        fw.dma('sp', st3[:], moe_w3[l, e_].rearrange("(k p) n -> p k n", p=128))
                        fw.dma('sp', st2[:], moe_w2[l, e_].rearrange("(k p) n -> p k n", p=128))
                        fw.copy(wa1[:], st1[:], e='pool')
                        fw.copy(wa3[:], st3[:], e='pool')
                        fw.copy(wb2[:], st2[:], e='pool')
                        for b in range(NB):
                            hT_ = hidT[bcount % 2]
                            bcount += 1
                            tk = slice(b * 512, (b + 1) * 512)
                            for c in range(2):
                                fs = slice(c * 128, (c + 1) * 128)
                                fw.mm([(psH1[c][:], wa1[:, k, fs], h2Ts[:, k, tk], k == 0, k == 7) for k in range(8)] +
                                      [(psH3[c][:], wa3[:, k, fs], h2Ts[:, k, tk], k == 0, k == 7) for k in range(8)])
                                fw.act(e1_[c][:], psH1[c][:], AF.Exp, scale=-1.0)
                                fw.act(e1_[c][:], e1_[c][:], AF.Ln, bias=oneT[:], scale=1.0)
                                fw.act(e1_[c][:], e1_[c][:], AF.Exp, scale=-1.0)
                                fw.tt(p_[c][:], psH1[c][:], e1_[c][:], ALU.mult)
                                fw.tt(hT_[:, c, :], p_[c][:], psH3[c][:], ALU.mult)
                                if pendE[0] is not None:
                                    pendE[0](c)
                                    if c == 1:
                                        pendE[0] = None

                            def mk(bb=b, hh=hT_, ee=e_, wb=wb2):
                                def f(half):
                                    for t4 in ((0, 1, 2, 3) if half is None else (2 * half, 2 * half + 1)):
                                        t = bb * 4 + t4
                                        py = psY[ycount[0] % 2]
                                        ycount[0] += 1
                                        ts4 = slice(t4 * 128, (t4 + 1) * 128)
                                        fw.mm([(py[:, 0:512], hh[:, j, ts4], wb[:, j, 0:512], j == 0, j == 1) for j in range(2)] +
                                              [(py[:, 512:1024], hh[:, j, ts4], wb[:, j, 512:1024], j == 0, j == 1) for j in range(2)])
                                        if ee == 0:
                                            fw.ts(yacc[:, t, :], py[:], Gs[:, t, ee:ee + 1], None, ALU.mult)
                                        else:
                                            fw.stt(yacc[:, t, :], py[:], Gs[:, t, ee:ee + 1], yacc[:, t, :], ALU.mult, ALU.add)
                                return f
                            pendE[0] = mk()
                    if pendE[0] is not None:
                        pendE[0](None)
                        pendE[0] = None
                    for t in range(TPB):
                        rows = slice(t0 + t * 128, t0 + (t + 1) * 128)
                        fw.dma('sp', xe1, xs[0][rows, :])
                        fw.tt(xo1, yacc[:, t, :], gt2, ALU.mult, e='pool')
                        fw.tt(xo1, xo1, xe1, ALU.add)
                        fw.dma('pool', x_dst[rows, :], xo1)
                fw.barrier()

        if dbg and os.environ.get("DBG_DN"):
            with ExitStack() as stp:
                t_f = sb(stp, "dbgdn_f", [128, D], F32)
                fw.memset(t_f[:], 0.0)
                for i in range(NT):
                    rows = slice(i * 128, (i + 1) * 128)
                    if os.environ.get("DBG_DN") == "U":
                        fw.dma('sp', t_f[:, 0:768], dU[rows, :])
                    else:
                        fw.dma('sp', t_f[:, 0:384], dO[0, rows, :])
                        fw.dma('sp', t_f[:, 384:768], dO[1, rows, :])
                    fw.dma('sp', t_f[:, 768:792], dE[rows, :])
                    fw.dma('sp', t_f[:, 800:812], gT[:, i, :])
                    fw.dma('sp', t_f[:, 812:824], betaT[:, i, :])
                    fw.dma('sp', dbg_out[rows, :], t_f[:])
                fw.barrier()
        elif dbg and stop_after in ('B', 'C'):
            with ExitStack() as stp:
                t_b = sb(stp, "dbg_b", [128, D], BF16)
                t_f = sb(stp, "dbg_f", [128, D], F32)
                for i in range(NT):
                    fw.dma('sp', t_b[:], omix[i * 128:(i + 1) * 128, :],
                           extra_reads=[("omix", qb, col) for qb in range(NQB) for col in range(384, 1024, 64)])
                    fw.copy(t_f[:], t_b[:])
                    fw.dma('sp', dbg_out[i * 128:(i + 1) * 128, :], t_f[:])
                fw.barrier()
        fw.barrier()
        print("instructions emitted:", fw.ninst)
    return nc


def rope_tables(S, rot_dim, grid_w=64):
    t = np.arange(S)
    row = (t // grid_w).astype(np.float32)
    col = (t % grid_w).astype(np.float32)
    n_freq = rot_dim // 4
    inv = (10000.0 ** (-np.arange(n_freq, dtype=np.float32) / n_freq)).astype(np.float32)
    ang = np.concatenate([row[:, None] * inv, col[:, None] * inv], axis=-1).astype(np.float32)
    return np.cos(ang).astype(np.float32), np.sin(ang).astype(np.float32)


def rep(v, n=128):
    v = np.asarray(v, np.float32)
    return np.ascontiguousarray(np.broadcast_to(v[..., None, :], v.shape[:-1] + (n, v.shape[-1])))


def prep_inputs(inp, S, depth):
    NT = S // 128
    f = lambda a: np.ascontiguousarray(np.asarray(a, np.float32))
    perm = np.arange(INC)
    base = 1560
    order = [0, 3, 1, 4, 2, 5]
    perm[base:base + 384] = np.concatenate([base + h * 64 + np.arange(64) for h in order])
    com = {}
    com["ada_w"] = f(inp["ada_w"][:depth])
    com["ada_b"] = rep(inp["ada_b"][:depth])
    com["g1"] = rep(inp["norm1_g"][:depth])
    com["g2"] = rep(inp["norm2_g"][:depth])
    com["w_in"] = f(np.asarray(inp["w_in"])[:depth][:, :, perm])
    com["w_out"] = f(inp["w_out"][:depth])
    com["gq_g"] = rep(inp["gqa_q_g"][:depth])
    com["gk_g"] = rep(inp["gqa_k_g"][:depth])
    com["mql_g"] = f(np.asarray(inp["mla_q_lat_g"])[:depth, :, None])
    com["mkvl_g"] = f(np.asarray(inp["mla_kv_lat_g"])[:depth, :, None])
    com["w_uq"] = f(inp["mla_w_uq"][:depth])
    com["w_ukv"] = f(inp["mla_w_ukv"][:depth])
    com["mqn_g"] = rep(inp["mla_qn_g"][:depth])
    com["mqr_g"] = rep(inp["mla_qr_g"][:depth])
    com["mkn_g"] = rep(inp["mla_kn_g"][:depth])
    com["mkr_g"] = rep(inp["mla_kr_g"][:depth])
    cg, sg = rope_tables(S, 64)
    cm, sm = rope_tables(S, 32)
    tm = lambda a: np.ascontiguousarray(a.reshape(NT, 128, -1).transpose(1, 0, 2))
    com["cosg"], com["sing"], com["cosm"], com["sinm"] = tm(cg), tm(sg), tm(cm), tm(sm)
    com["identf"] = np.eye(128, dtype=np.float32)
    com["w_r"] = f(np.concatenate([np.asarray(inp["moe_w_group"])[:depth], np.asarray(inp["moe_w_router"])[:depth]], axis=-1))
    com["b_r"] = rep(np.concatenate([np.asarray(inp["moe_b_group"])[:depth], np.asarray(inp["moe_b_router"])[:depth]], axis=-1))
    com["moe_w1"] = f(inp["moe_w1"][:depth])
    com["moe_w3"] = f(inp["moe_w3"][:depth])
    com["moe_w2"] = f(inp["moe_w2"][:depth])
    com["wconv"] = rep(np.asarray(inp["dn_conv"])[:depth].reshape(depth, 5 * 1152))
    com["alog"] = rep(np.asarray(inp["dn_a_log"])[:depth].reshape(depth, 12))
    com["dtb"] = rep(np.asarray(inp["dn_dt_bias"])[:depth].reshape(depth, 12))
    com["dng"] = rep(inp["dn_out_g"][:depth])
    ii = np.arange(128)[:, None]
    jj = np.arange(128)[None, :]
    NEG = -30000.0
    mk = np.stack([(ii <= jj), (ii >= jj),
                   np.where(jj <= ii, 0.0, NEG), np.where(jj >= ii, 0.0, NEG),
                   (jj < ii), (jj > ii),
                   (ii // 32 == jj // 32) & (ii != jj), (ii // 64 == jj // 64) & (ii // 32 != jj // 32), (ii // 64 != jj // 64)],
                  axis=1).astype(np.float32)
    mk[:, 7:9, :] *= -1.0
    com["masks"] = np.ascontiguousarray(mk)
    return com


def kernel(**inp):
    S, depth = 4096, 4
    com = prep_inputs(inp, S, depth)
    x = np.asarray(inp["x"], np.float32)
    c = np.asarray(inp["c"], np.float32)
    nc = build(S, depth)
    maps = []
    for core in range(8):
        b = core % 4
        m = dict(com)
        m["x"] = np.ascontiguousarray(x[b])
        m["c_pk"] = np.ascontiguousarray(c[b].reshape(8, 128).T)
        maps.append(m)
    res = run_bass_kernel_spmd(nc, maps, core_ids=list(range(8)))
    return np.stack([res.results[b]["y"] for b in range(4)], axis=0).astype(np.float32)
```

```python
import math
import os
import numpy as np
from contextlib import ExitStack
import concourse.bass as bass
import concourse.mybir as mybir
from concourse.bass_utils import run_bass_kernel_spmd

F32 = mybir.dt.float32
BF16 = mybir.dt.bfloat16
I32 = mybir.dt.int32
AF = mybir.ActivationFunctionType
ALU = mybir.AluOpType
AX = mybir.AxisListType
NDS = 20
import re
PSUM_RE = re.compile(r'p\d+_')

D = 1024
INC = 2552
EPS = 1e-6


class FW:
    def __init__(self, nc, stack):
        self.nc = nc
        self.stack = stack
        self.eng = {'pe': nc.tensor, 'act': nc.scalar, 'dve': nc.vector, 'pool': nc.gpsimd, 'sp': nc.sync}
        self.sem = {}
        self.cnt = {}
        for e in self.eng:
            self.sem[('c', e)] = stack.enter_context(nc.semaphore('s_' + e))
            self.cnt[('c', e)] = 0
        self.dring = {}
        self.dnext = {}
        for q in ('sp', 'act', 'pool'):
            self.dring[q] = []
            for i in range(NDS):
                k = ('d', q, i)
                self.sem[k] = stack.enter_context(nc.semaphore(f'd_{q}_{i}'))
                self.cnt[k] = 0
                self.dring[q].append(k)
            self.dnext[q] = 0
        self.waited = {e: {} for e in self.eng}
        self.W = {}
        self.R = {}
        self.ninst = 0

    @staticmethod
    def _res(x):
        if isinstance(x, tuple):
            if len(x) == 2 and not isinstance(x[0], (str, int)):
                return x[1]
            return x
        if isinstance(x, str):
            return x
        return x.name

    @staticmethod
    def ap(x):
        if isinstance(x, tuple):
            return x[0]
        return x

    def _wait(self, e, toks):
        eng = self.eng[e]
        for k, v in toks.items():
            if self.waited[e].get(k, 0) < v:
                eng.wait_ge(self.sem[k], v)
                self.waited[e][k] = v
                self.ninst += 1

    def _deps(self, reads, writes):
        toks = {}

        def add(d):
            for k, v in d.items():
                if toks.get(k, 0) < v:
                    toks[k] = v
        for r in reads:
            add(self.W.get(r, {}))
        for w in writes:
            add(self.W.get(w, {}))
            add(self.R.get(w, {}))
        return toks

    def _commit(self, tok, reads, writes):
        k, v = tok
        for r in reads:
            d = self.R.setdefault(r, {})
            d[k] = max(d.get(k, 0), v)
        for w in writes:
            self.W[w] = {k: v}
            self.R[w] = {}

    def op(self, e, fn, reads, writes):
        reads = [self._res(r) for r in reads if r is not None and not isinstance(r, (int, float))]
        writes = [self._res(w) for w in writes]
        writes = writes + [r for r in reads if isinstance(r, str) and PSUM_RE.match(r)]
        toks = self._deps(reads, writes)
        if e == 'pe':
            toks.pop(('c', 'pe'), None)
        self._wait(e, toks)
        ins = fn()
        k = ('c', e)
        self.cnt[k] += 1
        ins.then_inc(self.sem[k], 1)
        self.ninst += 1
        self._commit((k, self.cnt[k]), reads, writes)

    def dma(self, q, out, in_, extra_reads=(), extra_writes=()):
        reads = [self._res(in_)] + [self._res(r) for r in extra_reads]
        writes = [self._res(out)] + [self._res(w) for w in extra_writes]
        toks = self._deps(reads, writes)
        k = self.dring[q][self.dnext[q]]
        self.dnext[q] = (self.dnext[q] + 1) % NDS
        if self.cnt[k] > 0:
            toks[k] = max(toks.get(k, 0), self.cnt[k])
        self._wait(q, toks)
        ins = self.eng[q].dma_start(out=self.ap(out), in_=self.ap(in_))
        self.cnt[k] += 16
        ins.then_inc(self.sem[k], 16)
        self.ninst += 1
        self._commit((k, self.cnt[k]), reads, writes)

    def barrier(self, engines=('pe', 'act', 'dve', 'pool', 'sp')):
        toks = {k: v for k, v in self.cnt.items() if v > 0}
        for e in engines:
            self._wait(e, dict(toks))

    def act(self, out, in_, func, bias=None, scale=1.0, accum_out=None, e='act'):
        kw = {}
        if bias is not None:
            kw['bias'] = self.ap(bias)
        if accum_out is not None:
            kw['accum_out'] = self.ap(accum_out)
        sc = self.ap(scale) if not isinstance(scale, (int, float)) else scale
        wr = [out] + ([accum_out] if accum_out is not None else [])
        self.op(e, lambda: self.eng[e].activation(out=self.ap(out), in_=self.ap(in_), func=func, scale=sc, **kw),
                [in_, bias, scale], wr)

    def tt(self, out, in0, in1, op, e='dve'):
        self.op(e, lambda: self.eng[e].tensor_tensor(out=self.ap(out), in0=self.ap(in0), in1=self.ap(in1), op=op),
                [in0, in1], [out])

    def ts(self, out, in0, s1, s2, op0, op1=None, e='dve'):
        a1 = self.ap(s1) if not isinstance(s1, (int, float)) else s1
        a2 = (self.ap(s2) if not isinstance(s2, (int, float)) else s2) if s2 is not None else None
        kw = {}
        if op1 is not None:
            kw['op1'] = op1
        self.op(e, lambda: self.eng[e].tensor_scalar(out=self.ap(out), in0=self.ap(in0), scalar1=a1, scalar2=a2,
                                                     op0=op0, **kw), [in0, s1, s2], [out])

    def stt(self, out, in0, scalar, in1, op0, op1, e='dve'):
        a = self.ap(scalar) if not isinstance(scalar, (int, float)) else scalar
        self.op(e, lambda: self.eng[e].scalar_tensor_tensor(out=self.ap(out), in0=self.ap(in0), scalar=a,
                                                            in1=self.ap(in1), op0=op0, op1=op1),
                [in0, scalar, in1], [out])

    def red(self, out, in_, op=ALU.add, e='dve'):
        self.op(e, lambda: self.eng[e].tensor_reduce(out=self.ap(out), in_=self.ap(in_), axis=AX.X, op=op),
                [in_], [out])

    def recip(self, out, in_):
        self.op('dve', lambda: self.nc.vector.reciprocal(out=self.ap(out), in_=self.ap(in_)), [in_], [out])

    def copy(self, out, in_, e='dve'):
        if e == 'act':
            self.op(e, lambda: self.eng[e].copy(out=self.ap(out), in_=self.ap(in_)), [in_], [out])
        else:
            self.op(e, lambda: self.eng[e].tensor_copy(out=self.ap(out), in_=self.ap(in_)), [in_], [out])

    def memset(self, out, val, e='pool'):
        self.op(e, lambda: self.eng[e].memset(self.ap(out), val), [], [out])

    def mm(self, items, extra_reads=()):
        rd = list(extra_reads)
        wr = []
        for o, l, r, _, _ in items:
            rd += [l, r]
            wr.append(o)

        def fn():
            ins = None
            for o, l, r, st, sp in items:
                ins = self.nc.tensor.matmul(self.ap(o), self.ap(l), self.ap(r), start=st, stop=sp)
            return ins
        self.op('pe', fn, rd, wr)

    def mmk(self, out, pairs):
        n = len(pairs)
        self.mm([(out, l, r, i == 0, i == n - 1) for i, (l, r) in enumerate(pairs)])

    def tr(self, items):
        rd = []
        wr = []
        for o, i, idt in items:
            rd += [i, idt]
            wr.append(o)

        def fn():
            ins = None
            for o, i, idt in items:
                ins = self.nc.tensor.transpose(self.ap(o), self.ap(i), self.ap(idt))
            return ins
        self.op('pe', fn, rd, wr)


def bc(ap, axis, n):
    a = ap.unsqueeze(axis)
    shp = list(a.shape)
    shp[axis] = n
    return a.to_broadcast(shp)


def build(S=4096, depth=4, stop_after=None, dbg=False):
    NT = S // 128
    NQB = S // 512
    nc = bass.Bass("TRN2", target_bir_lowering=False)

    def din(name, shape, dt=F32):
        return nc.dram_tensor(name, list(shape), dt, kind="ExternalInput").ap()

    def scr(name, shape, dt):
        return nc.dram_tensor(name, list(shape), dt, kind="Internal").ap()

    x_in = din("x", [S, D])
    c_pk = din("c_pk", [128, 8])
    ada_w = din("ada_w", [depth, D, 6 * D])
    ada_b = din("ada_b", [depth, 128, 6 * D])
    g1_d = din("g1", [depth, 128, D])
    g2_d = din("g2", [depth, 128, D])
    w_in = din("w_in", [depth, D, INC])
    w_out = din("w_out", [depth, D, D])
    gq_g = din("gq_g", [depth, 128, 64])
    gk_g = din("gk_g", [depth, 128, 64])
    mql_g = din("mql_g", [depth, 192, 1])
    mkvl_g = din("mkvl_g", [depth, 128, 1])
    w_uq = din("w_uq", [depth, 192, 384])
    w_ukv = din("w_ukv", [depth, 128, 512])
    mqn_g = din("mqn_g", [depth, 128, 64])
    mqr_g = din("mqr_g", [depth, 128, 32])
    mkn_g = din("mkn_g", [depth, 128, 64])
    mkr_g = din("mkr_g", [depth, 128, 32])
    cosg_d = din("cosg", [128, NT, 32])
    sing_d = din("sing", [128, NT, 32])
    cosm_d = din("cosm", [128, NT, 16])
    sinm_d = din("sinm", [128, NT, 16])
    identf_d = din("identf", [128, 128])
    w_r = din("w_r", [depth, D, 36])
    b_r = din("b_r", [depth, 128, 36])
    moe_w1 = din("moe_w1", [depth, 32, D, 256])
    moe_w3 = din("moe_w3", [depth, 32, D, 256])
    moe_w2 = din("moe_w2", [depth, 32, 256, D])
    wconv_d = din("wconv", [depth, 128, 5 * 1152])
    alog_d = din("alog", [depth, 128, 12])
    dtb_d = din("dtb", [depth, 128, 12])
    dng_d = din("dng", [depth, 128, 64])
    masks_d = din("masks", [128, 9, 128])
    y_out = nc.dram_tensor("y", [S, D], F32, kind="ExternalOutput").ap()

    qTg = scr("qTg", [3, 128, S], BF16)
    kTg = scr("kTg", [128, S], BF16)
    vg = scr("vg", [S, 130], BF16)
    qTm = scr("qTm", [4, 96, S], BF16)
    kTm = scr("kTm", [4, 96, S], BF16)
    vm = scr("vm", [S, 260], BF16)
    omix = scr("omix", [S, D], BF16)
    xs = [scr("xs0", [S, D], F32), scr("xs1", [S, D], F32), scr("xs2", [S, D], F32)]
    h2Td = scr("h2Td", [128, 8, S], BF16)
    Gd = scr("Gd", [S, 32], F32)
    dnpre = scr("dnpre", [S + 4, 1152], F32)
    dnz = scr("dnz", [S, 384], F32)
    dU = scr("dU", [S, 768], F32)
    dWT = scr("dWT", [NT, 64, 12 * 128], BF16)
    dAT = scr("dAT", [NT, 128, 12 * 128], BF16)
    dKT = scr("dKT", [S, 768], BF16)
    dQT = scr("dQT", [NT, 64, 6 * 128], BF16)
    dE = scr("dE", [S, 24], F32)
    dO = scr("dO", [2, S, 384], F32)
    dbg_out = None
    if dbg:
        dbg_out = nc.dram_tensor("dbg", [S, D], F32, kind="ExternalOutput").ap()

    with ExitStack() as st0:
        fw = FW(nc, st0)

        uid = [0]

        def sb(stk, name, shape, dt):
            uid[0] += 1
            return stk.enter_context(nc.sbuf_tensor(f"s{uid[0]}_{name}", list(shape), dt))

        def ps(stk, name, shape, dt=F32):
            uid[0] += 1
            return stk.enter_context(nc.psum_tensor(f"p{uid[0]}_{name}", list(shape), dt))

        identf = sb(st0, "identf", [128, 128], F32)
        identb = sb(st0, "identb", [128, 128], BF16)
        epsT = sb(st0, "epsT", [128, 1], F32)
        cactB = sb(st0, "cactB", [128, 8, 128], F32)
        mod = sb(st0, "mod", [128, 6 * D], F32)
        A1 = sb(st0, "A1", [128, D], F32)
        A2 = sb(st0, "A2", [128, D], F32)
        fw.dma('sp', identf[:], identf_d)
        fw.memset(epsT[:], EPS)
        oneT = sb(st0, "oneT", [128, 1], F32)
        fw.memset(oneT[:], 1.0)
        onesf = sb(st0, "onesf", [128, 128], F32)
        fw.memset(onesf[:], 1.0)
        masks = sb(st0, "masks", [128, 9, 128], F32)
        fw.dma('sp', masks[:], masks_d)
        gT = sb(st0, "gT", [128, NT, 12], F32)
        betaT = sb(st0, "betaT", [128, NT, 12], F32)
        with ExitStack() as stp:
            zt = sb(stp, "zt", [128, 1152], F32)
            fw.memset(zt[:], 0.0)
            fw.dma('sp', dnpre[0:2, :], zt[0:2, :])
            fw.dma('sp', dnpre[S + 2:S + 4, :], zt[0:2, :])
            fw.barrier()
        fw.copy(identb[:], identf[:], e='dve')
        with ExitStack() as stp:
            ct = sb(stp, "ct", [128, 8], F32)
            ce = sb(stp, "ce", [128, 8], F32)
            fw.dma('sp', ct[:], c_pk)
            fw.act(ce[:], ct[:], AF.Exp, scale=-1.0)
            fw.ts(ce[:], ce[:], 1.0, None, ALU.add)
            fw.recip(ce[:], ce[:])
            fw.tt(ce[:], ce[:], ct[:], ALU.mult)
            fw.copy(cactB[:], bc(ce[:, :], 2, 128))
            fw.barrier()

        for l in range(depth):
            x_src = x_in if l == 0 else xs[1 + (l - 1) % 2]
            x_dst = y_out if l == depth - 1 else xs[1 + l % 2]
            last = (l == depth - 1)
            with ExitStack() as stp:
                psM = [ps(stp, f"psM{i}", [128, 512]) for i in range(2)]
                awt = [sb(stp, f"awt{i}", [128, 8, 512], F32) for i in range(2)]
                abt = sb(stp, "abt", [128, 6 * D], F32)
                g1t = sb(stp, "g1t", [128, D], F32)
                g2t = sb(stp, "g2t", [128, D], F32)
                fw.dma('sp', abt[:], ada_b[l])
                fw.dma('sp', g1t[:], g1_d[l])
                fw.dma('sp', g2t[:], g2_d[l])
                for cb in range(12):
                    a = awt[cb % 2]
                    fw.dma('sp', a[:], ada_w[l][:, cb * 512:(cb + 1) * 512].rearrange("(k p) n -> p k n", p=128))
                    fw.mmk(psM[cb % 2][:], [(cactB[:, k, :], a[:, k, :]) for k in range(8)])
                    fw.tt(mod[:, cb * 512:(cb + 1) * 512], psM[cb % 2][:], abt[:, cb * 512:(cb + 1) * 512], ALU.add)
                fw.stt(A1[:], mod[:, D:2 * D], 1.0, g1t[:], ALU.add, ALU.mult)
                fw.stt(A2[:], mod[:, 4 * D:5 * D], 1.0, g2t[:], ALU.add, ALU.mult)
                fw.barrier()
            sh1 = mod[:, 0:D]
            gt1 = mod[:, 2 * D:3 * D]
            sh2 = mod[:, 3 * D:4 * D]
            gt2 = mod[:, 5 * D:6 * D]

            with ExitStack() as stp:
                win = sb(stp, "win", [128, 8, INC], BF16)
                cosg = sb(stp, "cosg", [128, NT, 32], F32)
                sing = sb(stp, "sing", [128, NT, 32], F32)
                cosm = sb(stp, "cosm", [128, NT, 16], F32)
                sinm = sb(stp, "sinm", [128, NT, 16], F32)
                fw.dma('sp', cosg[:], cosg_d)
                fw.dma('sp', sing[:], sing_d)
                fw.dma('sp', cosm[:], cosm_d)
                fw.dma('sp', sinm[:], sinm_d)
                wst = [sb(stp, f"wst{i}", [128, INC], F32) for i in range(2)]
                for k in range(8):
                    fw.dma('sp', wst[k % 2][:], w_in[l][k * 128:(k + 1) * 128, :])
                    fw.copy(win[:, k, :], wst[k % 2][:], e=('pool' if k % 2 else 'dve'))
                wuq = sb(stp, "wuq", [128, 2, 384], BF16)
                wukv = sb(stp, "wukv", [128, 512], BF16)
                wuqs = sb(stp, "wuqs", [128, 2, 384], F32)
                wukvs = sb(stp, "wukvs", [128, 512], F32)
                gql = sb(stp, "gql", [128, 2], F32)
                gkvl = sb(stp, "gkvl", [128, 1], F32)
                fw.dma('sp', wuqs[:, 0, :], w_uq[l][0:128, :])
                fw.dma('sp', wuqs[0:64, 1, :], w_uq[l][128:192, :])
                fw.dma('sp', wukvs[:], w_ukv[l])
                fw.dma('sp', gql[:, 0:1], mql_g[l][0:128, :])
                fw.dma('sp', gql[0:64, 1:2], mql_g[l][128:192, :])
                fw.dma('sp', gkvl[:], mkvl_g[l])
                fw.ts(wuq[:, 0, :], wuqs[:, 0, :], gql[:, 0:1], None, ALU.mult)
                fw.ts(wuq[0:64, 1, :], wuqs[0:64, 1, :], gql[0:64, 1:2], None, ALU.mult)
                fw.ts(wukv[:], wukvs[:], gkvl[:, 0:1], None, ALU.mult)
                gains = {}
                for nm, dd, w_ in (("gq", gq_g, 64), ("gk", gk_g, 64), ("mqn", mqn_g, 64), ("mqr", mqr_g, 32),
                                   ("mkn", mkn_g, 64), ("mkr", mkr_g, 32)):
                    gains[nm] = sb(stp, "gn_" + nm, [128, w_], F32)
                    fw.dma('sp', gains[nm][:], dd[l])

                xt = [sb(stp, f"xt{i}", [128, D], F32) for i in range(2)]
                junk = sb(stp, "junk", [128, D], F32)
                ss = sb(stp, "ss", [128, 1], F32)
                hh = sb(stp, "hh", [128, D], F32)
                hT = sb(stp, "hT", [128, 8, 128], BF16)
                prs = [sb(stp, f"pr{i}", [128, INC], F32) for i in range(2)]
                qb_ = sb(stp, "qb_", [128, 384], BF16)
                kb_ = sb(stp, "kb_", [128, 128], BF16)
                vb_ = sb(stp, "vb_", [128, 2, 65], BF16)
                qTs = sb(stp, "qTs", [128, 3, 128], BF16)
                kTs = sb(stp, "kTs", [128, 128], BF16)
                cqn = sb(stp, "cqn", [128, 192], BF16)
                ckvn = sb(stp, "ckvn", [128, 128], BF16)
                cqT = sb(stp, "cqT", [128, 2, 128], BF16)
                ckvT = sb(stp, "ckvT", [128, 128], BF16)
                qup = sb(stp, "qup", [128, 384], F32)
                kvup = sb(stp, "kvup", [128, 512], F32)
                qm = sb(stp, "qm", [128, 4, 96], BF16)
                km = sb(stp, "km", [128, 4, 96], BF16)
                vmb = sb(stp, "vmb", [128, 4, 65], BF16)
                krr = sb(stp, "krr", [128, 32], F32)
                qTms = sb(stp, "qTms", [96, 4, 128], BF16)
                kTms = sb(stp, "kTms", [96, 4, 128], BF16)
                psW = ps(stp, "psW", [128, 8, 128])
                psP = [ps(stp, f"psP{i}", [128, 512]) for i in range(2)]
                psTb = ps(stp, "psTb", [128, 8, 128], BF16)
                psU = ps(stp, "psU", [128, 512])
                fw.memset(vb_[:], 1.0)
                fw.memset(vmb[:], 1.0)
                negA = sb(stp, "negA", [128, 12], F32)
                dtb = sb(stp, "dtb", [128, 12], F32)
                t12 = [sb(stp, f"t12_{i}", [128, 12], F32) for i in range(3)]
                fw.dma('sp', negA[:], alog_d[l])
                fw.dma('sp', dtb[:], dtb_d[l])
                fw.act(negA[:], negA[:], AF.Exp)
                fw.ts(negA[:], negA[:], -1.0, None, ALU.mult)

                tmpas = [sb(stp, f"tmpa_{i}", [128, 512], F32) for i in range(4)]
                tmpbs = [sb(stp, f"tmpb_{i}", [128, 512], F32) for i in range(2)]
                s6s = [sb(stp, f"s6_{i}", [128, 16], F32) for i in range(4)]
                nrms = [sb(stp, f"nrm_{i}", [128, 512], F32) for i in range(4)]

                def headnorm(src, H, Dh, gain, out, mean, sl):
                    t = tmpas[sl][:, 0:H * Dh].rearrange("p (h d) -> p h d", h=H)
                    s_ = s6s[sl]
                    fw.tt(t, src, src, ALU.mult)
                    yield
                    fw.red(s_[:, 0:H], t)
                    yield
                    fw.act(s_[:, 0:H], s_[:, 0:H], AF.Ln, bias=epsT[:], scale=(1.0 / Dh if mean else 1.0))
                    yield
                    fw.act(s_[:, 0:H], s_[:, 0:H], AF.Exp, scale=-0.5)
                    yield
                    fw.tt(t, src, bc(s_[:, 0:H], 2, Dh), ALU.mult)
                    yield
                    if gain is not None:
                        fw.tt(out, t, bc(gain[:, :], 1, H), ALU.mult)
                    else:
                        fw.copy(out, t)

                def rope(src, H, R, cs, sn, out, sl):
                    h2 = R // 2
                    x1 = src[:, :, 0:h2]
                    x2 = src[:, :, h2:R]
                    c_ = bc(cs, 1, H)
                    s_ = bc(sn, 1, H)
                    ta = tmpas[sl][:, 0:H * h2].rearrange("p (h d) -> p h d", h=H)
                    tb = tmpbs[sl][:, 0:H * h2].rearrange("p (h d) -> p h d", h=H)
                    fw.tt(ta, x1, c_, ALU.mult)
                    fw.tt(tb, x2, s_, ALU.mult)
                    yield
                    fw.tt(out[:, :, 0:h2], ta, tb, ALU.subtract)
                    yield
                    fw.tt(ta, x1, s_, ALU.mult)
                    fw.tt(tb, x2, c_, ALU.mult)
                    yield
                    fw.tt(out[:, :, h2:R], ta, tb, ALU.add)

                def run_il(gens):
                    gens = list(gens)
                    while gens:
                        for g_ in list(gens):
                            try:
                                next(g_)
                            except StopIteration:
                                gens.remove(g_)

                fw.dma('sp', xt[0][:], x_src[0:128, :])

                def frontA(i):
                    xc = xt[i % 2]
                    pr = prs[i % 2]
                    if i + 1 < NT:
                        fw.dma('sp', xt[(i + 1) % 2][:], x_src[(i + 1) * 128:(i + 2) * 128, :])
                    fw.act(junk[:], xc[:], AF.Square, accum_out=ss[:])
                    fw.act(ss[:], ss[:], AF.Ln, bias=epsT[:], scale=1.0 / D)
                    fw.act(ss[:], ss[:], AF.Exp, scale=-0.5)
                    fw.stt(hh[:], xc[:], ss[:, 0:1], A1[:], ALU.mult, ALU.mult)
                    fw.tt(hh[:], hh[:], sh1, ALU.add)
                    fw.tr([(psW[:, k, :], hh[:, k * 128:(k + 1) * 128], identf[:]) for k in range(8)])
                    fw.copy(hT[:], psW[:], e='act')
                    for cb in range(5):
                        c0 = cb * 512
                        c1 = min(INC, c0 + 512)
                        pp = psP[cb % 2]
                        fw.mmk(pp[:, 0:c1 - c0], [(hT[:, k, :], win[:, k, c0:c1]) for k in range(8)])
                        fw.copy(pr[:, c0:c1], pp[:, 0:c1 - c0], e=('dve' if cb % 2 == 0 else 'act'))

                def backA(i):
                    pr = prs[i % 2]
                    tsl = slice(i * 128, (i + 1) * 128)
                    fw.dma('pool', (dnpre[2 + i * 128:2 + (i + 1) * 128, :], ("dnpre", i)), pr[:, 0:1152])
                    fw.dma('pool', (dnz[tsl, :], ("dnz", i)), pr[:, 1152:1536])
                    fw.act(t12[0][:], pr[:, 1536:1548], AF.Exp, scale=-1.0)
                    fw.ts(t12[0][:], t12[0][:], 1.0, None, ALU.add)
                    fw.recip(betaT[:, i, :], t12[0][:])
                    fw.tt(t12[1][:], pr[:, 1548:1560], dtb[:], ALU.add)
                    fw.ts(t12[2][:], t12[1][:], -1.0, None, ALU.mult)
                    fw.tt(t12[2][:], t12[2][:], t12[1][:], ALU.max)
                    fw.act(t12[2][:], t12[2][:], AF.Exp, scale=-1.0)
                    fw.act(t12[2][:], t12[2][:], AF.Ln, bias=oneT[:], scale=1.0)
                    fw.ts(t12[1][:], t12[1][:], 0.0, None, ALU.max)
                    fw.tt(t12[1][:], t12[1][:], t12[2][:], ALU.add)
                    fw.tt(gT[:, i, :], t12[1][:], negA[:], ALU.mult)
                    qv = pr[:, 1560:1944].rearrange("p (h d) -> p h d", h=6)
                    nq = nrms[0][:, 0:384].rearrange("p (h d) -> p h d", h=6)
                    kv_ = pr[:, 1944:2072].rearrange("p (h d) -> p h d", h=2)
                    nk = nrms[1][:, 0:128].rearrange("p (h d) -> p h d", h=2)
                    cq = pr[:, 2200:2392].rearrange("p (h d) -> p h d", h=1)
                    ckv = pr[:, 2392:2520].rearrange("p (h d) -> p h d", h=1)
                    run_il([headnorm(qv, 6, 64, gains["gq"], nq, True, 0),
                            headnorm(kv_, 2, 64, gains["gk"], nk, True, 1),
                            headnorm(cq, 1, 192, None, cqn[:, :].rearrange("p (h d) -> p h d", h=1), True, 2),
                            headnorm(ckv, 1, 128, None, ckvn[:, :].rearrange("p (h d) -> p h d", h=1), True, 3)])
                    run_il([rope(nq, 6, 64, cosg[:, i, :], sing[:, i, :], qb_[:, :].rearrange("p (h d) -> p h d", h=6), 0),
                            rope(nk, 2, 64, cosg[:, i, :], sing[:, i, :], kb_[:, :].rearrange("p (h d) -> p h d", h=2), 1)])
                    fw.tr([(psTb[:, j, :], qb_[:, j * 128:(j + 1) * 128], identb[:]) for j in range(3)] +
                          [(psTb[:, 3, :], kb_[:], identb[:])] +
                          [(psTb[:, 4, :], cqn[:, 0:128], identb[:]),
                           (psTb[0:64, 5, :], cqn[:, 128:192], identb[:]),
                           (psTb[:, 6, :], ckvn[:], identb[:])])
                    fw.copy(cqT[:, 0, :], psTb[:, 4, :])
                    fw.copy(cqT[0:64, 1, :], psTb[0:64, 5, :])
                    fw.copy(ckvT[:], psTb[:, 6, :])
                    fw.copy(qTs[:], psTb[:, 0:3, :])
                    fw.copy(kTs[:], psTb[:, 3, :])
                    fw.mm([(psU[:, 0:384], cqT[:, 0, :], wuq[:, 0, :], True, False),
                           (psU[:, 0:384], cqT[0:64, 1, :], wuq[0:64, 1, :], False, True)])
                    fw.copy(qup[:], psU[:, 0:384])
                    fw.mm([(psU[:], ckvT[:], wukv[:], True, True)])
                    fw.copy(kvup[:], psU[:])
                    fw.dma('pool', (qTg[:, :, tsl].rearrange("j p t -> p j t"), ("qTg", i)), qTs[:])
                    fw.dma('pool', (kTg[:, tsl], ("kTg", i)), kTs[:])
                    fw.copy(vb_[:, :, 0:64], pr[:, 2072:2200].rearrange("p (h d) -> p h d", h=2), e='pool')
                    fw.dma('pool', (vg[tsl, :], ("vg", i)), vb_[:, :, :].rearrange("p h d -> p (h d)"))
                    qu = qup[:, :].rearrange("p (h d) -> p h d", h=4)
                    qmv = qm[:, :, :]
                    n4r = nrms[2][:, 0:128].rearrange("p (h d) -> p h d", h=4)
                    ku = kvup[:, :].rearrange("p (h d) -> p h d", h=4)
                    krv = pr[:, 2520:2552].rearrange("p (h d) -> p h d", h=1)
                    n1r = nrms[3][:, 0:32].rearrange("p (h d) -> p h d", h=1)
                    run_il([headnorm(qu[:, :, 0:64], 4, 64, gains["mqn"], qmv[:, :, 0:64], True, 0),
                            headnorm(qu[:, :, 64:96], 4, 32, gains["mqr"], n4r, True, 1),
                            headnorm(ku[:, :, 0:64], 4, 64, gains["mkn"], km[:, :, 0:64], True, 2),
                            headnorm(krv, 1, 32, gains["mkr"], n1r, True, 3)])
                    run_il([rope(n4r, 4, 32, cosm[:, i, :], sinm[:, i, :], qmv[:, :, 64:96], 0),
                            rope(n1r, 1, 32, cosm[:, i, :], sinm[:, i, :], krr[:, :].rearrange("p (h d) -> p h d", h=1), 1)])
                    fw.copy(km[:, :, 64:96], bc(krr[:, :], 1, 4), e='pool')
                    fw.copy(vmb[:, :, 0:64], ku[:, :, 64:128], e='pool')
                    fw.dma('pool', (vm[tsl, :], ("vm", i)), vmb[:, :, :].rearrange("p h d -> p (h d)"))
                    fw.tr([(psTb[0:96, hh_, :], qm[:, hh_, :], identb[:]) for hh_ in range(4)])
                    fw.copy(qTms[:], psTb[0:96, 0:4, :])
                    fw.dma('pool', (qTm[:, :, tsl].rearrange("h p t -> p h t"), ("qTm", i)), qTms[:])
                    fw.tr([(psTb[0:96, 4 + hh_, :], km[:, hh_, :], identb[:]) for hh_ in range(4)])
                    fw.copy(kTms[:], psTb[0:96, 4:8, :])
                    fw.dma('pool', (kTm[:, :, tsl].rearrange("h p t -> p h t"), ("kTm", i)), kTms[:])
                frontA(0)
                for i in range(NT):
                    if i + 1 < NT:
                        frontA(i + 1)
                    backA(i)
                fw.barrier()
            if stop_after == 'A':
                break

            with ExitStack() as stp:
                kT_all = sb(stp, "kT_all", [128, S], BF16)
                v_all = sb(stp, "v_all", [128, NT, 130], BF16)
                vm_all = sb(stp, "vm_all", [128, NT, 260], BF16)
                kTm_h = [sb(stp, f"kTm_h{i}", [96, S], BF16) for i in range(2)]
                qTt = [sb(stp, f"qTt{i}", [128, 512], BF16) for i in range(2)]
                pT = [sb(stp, f"pT{i}", [128, 2, 512], BF16) for i in range(2)]
                oTs = sb(stp, "oTs", [65, 512], F32)
                rcp = sb(stp, "rcp", [128, 4], F32)
                ob = [sb(stp, f"ob{i}", [128, 4, 64], BF16) for i in range(2)]
                psS = [ps(stp, f"psS{i}", [128, 2, 512]) for i in range(2)]
                acc = [ps(stp, f"acc{i}", [65, 512]) for i in range(2)]
                psO = ps(stp, "psO", [128, 4, 128])
                allq = [("qTg", i) for i in range(NT)]
                fw.dma('sp', kT_all[:], kTg, extra_reads=[("kTg", i) for i in range(NT)])
                fw.dma('sp', v_all[:], vg.rearrange("(t p) c -> p t c", p=128), extra_reads=[("vg", i) for i in range(NT)])
                fw.dma('sp', vm_all[:], vm.rearrange("(t p) c -> p t c", p=128), extra_reads=[("vm", i) for i in range(NT)])
                ucount = [0]

                pend = [None]

                def unit(kT_ap, qT_ap, vfn, scale, qb, col):
                    u = ucount[0]
                    ucount[0] += 1
                    ac = acc[u % 2]
                    NP = NT // 2

                    def Smm(kp):
                        pss = psS[kp % 2]
                        fw.mm([(pss[:, 0, :], kT_ap[:, (2 * kp) * 128:(2 * kp + 1) * 128], qT_ap, True, True),
                               (pss[:, 1, :], kT_ap[:, (2 * kp + 1) * 128:(2 * kp + 2) * 128], qT_ap, True, True)])
                    Smm(0)
                    if pend[0] is not None:
                        pend[0]()
                        pend[0] = None
                    for kp in range(NP):
                        if kp + 1 < NP:
                            Smm(kp + 1)
                        pt = pT[kp % 2]
                        fw.act(pt[:], psS[kp % 2][:], AF.Exp, scale=scale)
                        fw.mm([(ac[:], vfn(2 * kp), pt[:, 0, :], kp == 0, False),
                               (ac[:], vfn(2 * kp + 1), pt[:, 1, :], False, kp == NP - 1)])

                    def tail():
                        fw.copy(oTs[:], ac[:], e='dve')
                        fw.tr([(psO[:, s_, 0:65], oTs[:, s_ * 128:(s_ + 1) * 128], identf[0:65, 0:65]) for s_ in range(4)])
                        fw.recip(rcp[:], psO[:, :, 64])
                        o_ = ob[u % 2]
                        fw.tt(o_[:], psO[:, :, 0:64], bc(rcp[:, :], 2, 64), ALU.mult)
                        fw.dma('pool', (omix[qb * 512:(qb + 1) * 512, col:col + 64].rearrange("(s p) d -> p s d", p=128),
                                        ("omix", qb, col)), o_[:])
                    pend[0] = tail

                qcnt = 0
                for qb in range(NQB):
                    for j in range(3):
                        qt = qTt[qcnt % 2]
                        qcnt += 1
                        fw.dma('sp', qt[:], qTg[j, :, qb * 512:(qb + 1) * 512], extra_reads=allq)
                        for half in range(2):
                            head = j + 3 * half
                            rs = slice(half * 64, (half + 1) * 64)
                            unit(kT_all[rs, :], qt[rs, :],
                                 (lambda kt, half=half: v_all[:, kt, half * 65:(half + 1) * 65]),
                                 0.125, qb, 384 + head * 64)
                allqm = [("qTm", i) for i in range(NT)]
                for h in range(4):
                    kh = kTm_h[h % 2]
                    fw.dma('sp', kh[:], kTm[h], extra_reads=[("kTm", i) for i in range(NT)])
                    for qb in range(NQB):
                        qt = qTt[qcnt % 2]
                        qcnt += 1
                        fw.dma('sp', qt[0:96, :], qTm[h, :, qb * 512:(qb + 1) * 512], extra_reads=allqm)
                        unit(kh[:, :], qt[0:96, :], (lambda kt, h=h: vm_all[:, kt, h * 65:(h + 1) * 65]),
                             96 ** -0.5, qb, 768 + h * 64)
                if pend[0] is not None:
                    pend[0]()
                    pend[0] = None
                fw.barrier()
            if stop_after == 'B':
                break
            with ExitStack() as stp:
                wcv = sb(stp, "wcv", [128, 5, 1152], F32)
                fw.dma('sp', wcv[:], wconv_d[l].rearrange("p (j c) -> p j c", j=5))
                shf1 = [sb(stp, f"shf_{j}", [128, 1152], F32) for j in range(5)]
                shf = [shf1, shf1]
                cacc = sb(stp, "cacc", [128, 1152], F32)
                ctmp = sb(stp, "ctmp", [128, 1152], F32)
                qkvs = [sb(stp, f"qkv{i}", [128, 1152], F32) for i in range(2)]
                s12 = sb(stp, "s12", [128, 12], F32)
                qnb = sb(stp, "qnb", [128, 384], BF16)
                knb = sb(stp, "knb", [128, 384], BF16)
                kn32s = [sb(stp, f"kn32{i}", [128, 384], F32) for i in range(2)]
                qkTs = [sb(stp, f"qkT{i}", [64, 12, 128], BF16) for i in range(2)]
                gcss = [sb(stp, f"gcs{i}", [128, 24], F32) for i in range(2)]
                exs = [sb(stp, f"ex{i}", [128, 36], F32) for i in range(2)]
                Xd = sb(stp, "Xd", [128, 12, 128], F32)
                dec = sb(stp, "dec", [128, 12, 128], F32)
                Lt = sb(stp, "Lt", [128, 12, 128], F32)
                Pb = [sb(stp, f"Pb{i}", [128, 12, 128], F32) for i in range(2)]
                Qb = [sb(stp, f"Qb{i}", [128, 12, 128], F32) for i in range(2)]
                aqk = sb(stp, "aqk", [128, 12, 128], BF16)
                aqkT = sb(stp, "aqkT", [128, 12, 128], BF16)
                X32 = sb(stp, "X32", [128, 12, 128], F32)
                Wb = sb(stp, "Wb", [128, 12, 64], BF16)
                wT = sb(stp, "wT", [64, 12, 128], BF16)
                ktl = sb(stp, "ktl", [128, 12, 64], BF16)
                coef = sb(stp, "coef", [128, 12], F32)
                psA3 = ps(stp, "psA3", [128, 12, 128])
                psB3 = ps(stp, "psB3", [128, 12, 128])
                psT12f = ps(stp, "psT12", [128, 16, 128], BF16)
                psT12 = psT12f[:, 0:12, :]
                UTm, LTm = masks[:, 0, :], masks[:, 1, :]
                negm = masks[:, 2:4, :]
                strm = masks[:, 4:6, :]
                m0, m1s, m2s = masks[:, 6, :], masks[:, 7, :], masks[:, 8, :]
                bfn = {}
                for nm_ in ('L1Tb', 'L2Tb', 'T32b', 'T32Tb', 'Wz', 'T64b', 'T64Tb', 'Rb', 'Yb'):
                    bfn[nm_] = sb(stp, nm_, [128, 12, 128], BF16)

                def v4(t, w=128):
                    return t[:, :, :].rearrange("p (a h) d -> p a h d", a=2)

                def load_shift(i):
                    for j in range(5):
                        fw.dma('sp', shf[i % 2][j][:], dnpre[i * 128 + j:i * 128 + j + 128, :])
                load_shift(0)

                def frontC(i):
                    qkv, kn32, qkT, gcs, ex = qkvs[i % 2], kn32s[i % 2], qkTs[i % 2], gcss[i % 2], exs[i % 2]
                    sh = shf[i % 2]
                    tsl = slice(i * 128, (i + 1) * 128)
                    fw.tt(cacc[:], sh[0][:], wcv[:, 0, :], ALU.mult, e='pool')
                    for j in range(1, 5):
                        fw.tt(ctmp[:], sh[j][:], wcv[:, j, :], ALU.mult, e='pool')
                        fw.tt(cacc[:], cacc[:], ctmp[:], ALU.add)
                    if i + 1 < NT:
                        load_shift(i + 1)
                    fw.act(ctmp[:], cacc[:], AF.Exp, scale=-1.0)
                    fw.act(ctmp[:], ctmp[:], AF.Ln, bias=oneT[:], scale=1.0)
                    fw.act(ctmp[:], ctmp[:], AF.Exp, scale=-1.0)
                    fw.tt(qkv[:], cacc[:], ctmp[:], ALU.mult)
                    qk12 = qkv[:, 0:768].rearrange("p (h d) -> p h d", h=12)
                    c12 = cacc[:, 0:768].rearrange("p (h d) -> p h d", h=12)
                    fw.tt(c12, qk12, qk12, ALU.mult, e='pool')
                    fw.red(s12[:], c12)
                    fw.act(s12[:], s12[:], AF.Ln, bias=epsT[:], scale=1.0)
                    fw.act(s12[:], s12[:], AF.Exp, scale=-0.5)
                    fw.ts(s12[:, 0:6], s12[:, 0:6], 0.125, None, ALU.mult)
                    fw.tt(qnb[:, :].rearrange("p (h d) -> p h d", h=6), qk12[:, 0:6, :], bc(s12[:, 0:6], 2, 64), ALU.mult)
                    fw.tt(kn32[:, :].rearrange("p (h d) -> p h d", h=6), qk12[:, 6:12, :], bc(s12[:, 6:12], 2, 64), ALU.mult)
                    fw.copy(knb[:], kn32[:], e='pool')
                    fw.tr([(psT12[0:64, h, :], qnb[:, h * 64:(h + 1) * 64], identb[:]) for h in range(6)] +
                          [(psT12[0:64, 6 + h, :], knb[:, h * 64:(h + 1) * 64], identb[:]) for h in range(6)])
                    fw.copy(qkT[:], psT12[0:64, :, :], e='act')
                    fw.dma('pool', dQT[i].rearrange("p (h t) -> p h t", h=6), qkT[:, 0:6, :])
                    g_ = gT[:, i, :]
                    psG = psB3[:, 11, 0:24]
                    fw.mm([(psB3[:, 11, 0:6], UTm, g_[:, 0:6], True, True),
                           (psB3[:, 11, 6:12], LTm, g_[:, 6:12], True, True),
                           (psB3[:, 11, 12:24], onesf[:], g_, True, True)])
                    fw.copy(gcs[:], psG)
                    fw.act(ex[:, 0:24], gcs[:], AF.Exp)
                    fw.tt(ex[:, 24:36], gcs[:, 12:24], gcs[:, 0:12], ALU.subtract)
                    fw.act(ex[:, 24:36], ex[:, 24:36], AF.Exp)
                    fw.dma('pool', dE[tsl, :], ex[:, 0:24])

                def backC(i):
                    qkv, kn32, qkT, gcs, ex = qkvs[i % 2], kn32s[i % 2], qkTs[i % 2], gcss[i % 2], exs[i % 2]
                    tsl = slice(i * 128, (i + 1) * 128)
                    fw.tt(Xd[:], bc(identf[:, :], 1, 12), bc(gcs[:, 0:12], 2, 128), ALU.mult)
                    Xf = Xd[:, :, :].rearrange("p u j -> p (u j)")
                    Af = psA3[:, :, :].rearrange("p u j -> p (u j)")
                    fw.mm([(Af[:, c * 512:(c + 1) * 512], onesf[:], Xf[:, c * 512:(c + 1) * 512], True, True) for c in range(3)])
                    fw.tt(dec[:], bc(gcs[:, 0:12], 2, 128), psA3[:], ALU.subtract)
                    fw.tt(v4(dec), v4(dec), bc(negm, 2, 6), ALU.add)
                    fw.act(dec[:], dec[:], AF.Exp)
                    fw.mm([(psB3[:, h, :], qkT[:, 6 + h, :], qkT[:, 6 + h, :], True, True) for h in range(6)] +
                          [(psB3[:, 6 + h, :], qkT[:, h, :], qkT[:, 6 + h, :], True, True) for h in range(6)])
                    fw.tt(v4(Lt), v4(dec), bc(psB3[:, 0:6, :], 1, 2), ALU.mult)
                    fw.tt(v4(aqk), v4(dec), bc(psB3[:, 6:12, :], 1, 2), ALU.mult)
                    fw.stt(Xd[:], Lt[:], -1.0, bc(betaT[:, i, :], 2, 128), ALU.mult, ALU.mult)
                    fw.tr([(psB3[:, u, :], Xd[:, u, :], identf[:]) for u in range(12)])
                    fw.tt(Pb[0][:], Xd[:], bc(m0, 1, 12), ALU.mult, e='pool')
                    fw.tt(Qb[0][:], psB3[:], bc(m0, 1, 12), ALU.mult)
                    fw.tt(bfn['L1Tb'][:], psB3[:], bc(m1s, 1, 12), ALU.mult)
                    fw.tt(bfn['L2Tb'][:], psB3[:], bc(m2s, 1, 12), ALU.mult)
                    fw.tr([(psT12[:, u, :], aqk[:, u, :], identb[:]) for u in range(12)])
                    fw.copy(aqkT[:], psT12, e='act')
                    fw.dma('pool', dAT[i].rearrange("p (u t) -> p u t", u=12), aqkT[:])
                    fw.tt(coef[:], betaT[:, i, :], ex[:, 0:12], ALU.mult)
                    X4 = v4(bfn['Rb'])
                    v6 = qkv[:, 768:1152].rearrange("p (h d) -> p h d", h=6)
                    k6 = kn32[:, :].rearrange("p (h d) -> p h d", h=6)
                    b4 = betaT[:, i, :].rearrange("p (a h) -> p a h", a=2)
                    c4 = coef[:, :].rearrange("p (a h) -> p a h", a=2)
                    e4 = ex[:, 24:36].rearrange("p (a h) -> p a h", a=2)
                    fw.tt(X4[:, :, :, 0:64], bc(v6, 1, 2), bc(b4, 3, 64), ALU.mult)
                    fw.tt(X4[:, :, :, 64:128], bc(k6, 1, 2), bc(c4, 3, 64), ALU.mult, e='pool')
                    fw.tt(ktl[:, :, :].rearrange("p (a h) d -> p a h d", a=2), bc(k6, 1, 2), bc(e4, 3, 64), ALU.mult, e='pool')
                    fw.dma('pool', dKT[tsl, :], ktl[:, :, :].rearrange("p u d -> p (u d)"))
                    fw.tt(Lt[:], Pb[0][:], bc(identf[:, :], 1, 12), ALU.add)
                    for k in range(4):
                        P_, Q_ = Pb[k % 2], Qb[k % 2]
                        Pn, Qn = Pb[(k + 1) % 2], Qb[(k + 1) % 2]
                        fw.mm([(psA3[:, u, :], P_[:, u, :], Q_[:, u, :], True, True) for u in range(12)])
                        fw.copy(Qn[:], psA3[:], e='act')
                        if k < 3:
                            fw.mm([(psB3[:, u, :], Q_[:, u, :], P_[:, u, :], True, True) for u in range(12)])
                            fw.copy(Pn[:], psB3[:], e='act')
                        fw.mm([(psA3[:, u, :], Qn[:, u, :], Lt[:, u, :], True, True) for u in range(12)])
                        if k < 3:
                            fw.tt(Lt[:], Lt[:], psA3[:], ALU.add)
                        else:
                            fw.tt(bfn['T32b'][:], Lt[:], psA3[:], ALU.add)
                    fw.tr([(psT12[:, u, :], bfn['T32b'][:, u, :], identb[:]) for u in range(12)])
                    fw.copy(bfn['T32Tb'][:], psT12, e='act')
                    fw.mm([(psA3[:, u, :], bfn['L1Tb'][:, u, :], bfn['T32b'][:, u, :], True, True) for u in range(12)])
                    fw.copy(bfn['Wz'][:], psA3[:], e='act')
                    fw.mm([(psB3[:, u, :], bfn['T32Tb'][:, u, :], bfn['Wz'][:, u, :], True, True) for u in range(12)])
                    fw.tt(bfn['T64b'][:], bfn['T32b'][:], psB3[:], ALU.subtract)
                    fw.tr([(psT12[:, u, :], bfn['T64b'][:, u, :], identb[:]) for u in range(12)])
                    fw.copy(bfn['T64Tb'][:], psT12, e='act')
                    fw.mm([(psA3[:, u, :], bfn['T64Tb'][:, u, :], bfn['Rb'][:, u, :], True, True) for u in range(12)])
                    fw.copy(dec[:], psA3[:], e='act')
                    fw.copy(bfn['Yb'][:], dec[:], e='act')
                    fw.mm([(psB3[:, u, :], bfn['L2Tb'][:, u, :], bfn['Yb'][:, u, :], True, True) for u in range(12)])
                    fw.copy(bfn['Wz'][:], psB3[:], e='act')
                    fw.mm([(psA3[:, u, :], bfn['T64Tb'][:, u, :], bfn['Wz'][:, u, :], True, True) for u in range(12)])
                    fw.tt(X32[:], dec[:], psA3[:], ALU.subtract)
                    fw.dma('pool', dU[tsl, :].rearrange("p (u d) -> p u d", u=12), X32[:, :, 0:64])
                    fw.copy(Wb[:], X32[:, :, 64:128], e='act')
                    fw.tr([(psT12[0:64, u, :], Wb[:, u, :], identb[:]) for u in range(12)])
                    fw.copy(wT[:], psT12[0:64, :, :], e='act')
                    fw.dma('pool', dWT[i].rearrange("p (u t) -> p u t", u=12), wT[:])
                frontC(0)
                for i in range(NT):
                    if i + 1 < NT:
                        frontC(i + 1)
                    backC(i)
                fw.barrier()

            with ExitStack() as stp:
                S32 = [sb(stp, f"S32_{d}", [64, 6, 64], F32) for d in range(2)]
                Sb = [sb(stp, f"Sb_{d}", [64, 6, 64], BF16) for d in range(2)]
                for d in range(2):
                    fw.memset(S32[d][:], 0.0)
                    fw.memset(Sb[d][:], 0.0)
                Ud = [[sb(stp, f"Ud{d}{b}", [128, 6, 64], F32) for b in range(2)] for d in range(2)]
                WTd = [[sb(stp, f"WTd{d}{b}", [64, 6, 128], BF16) for b in range(2)] for d in range(2)]
                ATd = [[sb(stp, f"ATd{d}{b}", [128, 6, 128], BF16) for b in range(2)] for d in range(2)]
                KTd = [[sb(stp, f"KTd{d}{b}", [128, 6, 64], BF16) for b in range(2)] for d in range(2)]
                QTd = [[sb(stp, f"QTd{d}{b}", [64, 6, 128], BF16) for b in range(2)] for d in range(2)]
                Ed = [[sb(stp, f"Ed{d}{b}", [128, 24], F32) for b in range(2)] for d in range(2)]
                vnew = [sb(stp, f"vnew{d}", [128, 6, 64], BF16) for d in range(2)]
                ot = [sb(stp, f"ot{d}", [128, 6, 64], F32) for d in range(2)]
                od = [sb(stp, f"od{d}", [128, 6, 64], F32) for d in range(2)]
                stmp = [sb(stp, f"stmp{d}", [64, 6, 64], F32) for d in range(2)]
                psV = [ps(stp, f"psV{d}", [128, 8, 64])[:, 0:6, :] for d in range(2)]
                psO1 = [ps(stp, f"psO1{d}", [128, 8, 64])[:, 0:6, :] for d in range(2)]
                psO2 = [ps(stp, f"psO2{d}", [128, 8, 64])[:, 0:6, :] for d in range(2)]
                psS = [ps(stp, f"psS{d}", [64, 8, 64])[:, 0:6, :] for d in range(2)]

                def load_scan(s_):
                    for d in range(2):
                        n = s_ if d == 0 else NT - 1 - s_
                        b = s_ % 2
                        rows = slice(n * 128, (n + 1) * 128)
                        fw.dma('sp', Ud[d][b][:], dU[rows, d * 384:(d + 1) * 384].rearrange("p (h v) -> p h v", h=6))
                        fw.dma('sp', WTd[d][b][:], dWT[n][:, d * 768:(d + 1) * 768].rearrange("p (h t) -> p h t", h=6))
                        fw.dma('sp', ATd[d][b][:], dAT[n][:, d * 768:(d + 1) * 768].rearrange("p (h t) -> p h t", h=6))
                        fw.dma('sp', KTd[d][b][:], dKT[rows, d * 384:(d + 1) * 384].rearrange("p (h v) -> p h v", h=6))
                        fw.dma('sp', QTd[d][b][:], dQT[n].rearrange("p (h t) -> p h t", h=6))
                        fw.dma('sp', Ed[d][b][:], dE[rows, :])
                load_scan(0)
                for s_ in range(NT):
                    if s_ + 1 < NT:
                        load_scan(s_ + 1)
                    b = s_ % 2
                    ns = [s_, NT - 1 - s_]
                    for d in range(2):
                        fw.tt(stmp[d][:], S32[d][:], bc(Ed[d][b][0:64, 12 + d * 6:18 + d * 6], 2, 64), ALU.mult)
                    for d in range(2):
                        fw.mm([(psV[d][:, h, :], WTd[d][b][:, h, :], Sb[d][:, h, :], True, True) for h in range(6)] +
                              [(psO1[d][:, h, :], QTd[d][b][:, h, :], Sb[d][:, h, :], True, True) for h in range(6)])
                    for d in range(2):
                        fw.tt(vnew[d][:], Ud[d][b][:], psV[d][:], ALU.subtract)
                    for d in range(2):
                        fw.mm([(psS[d][:, h, :], KTd[d][b][:, h, :], vnew[d][:, h, :], True, True) for h in range(6)] +
                              [(psO2[d][:, h, :], ATd[d][b][:, h, :], vnew[d][:, h, :], True, True) for h in range(6)])
                    for d in range(2):
                        fw.tt(S32[d][:], stmp[d][:], psS[d][:], ALU.add)
                        fw.copy(Sb[d][:], S32[d][:], e='act')
                    for d in range(2):
                        fw.tt(ot[d][:], psO1[d][:], bc(Ed[d][b][:, d * 6:d * 6 + 6], 2, 64), ALU.mult)
                        fw.tt(od[d][:], ot[d][:], psO2[d][:], ALU.add)
                        fw.dma('pool', dO[d, ns[d] * 128:(ns[d] + 1) * 128, :], od[d][:, :, :].rearrange("p h v -> p (h v)"))
                fw.barrier()

            with ExitStack() as stp:
                gdn = sb(stp, "gdn", [128, 64], F32)
                fw.dma('sp', gdn[:], dng_d[l])
                o0 = [sb(stp, f"o0_{b}", [128, 384], F32) for b in range(2)]
                o1 = [sb(stp, f"o1_{b}", [128, 384], F32) for b in range(2)]
                zt_ = [sb(stp, f"zt_{b}", [128, 384], F32) for b in range(2)]
                osum = sb(stp, "osum", [128, 384], F32)
                otmp = sb(stp, "otmp", [128, 384], F32)
                ze = sb(stp, "ze", [128, 384], F32)
                s6c = sb(stp, "s6c", [128, 6], F32)
                oab = sb(stp, "oab", [128, 384], BF16)

                def load_c3(i):
                    fw.dma('sp', o0[i % 2][:], dO[0, i * 128:(i + 1) * 128, :])
                    fw.dma('sp', o1[i % 2][:], dO[1, i * 128:(i + 1) * 128, :])
                    fw.dma('sp', zt_[i % 2][:], dnz[i * 128:(i + 1) * 128, :])
                load_c3(0)
                for i in range(NT):
                    if i + 1 < NT:
                        load_c3(i + 1)
                    b = i % 2
                    fw.tt(osum[:], o0[b][:], o1[b][:], ALU.add)
                    o6 = osum[:, :].rearrange("p (h d) -> p h d", h=6)
                    t6 = otmp[:, :].rearrange("p (h d) -> p h d", h=6)
                    fw.tt(t6, o6, o6, ALU.mult)
                    fw.red(s6c[:], t6)
                    fw.act(s6c[:], s6c[:], AF.Ln, bias=epsT[:], scale=1.0 / 64)
                    fw.act(s6c[:], s6c[:], AF.Exp, scale=-0.5)
                    fw.tt(t6, o6, bc(s6c[:, :], 2, 64), ALU.mult)
                    fw.tt(t6, t6, bc(gdn[:, :], 1, 6), ALU.mult)
                    fw.act(ze[:], zt_[b][:], AF.Exp, scale=-1.0)
                    fw.ts(ze[:], ze[:], 1.0, None, ALU.add, e='pool')
                    fw.recip(ze[:], ze[:])
                    fw.tt(ze[:], ze[:], zt_[b][:], ALU.mult)
                    fw.tt(oab[:], otmp[:], ze[:], ALU.mult)
                    fw.dma('pool', omix[i * 128:(i + 1) * 128, 0:384], oab[:])
                fw.barrier()
            if stop_after == 'C':
                break

            with ExitStack() as stp:
                wo = sb(stp, "wo", [128, 8, D], BF16)
                wos = [sb(stp, f"wos{i}", [128, D], F32) for i in range(2)]
                for k in range(8):
                    fw.dma('sp', wos[k % 2][:], w_out[l][k * 128:(k + 1) * 128, :])
                    fw.copy(wo[:, k, :], wos[k % 2][:], e=('pool' if k % 2 else 'dve'))
                wr = sb(stp, "wr", [128, 8, 36], F32)
                br = sb(stp, "br", [128, 36], F32)
                fw.dma('sp', wr[:], w_r[l].rearrange("(k p) n -> p k n", p=128))
                fw.dma('sp', br[:], b_r[l])
                om = [sb(stp, f"om{i}", [128, D], BF16) for i in range(2)]
                xd = [sb(stp, f"xd{i}", [128, D], F32) for i in range(2)]
                oT = sb(stp, "oT", [128, 8, 128], BF16)
                x1 = sb(stp, "x1", [128, D], F32)
                dtmp = sb(stp, "dtmp", [128, D], F32)
                h2 = sb(stp, "h2", [128, D], F32)
                h2T32 = sb(stp, "h2T32", [128, 8, 128], F32)
                h2Tb = sb(stp, "h2Tb", [128, 8, 128], BF16)
                ssd = sb(stp, "ssd", [128, 1], F32)
                lg = sb(stp, "lg", [128, 36], F32)
                r1 = [sb(stp, f"r1_{i}", [128, 1], F32) for i in range(8)]
                ohg = sb(stp, "ohg", [128, 4], F32)
                eg4 = sb(stp, "eg4", [128, 4], F32)
                t32 = sb(stp, "t32", [128, 32], F32)
                esel = sb(stp, "esel", [128, 8], F32)
                es2 = sb(stp, "es2", [128, 8], F32)
                mk1 = sb(stp, "mk1", [128, 8], F32)
                mk2 = sb(stp, "mk2", [128, 8], F32)
                ge = sb(stp, "ge", [128, 8], F32)
                Gt = sb(stp, "Gt", [128, 32], F32)
                psTb2 = ps(stp, "psTb2", [128, 8, 128], BF16)
                psY2 = ps(stp, "psY2", [128, D])
                psW2 = ps(stp, "psW2", [128, 8, 128])
                psR = ps(stp, "psR", [128, 512])[:, 0:36]

                def load_d(i):
                    fw.dma('sp', om[i % 2][:], omix[i * 128:(i + 1) * 128, :])
                    fw.dma('sp', xd[i % 2][:], x_src[i * 128:(i + 1) * 128, :])
                load_d(0)
                for i in range(NT):
                    if i + 1 < NT:
                        load_d(i + 1)
                    tsl = slice(i * 128, (i + 1) * 128)
                    o_, x_ = om[i % 2], xd[i % 2]
                    fw.tr([(psTb2[:, k, :], o_[:, k * 128:(k + 1) * 128], identb[:]) for k in range(8)])
                    fw.copy(oT[:], psTb2[:], e='act')
                    fw.mm([(psY2[:, 0:512], oT[:, k, :], wo[:, k, 0:512], k == 0, k == 7) for k in range(8)] +
                          [(psY2[:, 512:1024], oT[:, k, :], wo[:, k, 512:1024], k == 0, k == 7) for k in range(8)])
                    fw.tt(dtmp[:], psY2[:], gt1, ALU.mult)
                    fw.tt(x1[:], dtmp[:], x_[:], ALU.add)
                    if not os.environ.get('SKIP_XS0'):
                        fw.dma('pool', xs[0][tsl, :], x1[:])
                    fw.act(dtmp[:], x1[:], AF.Square, accum_out=ssd[:])
                    fw.act(ssd[:], ssd[:], AF.Ln, bias=epsT[:], scale=1.0 / D)
                    fw.act(ssd[:], ssd[:], AF.Exp, scale=-0.5)
                    fw.stt(h2[:], x1[:], ssd[:, 0:1], A2[:], ALU.mult, ALU.mult)
                    fw.tt(h2[:], h2[:], sh2, ALU.add)
                    fw.tr([(psW2[:, k, :], h2[:, k * 128:(k + 1) * 128], identf[:]) for k in range(8)])
                    fw.copy(h2T32[:], psW2[:])
                    fw.copy(h2Tb[:], psW2[:], e='act')
                    if not os.environ.get('SKIP_H2TD'):
                        fw.dma('pool', h2Td[:, :, tsl], h2Tb[:])
                    if os.environ.get("SKIP_ROUTER"):
                        continue
                    fw.mmk(psR[:], [(h2T32[:, k, :], wr[:, k, :]) for k in range(8)])
                    fw.tt(lg[:], psR[:], br[:], ALU.add)
                    gm, ngm, sume, gtp, m1, m2, dd, w1_ = r1
                    fw.red(gm[:], lg[:, 0:4], op=ALU.max)
                    fw.ts(ohg[:], lg[:, 0:4], gm[:, 0:1], None, ALU.is_equal)
                    fw.ts(ngm[:], gm[:], -1.0, None, ALU.mult)
                    fw.act(eg4[:], lg[:, 0:4], AF.Exp, bias=ngm[:], scale=1.0, accum_out=sume[:])
                    fw.recip(gtp[:], sume[:])
                    fw.tt(t32[:, :].rearrange("p (g e) -> p g e", g=4), lg[:, 4:36].rearrange("p (g e) -> p g e", g=4),
                          bc(ohg[:, :], 2, 8), ALU.mult)
                    fw.red(esel[:], t32[:, :].rearrange("p (g e) -> p e g", g=4))
                    fw.red(m1[:], esel[:], op=ALU.max)
                    fw.ts(mk1[:], esel[:], m1[:, 0:1], None, ALU.is_equal)
                    fw.stt(es2[:], mk1[:], -1e30, esel[:], ALU.mult, ALU.add)
                    fw.red(m2[:], es2[:], op=ALU.max)
                    fw.ts(mk2[:], es2[:], m2[:, 0:1], None, ALU.is_equal)
                    fw.tt(dd[:], m2[:], m1[:], ALU.subtract)
                    fw.act(dd[:], dd[:], AF.Exp)
                    fw.ts(w1_[:], dd[:], 1.0, None, ALU.add)
                    fw.recip(w1_[:], w1_[:])
                    fw.tt(dd[:], dd[:], w1_[:], ALU.mult)
                    fw.tt(w1_[:], w1_[:], gtp[:], ALU.mult)
                    fw.tt(dd[:], dd[:], gtp[:], ALU.mult)
                    fw.ts(ge[:], mk1[:], w1_[:, 0:1], None, ALU.mult)
                    fw.stt(ge[:], mk2[:], dd[:, 0:1], ge[:], ALU.mult, ALU.add)
                    fw.tt(Gt[:, :].rearrange("p (g e) -> p g e", g=4), bc(ohg[:, :], 2, 8), bc(ge[:, :], 1, 4), ALU.mult)
                    fw.dma('pool', Gd[tsl, :], Gt[:])
                fw.barrier()

            if stop_after == 'D':
                break
            SBK = min(S, 2048)
            TPB = SBK // 128
            NB = SBK // 512
            with ExitStack() as stp:
                h2Ts = sb(stp, "h2Ts", [128, 8, SBK], BF16)
                yacc = sb(stp, "yacc", [128, TPB, D], F32)
                Gs = sb(stp, "Gs", [128, TPB, 32], F32)
                w1b = [sb(stp, f"w1b_{i}", [128, 8, 256], BF16) for i in range(2)]
                w3b = [sb(stp, f"w3b_{i}", [128, 8, 256], BF16) for i in range(2)]
                w2b = [sb(stp, f"w2b_{i}", [128, 2, D], BF16) for i in range(2)]
                st1 = sb(stp, "st1", [128, 8, 256], F32)
                st3 = sb(stp, "st3", [128, 8, 256], F32)
                st2 = sb(stp, "st2", [128, 2, D], F32)
                e1_ = [sb(stp, f"e1_{i}", [128, 512], F32) for i in range(2)]
                p_ = [sb(stp, f"p_{i}", [128, 512], F32) for i in range(2)]
                hidT = [sb(stp, f"hidT{i}", [128, 2, 512], BF16) for i in range(2)]
                xe1 = st1[:, 0:4, :].rearrange("p k n -> p (k n)")
                xo1 = st3[:, 0:4, :].rearrange("p k n -> p (k n)")
                psH1 = [ps(stp, f"psH1{i}", [128, 512]) for i in range(2)]
                psH3 = [ps(stp, f"psH3{i}", [128, 512]) for i in range(2)]
                psY = [ps(stp, f"psY{i}", [128, D]) for i in range(2)]
                bcount = 0
                ycount = [0]
                pendE = [None]
                for sbk in range(S // SBK):
                    t0 = sbk * SBK
                    fw.dma('sp', h2Ts[:], h2Td[:, :, t0:t0 + SBK])
                    fw.dma('sp', Gs[:], Gd[t0:t0 + SBK, :].rearrange("(t p) e -> p t e", p=128))
                    for e_ in range(32):
                        wa1, wa3, wb2 = w1b[e_ % 2], w3b[e_ % 2], w2b[e_ % 2]
                        fw.dma('sp', st1[:], moe_w1[l, e_].rearrange("(k p) n -> p k n", p=128))
                        fw.dma('sp', st3[:], moe_w3[l, e_].rearrange("(k p) n -> p k n", p=128))
                        fw.dma('sp', st2[:], moe_w2[l, e_].rearrange("(k p) n -> p k n", p=128))
                        fw.copy(wa1[:], st1[:], e='pool')
                        fw.copy(wa3[:], st3[:], e='pool')
                        fw.copy(wb2[:], st2[:], e='pool')
                        for b in range(NB):
                            hT_ = hidT[bcount % 2]
                            bcount += 1
                            tk = slice(b * 512, (b + 1) * 512)
                            for c in range(2):
                                fs = slice(c * 128, (c + 1) * 128)
                                fw.mm([(psH1[c][:], wa1[:, k, fs], h2Ts[:, k, tk], k == 0, k == 7) for k in range(8)] +
                                      [(psH3[c][:], wa3[:, k, fs], h2Ts[:, k, tk], k == 0, k == 7) for k in range(8)])
                                fw.act(e1_[c][:], psH1[c][:], AF.Exp, scale=-1.0)
                                fw.act(e1_[c][:], e1_[c][:], AF.Ln, bias=oneT[:], scale=1.0)
                                fw.act(e1_[c][:], e1_[c][:], AF.Exp, scale=-1.0)
                                fw.tt(p_[c][:], psH1[c][:], e1_[c][:], ALU.mult)
                                fw.tt(hT_[:, c, :], p_[c][:], psH3[c][:], ALU.mult)
                                if pendE[0] is not None:
                                    pendE[0](c)
                                    if c == 1:
                                        pendE[0] = None

                            def mk(bb=b, hh=hT_, ee=e_, wb=wb2):
                                def f(half):
                                    for t4 in ((0, 1, 2, 3) if half is None else (2 * half, 2 * half + 1)):
                                        t = bb * 4 + t4
                                        py = psY[ycount[0] % 2]
                                        ycount[0] += 1
                                        ts4 = slice(t4 * 128, (t4 + 1) * 128)
                                        fw.mm([(py[:, 0:512], hh[:, j, ts4], wb[:, j, 0:512], j == 0, j == 1) for j in range(2)] +
                                              [(py[:, 512:1024], hh[:, j, ts4], wb[:, j, 512:1024], j == 0, j == 1) for j in range(2)])
                                        if ee == 0:
                                            fw.ts(yacc[:, t, :], py[:], Gs[:, t, ee:ee + 1], None, ALU.mult)
                                        else:
                                            fw.stt(yacc[:, t, :], py[:], Gs[:, t, ee:ee + 1], yacc[:, t, :], ALU.mult, ALU.add)
                                return f
                            pendE[0] = mk()
                    if pendE[0] is not None:
                        pendE[0](None)
                        pendE[0] = None
                    for t in range(TPB):
                        rows = slice(t0 + t * 128, t0 + (t + 1) * 128)
                        fw.dma('sp', xe1, xs[0][rows, :])
                        fw.tt(xo1, yacc[:, t, :], gt2, ALU.mult, e='pool')
                        fw.tt(xo1, xo1, xe1, ALU.add)
                        fw.dma('pool', x_dst[rows, :], xo1)
                fw.barrier()

        if dbg and os.environ.get("DBG_DN"):
            with ExitStack() as stp:
                t_f = sb(stp, "dbgdn_f", [128, D], F32)
                fw.memset(t_f[:], 0.0)
                for i in range(NT):
                    rows = slice(i * 128, (i + 1) * 128)
                    if os.environ.get("DBG_DN") == "U":
                        fw.dma('sp', t_f[:, 0:768], dU[rows, :])
                    else:
                        fw.dma('sp', t_f[:, 0:384], dO[0, rows, :])
                        fw.dma('sp', t_f[:, 384:768], dO[1, rows, :])
                    fw.dma('sp', t_f[:, 768:792], dE[rows, :])
                    fw.dma('sp', t_f[:, 800:812], gT[:, i, :])
                    fw.dma('sp', t_f[:, 812:824], betaT[:, i, :])
                    fw.dma('sp', dbg_out[rows, :], t_f[:])
                fw.barrier()
        elif dbg and stop_after in ('B', 'C'):
            with ExitStack() as stp:
                t_b = sb(stp, "dbg_b", [128, D], BF16)
                t_f = sb(stp, "dbg_f", [128, D], F32)
                for i in range(NT):
                    fw.dma('sp', t_b[:], omix[i * 128:(i + 1) * 128, :],
                           extra_reads=[("omix", qb, col) for qb in range(NQB) for col in range(384, 1024, 64)])
                    fw.copy(t_f[:], t_b[:])
                    fw.dma('sp', dbg_out[i * 128:(i + 1) * 128, :], t_f[:])
                fw.barrier()
        fw.barrier()
        print("instructions emitted:", fw.ninst)
    return nc


def rope_tables(S, rot_dim, grid_w=64):
    t = np.arange(S)
    row = (t // grid_w).astype(np.float32)
    col = (t % grid_w).astype(np.float32)
    n_freq = rot_dim // 4
    inv = (10000.0 ** (-np.arange(n_freq, dtype=np.float32) / n_freq)).astype(np.float32)
    ang = np.concatenate([row[:, None] * inv, col[:, None] * inv], axis=-1).astype(np.float32)
    return np.cos(ang).astype(np.float32), np.sin(ang).astype(np.float32)


def rep(v, n=128):
    v = np.asarray(v, np.float32)
    return np.ascontiguousarray(np.broadcast_to(v[..., None, :], v.shape[:-1] + (n, v.shape[-1])))


def prep_inputs(inp, S, depth):
    NT = S // 128
    f = lambda a: np.ascontiguousarray(np.asarray(a, np.float32))
    perm = np.arange(INC)
    base = 1560
    order = [0, 3, 1, 4, 2, 5]
    perm[base:base + 384] = np.concatenate([base + h * 64 + np.arange(64) for h in order])
    com = {}
    com["ada_w"] = f(inp["ada_w"][:depth])
    com["ada_b"] = rep(inp["ada_b"][:depth])
    com["g1"] = rep(inp["norm1_g"][:depth])
    com["g2"] = rep(inp["norm2_g"][:depth])
    com["w_in"] = f(np.asarray(inp["w_in"])[:depth][:, :, perm])
    com["w_out"] = f(inp["w_out"][:depth])
    com["gq_g"] = rep(inp["gqa_q_g"][:depth])
    com["gk_g"] = rep(inp["gqa_k_g"][:depth])
    com["mql_g"] = f(np.asarray(inp["mla_q_lat_g"])[:depth, :, None])
    com["mkvl_g"] = f(np.asarray(inp["mla_kv_lat_g"])[:depth, :, None])
    com["w_uq"] = f(inp["mla_w_uq"][:depth])
    com["w_ukv"] = f(inp["mla_w_ukv"][:depth])
    com["mqn_g"] = rep(inp["mla_qn_g"][:depth])
    com["mqr_g"] = rep(inp["mla_qr_g"][:depth])
    com["mkn_g"] = rep(inp["mla_kn_g"][:depth])
    com["mkr_g"] = rep(inp["mla_kr_g"][:depth])
    cg, sg = rope_tables(S, 64)
    cm, sm = rope_tables(S, 32)
    tm = lambda a: np.ascontiguousarray(a.reshape(NT, 128, -1).transpose(1, 0, 2))
    com["cosg"], com["sing"], com["cosm"], com["sinm"] = tm(cg), tm(sg), tm(cm), tm(sm)
    com["identf"] = np.eye(128, dtype=np.float32)
    com["w_r"] = f(np.concatenate([np.asarray(inp["moe_w_group"])[:depth], np.asarray(inp["moe_w_router"])[:depth]], axis=-1))
    com["b_r"] = rep(np.concatenate([np.asarray(inp["moe_b_group"])[:depth], np.asarray(inp["moe_b_router"])[:depth]], axis=-1))
    com["moe_w1"] = f(inp["moe_w1"][:depth])
    com["moe_w3"] = f(inp["moe_w3"][:depth])
    com["moe_w2"] = f(inp["moe_w2"][:depth])
    com["wconv"] = rep(np.asarray(inp["dn_conv"])[:depth].reshape(depth, 5 * 1152))
    com["alog"] = rep(np.asarray(inp["dn_a_log"])[:depth].reshape(depth, 12))
    com["dtb"] = rep(np.asarray(inp["dn_dt_bias"])[:depth].reshape(depth, 12))
    com["dng"] = rep(inp["dn_out_g"][:depth])
    ii = np.arange(128)[:, None]
    jj = np.arange(128)[None, :]
    NEG = -30000.0
    mk = np.stack([(ii <= jj), (ii >= jj),
                   np.where(jj <= ii, 0.0, NEG), np.where(jj >= ii, 0.0, NEG),
                   (jj < ii), (jj > ii),
                   (ii // 32 == jj // 32) & (ii != jj), (ii // 64 == jj // 64) & (ii // 32 != jj // 32), (ii // 64 != jj // 64)],
                  axis=1).astype(np.float32)
    mk[:, 7:9, :] *= -1.0
    com["masks"] = np.ascontiguousarray(mk)
    return com


def kernel(**inp):
    S, depth = 4096, 4
    com = prep_inputs(inp, S, depth)
    x = np.asarray(inp["x"], np.float32)
    c = np.asarray(inp["c"], np.float32)
    nc = build(S, depth)
    active = [0, 1, 4, 5]
    zero_com = {k: np.zeros_like(v) for k, v in com.items()}
    zero_com["x"] = np.zeros((S, D), np.float32)
    zero_com["c_pk"] = np.zeros((128, 8), np.float32)
    maps = []
    for core in range(8):
        if core in active:
            b = active.index(core)
            m = dict(com)
            m["x"] = np.ascontiguousarray(x[b])
            m["c_pk"] = np.ascontiguousarray(c[b].reshape(8, 128).T)
        else:
            m = zero_com
        maps.append(m)
    res = run_bass_kernel_spmd(nc, maps, core_ids=list(range(8)))
    return np.stack([res.results[core]["y"] for core in active], axis=0).astype(np.float32)
```
